# Optimizing a Trainium2 kernel written in Bass

```python
import math
import jax, jax.numpy as jnp
from jax import lax
import numpy as np

D_MODEL = 1024
BATCH = 4
SEQ = 8192
DEPTH = 2

CHUNK = 64
EPS = 1e-6
F32 = jnp.float32
NEG_INF_SCORE = -1e30

SSM_WIDTH = 256
SSM_GROUP = 16
SSM_GROUPS = SSM_WIDTH // SSM_GROUP
SSM_STATE = 64
SSM_DT_MIN = 1e-3
SSM_DT_MAX = 1e-1

DSA_HEADS = 6
DSA_HEAD_DIM = 64
DSA_WIDTH = DSA_HEADS * DSA_HEAD_DIM
IDX_HEADS = 4
IDX_DIM = 32
DSA_TOPK = 256
Q_BLOCK = 128
ROPE_THETA = 500000.0
ROPE_FRACTION = 4

RET_HEADS = 4
RET_QK_DIM = 48
RET_V_DIM = 96
RET_WIDTH = RET_HEADS * RET_V_DIM
RET_THETA = 10000.0

N_BRANCHES = 3
D_FF = 4 * D_MODEL

IN_SIZES = (
    SSM_WIDTH,
    DSA_WIDTH,
    DSA_HEAD_DIM,
    DSA_HEAD_DIM,
    IDX_HEADS * IDX_DIM,
    IDX_DIM,
    IDX_HEADS,
    RET_HEADS * RET_QK_DIM,
    RET_HEADS * RET_QK_DIM,
    RET_WIDTH,
    RET_WIDTH,
    N_BRANCHES * D_MODEL,
)
IN_DIM = sum(IN_SIZES)

kernel_name = 'hybrid_s5_dsa_retention_block'


def rms_norm(x, g):
    xf = x.astype(F32)
    y = xf * lax.rsqrt(jnp.mean(xf * xf, axis=-1, keepdims=True) + EPS)
    return (y * g.astype(F32)).astype(x.dtype)


def rope(x, positions, rot_dim, theta):
    half = rot_dim // 2
    inv = jnp.exp(-math.log(theta) * jnp.arange(half, dtype=F32) * (2.0 / rot_dim))
    ang = positions.astype(F32)[:, :, None, None] * inv
    cos, sin = jnp.cos(ang), jnp.sin(ang)
    xf = x.astype(F32)
    x1 = xf[..., :half]
    x2 = xf[..., half:rot_dim]
    out = jnp.concatenate([x1 * cos - x2 * sin, x2 * cos + x1 * sin, xf[..., rot_dim:]], axis=-1)
    return out.astype(x.dtype)


def split_cols(z):
    parts = []
    off = 0
    for n in IN_SIZES:
        parts.append(z[..., off:off + n])
        off += n
    return parts


def s5_mixer(u, lam_re, lam_im, log_step, b_re, b_im, c_re, c_im, d_skip, glu_w, glu_b):
    bsz, seq_len, _ = u.shape
    uf = u.astype(F32).reshape(bsz, seq_len, SSM_GROUPS, SSM_GROUP)
    lam_re = lam_re.astype(F32)
    lam_im = lam_im.astype(F32)
    step = jnp.exp(log_step.astype(F32))[:, None]
    mag = jnp.exp(lam_re * step)
    lb_re = mag * jnp.cos(lam_im * step)
    lb_im = mag * jnp.sin(lam_im * step)
    den = lam_re * lam_re + lam_im * lam_im
    f_re = ((lb_re - 1.0) * lam_re + lb_im * lam_im) / den
    f_im = (lb_im * lam_re - (lb_re - 1.0) * lam_im) / den
    b_re = b_re.astype(F32)
    b_im = b_im.astype(F32)
    bb_re = f_re[..., None] * b_re - f_im[..., None] * b_im
    bb_im = f_re[..., None] * b_im + f_im[..., None] * b_re
    bu_re = jnp.einsum('blgc,gpc->blgp', uf, bb_re)
    bu_im = jnp.einsum('blgc,gpc->blgp', uf, bb_im)
    a_re = jnp.broadcast_to(lb_re[None, None], (1, seq_len, SSM_GROUPS, SSM_STATE))
    a_im = jnp.broadcast_to(lb_im[None, None], (1, seq_len, SSM_GROUPS, SSM_STATE))

    def combine(ei, ej):
        ar_i, ai_i, br_i, bi_i = ei
        ar_j, ai_j, br_j, bi_j = ej
        ar = ar_j * ar_i - ai_j * ai_i
        ai = ar_j * ai_i + ai_j * ar_i
        br = ar_j * br_i - ai_j * bi_i + br_j
        bi = ar_j * bi_i + ai_j * br_i + bi_j
        return (ar, ai, br, bi)

    _, _, x_re, x_im = lax.associative_scan(combine, (a_re, a_im, bu_re, bu_im), axis=1)
    y = (jnp.einsum('blgp,gcp->blgc', x_re, c_re.astype(F32))
         - jnp.einsum('blgp,gcp->blgc', x_im, c_im.astype(F32))
         + d_skip.astype(F32) * uf)
    y = jax.nn.gelu(y.reshape(bsz, seq_len, SSM_WIDTH))
    y = y * jax.nn.sigmoid(y @ glu_w.astype(F32) + glu_b.astype(F32))
    return y.astype(u.dtype)


def dsa_mixer(q, k, v, q_idx, k_idx, w_idx):
    bsz, seq_len = q.shape[:2]
    topk = min(DSA_TOPK, seq_len // 4)
    nblk = seq_len // Q_BLOCK
    key_pos = jnp.arange(seq_len)
    kif = k_idx.astype(F32)

    def to_blocks(a):
        return a.reshape((bsz, nblk, Q_BLOCK) + a.shape[2:]).swapaxes(0, 1)

    def block(args):
        qb, qib, wb, start = args
        qpos = start + jnp.arange(Q_BLOCK)
        visible_end = (qpos // CHUNK + 1) * CHUNK
        allowed = key_pos[None, :] < visible_end[:, None]
        logits = jnp.einsum('bqhd,bsd->bqhs', qib.astype(F32), kif) * (IDX_DIM ** -0.5)
        score = jnp.einsum('bqhs,bqh->bqs', jax.nn.relu(logits), wb.astype(F32) * (IDX_HEADS ** -0.5))
        score = jnp.where(allowed[None], score, -jnp.inf)
        top_val, top_idx = lax.top_k(score, topk)
        valid = top_val > -jnp.inf
        kg = jax.vmap(lambda a, i: a[i])(k, top_idx)
        vg = jax.vmap(lambda a, i: a[i])(v, top_idx)
        s = jnp.einsum('bqhd,bqkd->bqhk', qb, kg).astype(F32) * (DSA_HEAD_DIM ** -0.5)
        s = jnp.where(valid[:, :, None, :], s, NEG_INF_SCORE)
        p = jax.nn.softmax(s, axis=-1).astype(vg.dtype)
        return jnp.einsum('bqhk,bqkd->bqhd', p, vg)

    starts = jnp.arange(nblk) * Q_BLOCK
    out = lax.map(block, (to_blocks(q), to_blocks(q_idx), to_blocks(w_idx), starts))
    return out.swapaxes(0, 1).reshape(bsz, seq_len, DSA_WIDTH)


def retention_mixer(q, k, v, gate, norm_g):
    bsz, seq_len = q.shape[:2]
    nc = seq_len // CHUNK
    qf = q.astype(F32)
    kf = k.astype(F32) * (RET_QK_DIM ** -0.5)
    vf = v.astype(F32)

    def to_chunks(a):
        return a.reshape(bsz, nc, CHUNK, RET_HEADS, a.shape[-1]).transpose(1, 0, 3, 2, 4)

    log_g = jnp.log1p(-jnp.exp2(-5.0 - jnp.arange(RET_HEADS, dtype=F32)))
    pos = jnp.arange(CHUNK, dtype=F32)
    diff = pos[:, None] - pos[None, :]
    intra = jnp.where(diff >= 0, jnp.exp(log_g[:, None, None] * jnp.maximum(diff, 0.0)), 0.0)
    q_dec = jnp.exp(log_g[:, None] * (pos + 1.0))[None, :, :, None]
    k_dec = jnp.exp(log_g[:, None] * (CHUNK - 1.0 - pos))[None, :, :, None]
    c_dec = jnp.exp(log_g * CHUNK)[None, :, None, None]

    def step(state, inp):
        qc, kc, vc = inp
        inner = jnp.einsum('bhcm,bhme->bhce', jnp.einsum('bhcd,bhmd->bhcm', qc, kc) * intra, vc)
        cross = jnp.einsum('bhcd,bhde->bhce', qc, state) * q_dec
        state = state * c_dec + jnp.einsum('bhmd,bhme->bhde', kc * k_dec, vc)
        return state, inner + cross

    init = jnp.zeros((bsz, RET_HEADS, RET_QK_DIM, RET_V_DIM), F32)
    _, ys = lax.scan(step, init, (to_chunks(qf), to_chunks(kf), to_chunks(vf)))
    y = ys.transpose(1, 0, 3, 2, 4).reshape(bsz, seq_len, RET_HEADS, RET_V_DIM)
    mu = jnp.mean(y, axis=-1, keepdims=True)
    var = jnp.mean(jnp.square(y - mu), axis=-1, keepdims=True)
    yn = ((y - mu) * lax.rsqrt(var + EPS)).reshape(bsz, seq_len, RET_WIDTH) * norm_g.astype(F32)
    return (jax.nn.silu(gate.astype(F32)) * yn).astype(gate.dtype)


def setup_inputs(seed: int = 0) -> dict:
    key = jax.random.key(seed)
    ks = jax.random.split(key, 24)

    def nrm(k, shape, scale):
        return jax.random.normal(k, shape, F32) * scale

    x = nrm(ks[0], (BATCH, SEQ, D_MODEL), 1.0)
    start = jax.random.randint(ks[1], (BATCH, 1), 0, 64, dtype=jnp.int32) * CHUNK
    positions = (start + jnp.arange(SEQ, dtype=jnp.int32)[None, :]).astype(jnp.int32)
    norm1_g = 1.0 + nrm(ks[2], (DEPTH, D_MODEL), 0.02)
    w_in = nrm(ks[3], (DEPTH, D_MODEL, IN_DIM), D_MODEL ** -0.5)
    ssm_lambda_re = -0.5 + nrm(ks[4], (DEPTH, SSM_GROUPS, SSM_STATE), 0.01)
    ssm_lambda_im = jnp.broadcast_to(math.pi * jnp.arange(SSM_STATE, dtype=F32), (DEPTH, SSM_GROUPS, SSM_STATE))
    ssm_log_step = jax.random.uniform(ks[5], (DEPTH, SSM_GROUPS), F32, math.log(SSM_DT_MIN), math.log(SSM_DT_MAX))
    ssm_b_re = nrm(ks[6], (DEPTH, SSM_GROUPS, SSM_STATE, SSM_GROUP), (2 * SSM_GROUP) ** -0.5)
    ssm_b_im = nrm(ks[7], (DEPTH, SSM_GROUPS, SSM_STATE, SSM_GROUP), (2 * SSM_GROUP) ** -0.5)
    ssm_c_re = nrm(ks[8], (DEPTH, SSM_GROUPS, SSM_GROUP, SSM_STATE), SSM_STATE ** -0.5)
    ssm_c_im = nrm(ks[9], (DEPTH, SSM_GROUPS, SSM_GROUP, SSM_STATE), SSM_STATE ** -0.5)
    ssm_d = nrm(ks[10], (DEPTH, SSM_GROUPS, SSM_GROUP), 1.0)
    ssm_glu_w = nrm(ks[11], (DEPTH, SSM_WIDTH, SSM_WIDTH), SSM_WIDTH ** -0.5)
    ssm_glu_b = nrm(ks[12], (DEPTH, SSM_WIDTH), 0.02)
    ret_norm_g = 1.0 + nrm(ks[13], (DEPTH, RET_WIDTH), 0.02)
    w_proj_a = nrm(ks[14], (DEPTH, SSM_WIDTH, D_MODEL), SSM_WIDTH ** -0.5)
    w_proj_b = nrm(ks[15], (DEPTH, DSA_WIDTH, D_MODEL), DSA_WIDTH ** -0.5)
    w_proj_c = nrm(ks[16], (DEPTH, RET_WIDTH, D_MODEL), RET_WIDTH ** -0.5)
    w_out = nrm(ks[17], (DEPTH, D_MODEL, D_MODEL), D_MODEL ** -0.5)
    norm2_g = 1.0 + nrm(ks[18], (DEPTH, D_MODEL), 0.02)
    w_ff1 = nrm(ks[19], (DEPTH, D_MODEL, D_FF), D_MODEL ** -0.5)
    w_ff2 = nrm(ks[20], (DEPTH, D_FF, D_MODEL), D_FF ** -0.5)
    final_norm_g = 1.0 + nrm(ks[21], (D_MODEL,), 0.02)
    return {'x': x, 'positions': positions, 'norm1_g': norm1_g, 'w_in': w_in,
            'ssm_lambda_re': ssm_lambda_re, 'ssm_lambda_im': ssm_lambda_im, 'ssm_log_step': ssm_log_step,
            'ssm_b_re': ssm_b_re, 'ssm_b_im': ssm_b_im, 'ssm_c_re': ssm_c_re, 'ssm_c_im': ssm_c_im,
            'ssm_d': ssm_d, 'ssm_glu_w': ssm_glu_w, 'ssm_glu_b': ssm_glu_b, 'ret_norm_g': ret_norm_g,
            'w_proj_a': w_proj_a, 'w_proj_b': w_proj_b, 'w_proj_c': w_proj_c, 'w_out': w_out,
            'norm2_g': norm2_g, 'w_ff1': w_ff1, 'w_ff2': w_ff2, 'final_norm_g': final_norm_g}


def reference(x, positions, norm1_g, w_in, ssm_lambda_re, ssm_lambda_im, ssm_log_step,
              ssm_b_re, ssm_b_im, ssm_c_re, ssm_c_im, ssm_d, ssm_glu_w, ssm_glu_b, ret_norm_g,
              w_proj_a, w_proj_b, w_proj_c, w_out, norm2_g, w_ff1, w_ff2, final_norm_g):
    bsz, seq_len, _ = x.shape
    for l in range(DEPTH):
        h = rms_norm(x, norm1_g[l])
        z = h @ w_in[l]
        (u_ssm, dq, dk, dv, iq, ik, iw, rq, rk, rv, rg, gates) = split_cols(z)

        ya = s5_mixer(u_ssm, ssm_lambda_re[l], ssm_lambda_im[l], ssm_log_step[l], ssm_b_re[l], ssm_b_im[l],
                      ssm_c_re[l], ssm_c_im[l], ssm_d[l], ssm_glu_w[l], ssm_glu_b[l])

        dq = rope(dq.reshape(bsz, seq_len, DSA_HEADS, DSA_HEAD_DIM), positions, DSA_HEAD_DIM // ROPE_FRACTION, ROPE_THETA)
        dk = rope(dk.reshape(bsz, seq_len, 1, DSA_HEAD_DIM), positions, DSA_HEAD_DIM // ROPE_FRACTION, ROPE_THETA)[:, :, 0]
        iq = rope(iq.reshape(bsz, seq_len, IDX_HEADS, IDX_DIM), positions, IDX_DIM // ROPE_FRACTION, ROPE_THETA)
        ik = rope(ik.reshape(bsz, seq_len, 1, IDX_DIM), positions, IDX_DIM // ROPE_FRACTION, ROPE_THETA)[:, :, 0]
        yb = dsa_mixer(dq, dk, dv, iq, ik, iw)

        rq = rope(rq.reshape(bsz, seq_len, RET_HEADS, RET_QK_DIM), positions, RET_QK_DIM, RET_THETA)
        rk = rope(rk.reshape(bsz, seq_len, RET_HEADS, RET_QK_DIM), positions, RET_QK_DIM, RET_THETA)
        yc = retention_mixer(rq, rk, rv.reshape(bsz, seq_len, RET_HEADS, RET_V_DIM), rg, ret_norm_g[l])

        g = jax.nn.sigmoid(gates)
        merged = (g[..., :D_MODEL] * (ya @ w_proj_a[l])
                  + g[..., D_MODEL:2 * D_MODEL] * (yb @ w_proj_b[l])
                  + g[..., 2 * D_MODEL:] * (yc @ w_proj_c[l]))
        x = x + merged @ w_out[l]

        h2 = rms_norm(x, norm2_g[l])
        x = x + jnp.square(jax.nn.relu(h2 @ w_ff1[l])) @ w_ff2[l]
    return rms_norm(x, final_norm_g)
```

```python
import math
import os
from contextlib import ExitStack
import numpy as np
import concourse.bass as bass
import concourse.mybir as mybir
from concourse.bass_utils import run_bass_kernel_spmd

F32 = mybir.dt.float32
BF16 = mybir.dt.bfloat16
I32 = mybir.dt.int32
ALU = mybir.AluOpType
AF = mybir.ActivationFunctionType
AX = mybir.AxisListType

D = 1024
SEQ = 8192
BATCH = 4
DEPTH = 2
TT = 512
EPS = 1e-6
BIG = 30000.0
TOPK = 256
NBIS = 22
BIS_LO, BIS_W = -16.0, 32.0


class Reg:
    __slots__ = ("w", "r")

    def __init__(self):
        self.w = None
        self.r = {}


class T:
    __slots__ = ("ap", "reg")

    def __init__(self, ap, reg=None):
        self.ap = ap
        self.reg = reg if reg is not None else Reg()

    def __getitem__(self, idx):
        return T(self.ap[idx], self.reg)

    def v(self, fn):
        return T(fn(self.ap), self.reg)

    def wr(self, reg):
        return T(self.ap, reg)


def _regs(xs):
    out = []
    for x in xs:
        if isinstance(x, T):
            x = x.reg
        if isinstance(x, Reg):
            out.append(x)
        elif isinstance(x, (tuple, list)):
            out.extend(x)
    return out


def _ap(x):
    return x.ap if isinstance(x, T) else x


class Sched:
    def __init__(self, nc, stack):
        self.nc = nc
        self.eng = {"pe": nc.tensor, "act": nc.scalar, "dve": nc.vector, "pool": nc.gpsimd, "sp": nc.sync}
        self.sem, self.cnt, self.seen = {}, {}, {}
        for e in ["pe", "act", "dve", "pool"]:
            self.sem[e] = stack.enter_context(nc.semaphore("s_" + e))
        self.NDS = 4
        self.dcnt = {}
        for q in ["sp", "pool"]:
            self.dcnt[q] = 0
            for i in range(self.NDS):
                self.sem["dma_%s%d" % (q, i)] = stack.enter_context(nc.semaphore("s_dma_%s%d" % (q, i)))
        for k in self.sem:
            self.cnt[k] = 0
        self.names = list(self.sem.keys())
        for e in ["pe", "act", "dve", "pool", "sp"]:
            self.seen[e] = {k: 0 for k in self.names}
        self.nins = 0
        self.psum_regs = set()
        self.act_dummy = None
        self.fence = None

    def _waits(self, e, reads, writes, acc):
        deps = {}

        def add(d):
            if d is not None and deps.get(d[0], 0) < d[1]:
                deps[d[0]] = d[1]
        for r in reads:
            add(r.w)
        for w in writes:
            if not acc:
                add(w.w)
            for k, t in w.r.items():
                add((k, t))
        eh, seen = self.eng[e], self.seen[e]
        if e == "pe" and self.fence is not None and seen["act"] < deps.get("act", 0):
            ta = deps.pop("act")
            pseen = self.seen["pool"]
            if pseen["act"] < ta:
                self.eng["pool"].wait_ge(self.sem["act"], ta)
                pseen["act"] = ta
            self.eng["pool"].memset(self.fence, 0.0).then_inc(self.sem["pool"], 1)
            self.cnt["pool"] += 1
            self.nins += 1
            seen["act"] = ta
            if deps.get("pool", 0) < self.cnt["pool"]:
                deps["pool"] = self.cnt["pool"]
        for k, t in deps.items():
            if seen[k] < t:
                eh.wait_ge(self.sem[k], t)
                seen[k] = t

    def op(self, e, fn, reads=(), writes=(), acc=False):
        reads, writes = _regs(reads), _regs(writes)
        self._waits(e, reads, writes, acc)
        ins = fn(self.eng[e])
        self.cnt[e] += 1
        t = self.cnt[e]
        ins.then_inc(self.sem[e], 1)
        self.nins += 1
        if e == "act" and self.act_dummy is not None and any(id(r) in self.psum_regs for r in reads):
            d0, d1 = self.act_dummy
            self.eng["act"].copy(d0, d1).then_inc(self.sem[e], 1)
            self.cnt[e] += 1
            t = self.cnt[e]
            self.nins += 1
        for r in reads:
            if r.r.get(e, 0) < t:
                r.r[e] = t
        for w in writes:
            w.w = (e, t)
            if not acc:
                w.r = {}
        return ins

    def dma(self, q, out, in_, **kw):
        reads, writes = _regs([in_]), _regs([out])
        self._waits(q, reads, writes, False)
        k = "dma_%s%d" % (q, self.dcnt[q] % self.NDS)
        self.dcnt[q] += 1
        if self.seen[q][k] < self.cnt[k]:
            self.eng[q].wait_ge(self.sem[k], self.cnt[k])
            self.seen[q][k] = self.cnt[k]
        ins = self.eng[q].dma_start(out=_ap(out), in_=_ap(in_), **kw)
        self.cnt[k] += 16
        t = self.cnt[k]
        ins.then_inc(self.sem[k], 16)
        self.nins += 1
        for r in reads:
            if r.r.get(k, 0) < t:
                r.r[k] = t
        for w in writes:
            w.w = (k, t)
            w.r = {}
        return ins

    def finish(self):
        for e in ["sp", "act", "pool", "dve", "pe"]:
            for k in self.names:
                if self.cnt[k] > self.seen[e][k]:
                    self.eng[e].wait_ge(self.sem[k], self.cnt[k])
                    self.seen[e][k] = self.cnt[k]

    def mm(self, out, lhsT, rhs, start, stop):
        return self.op("pe", lambda p: p.matmul(out.ap, lhsT=lhsT.ap, rhs=rhs.ap, start=start, stop=stop),
                       reads=[lhsT, rhs], writes=[out], acc=not start)

    def tr(self, out, in_, ident):
        return self.op("pe", lambda p: p.transpose(out.ap, in_.ap, ident.ap), reads=[in_, ident], writes=[out])

    def actf(self, out, in_, func, bias=None, scale=1.0, accum=None, eng="act"):
        kw = {}
        if bias is not None:
            kw["bias"] = _ap(bias)
        if accum is not None:
            kw["accum_out"] = accum.ap
        wr = [out] + ([accum] if accum is not None else [])
        return self.op(eng, lambda a: a.activation(out.ap, in_.ap, func, scale=_ap(scale), **kw),
                       reads=[in_, bias, scale], writes=wr)

    def tt(self, eng, out, a, b, op):
        return self.op(eng, lambda v: v.tensor_tensor(out.ap, a.ap, b.ap, op), reads=[a, b], writes=[out])

    def ts(self, eng, out, a, s1, op0, s2=None, op1=None, accum=None):
        kw = {}
        if op1 is not None:
            kw["op1"] = op1
        if accum is not None:
            kw["accum_out"] = accum.ap
        wr = [out] + ([accum] if accum is not None else [])
        return self.op(eng, lambda v: v.tensor_scalar(out.ap, a.ap, _ap(s1), _ap(s2) if s2 is not None else None, op0, **kw),
                       reads=[a, s1, s2], writes=wr)

    def stt(self, eng, out, a, s, b, op0, op1):
        eng = "dve"
        return self.op(eng, lambda v: v.scalar_tensor_tensor(out.ap, a.ap, _ap(s), b.ap, op0, op1),
                       reads=[a, s, b], writes=[out])

    def copy(self, eng, out, in_):
        if eng == "act":
            return self.op("act", lambda a: a.copy(out.ap, in_.ap), reads=[in_], writes=[out])
        return self.op(eng, lambda v: v.tensor_copy(out.ap, in_.ap), reads=[in_], writes=[out])

    def memset(self, eng, out, val):
        return self.op(eng, lambda v: v.memset(out.ap, val), writes=[out])


IN_SIZES = (256, 384, 64, 64, 128, 32, 4, 192, 192, 384, 384, 3072)
IN_OFF = np.concatenate([[0], np.cumsum(IN_SIZES)]).astype(int)
(O_U, O_DQ, O_DK, O_DV, O_IQ, O_IK, O_IW, O_RQ, O_RK, O_RV, O_RG, O_GT) = [int(v) for v in IN_OFF[:12]]


def _swap_cols(base, hd, rot):
    half = rot // 2
    idx = list(range(hd))
    for i in range(half):
        idx[i], idx[i + half] = i + half, i
    return [base + i for i in idx]


def _wa_columns():
    fm = list(range(O_U, O_U + 256))
    for h in range(6):
        fm += list(range(O_DQ + 64 * h, O_DQ + 64 * h + 64)) + _swap_cols(O_DQ + 64 * h, 64, 16)
    fm += list(range(O_DK, O_DK + 64)) + _swap_cols(O_DK, 64, 16)
    for h in range(4):
        fm += list(range(O_IQ + 32 * h, O_IQ + 32 * h + 32)) + _swap_cols(O_IQ + 32 * h, 32, 8)
    fm += list(range(O_IK, O_IK + 32)) + _swap_cols(O_IK, 32, 8)
    for off in (O_RQ, O_RK):
        for i in range(2):
            z, s = [], []
            for h in (2 * i, 2 * i + 1):
                z += list(range(off + 48 * h, off + 48 * h + 48)) + [-1] * 16
                s += _swap_cols(off + 48 * h, 48, 48) + [-1] * 16
            fm += z + s
    tm = list(range(O_DV, O_DV + 64)) + list(range(O_IW, O_IW + 4))
    tm += list(range(O_RV, O_RV + 384)) + list(range(O_RG, O_RG + 384))
    gt = list(range(O_GT, O_GT + 3072))
    return fm + tm + gt


WA_COLS = _wa_columns()
NA = len(WA_COLS)
A_U = 0
A_DQ = 256
A_DK = A_DQ + 768
A_IQ = A_DK + 128
A_IK = A_IQ + 256
A_RQ = A_IK + 64
A_RK = A_RQ + 512
A_VW = A_RK + 512
A_RV = A_VW + 68
A_RG = A_RV + 384
A_GT = A_RG + 384
assert A_GT + 3072 == NA


def _plan():
    r = lambda a, n: list(range(a, a + n))
    pl = [("u", "wa", 8, r(A_U, 256))]
    for i in range(3):
        pl.append(("dq%d" % i, "wa", 8, r(A_DQ + 256 * i, 256)))
    pl.append(("dkik", "wa", 8, r(A_DK, 128) + r(A_IK, 64)))
    pl.append(("iq", "wa", 8, r(A_IQ, 256)))
    for i in range(2):
        pl.append(("rq%d" % i, "wa", 8, r(A_RQ + 256 * i, 256)))
    for i in range(2):
        pl.append(("rk%d" % i, "wa", 8, r(A_RK + 256 * i, 256)))
    pl.append(("vw", "wa", 8, r(A_VW, 68)))
    pl.append(("rv", "wa", 8, r(A_RV, 384)))
    pl.append(("rg", "wa", 8, r(A_RG, 384)))
    pl.append(("glu", "wglu", 2, r(0, 256)))
    for ft in range(8):
        pl.append(("pr%d" % ft, "wpr", 8, r(128 * ft, 128)))
        pl.append(("gt%d" % ft, "wa", 8, r(A_GT + 128 * ft, 128) + r(A_GT + 1024 + 128 * ft, 128) + r(A_GT + 2048 + 128 * ft, 128)))
    for ft in range(8):
        pl.append(("wo%d" % ft, "wo", 8, r(128 * ft, 128)))
    for f4 in range(8):
        pl.append(("w1_%d" % f4, "w1", 8, r(512 * f4, 512)))
    for ft in range(8):
        pl.append(("w2_%d" % ft, "w2", 32, r(128 * ft, 128)))
    return pl


PLAN = _plan()
PLAN_OFF = []
_o = 0
for _nm, _s, _k, _c in PLAN:
    PLAN_OFF.append(_o)
    _o += _k * len(_c)
NW = _o


def _flat_weights(ws):
    out = np.empty((128, NW), np.float32)
    for (nm, s, nkt, cols), o in zip(PLAN, PLAN_OFF):
        w = ws[s][:, cols]
        n = len(cols)
        out[:, o:o + nkt * n] = w.reshape(nkt, 128, n).transpose(1, 0, 2).reshape(128, nkt * n)
    return out


_pc = {}
_off = 0
for _n, _w in [("g1", 8), ("g2", 8), ("gf", 8), ("glub", 2), ("ssd", 2), ("lre", 8), ("lim", 8), ("lst", 8),
               ("bre", 128), ("bim", 128), ("cre", 128), ("cim", 128), ("flag", 1), ("retg", 384),
               ("invD", 1), ("sgnD", 1), ("invI", 1), ("sgnI", 1), ("invR", 1), ("sgnR", 1),
               ("ramp", 256), ("intra", 256), ("qdec", 128), ("kdec", 4), ("diag", 128)]:
    _pc[_n] = (_off, _w)
    _off += _w
NP_ = _off


def _const_tables():
    c = {}
    p = np.arange(128)
    d = p % 64
    inv = np.where(d < 16, np.exp(-math.log(500000.0) * (d % 8) * (2.0 / 16)), 0.0)
    c["invD"] = inv[:, None]
    c["sgnD"] = np.where(d < 8, -1.0, np.where(d < 16, 1.0, 0.0))[:, None]
    d = p % 32
    inv = np.where(d < 8, np.exp(-math.log(500000.0) * (d % 4) * (2.0 / 8)), 0.0)
    c["invI"] = inv[:, None]
    c["sgnI"] = np.where(d < 4, -1.0, np.where(d < 8, 1.0, 0.0))[:, None]
    d = p % 64
    inv = np.where(d < 48, np.exp(-math.log(10000.0) * (d % 24) * (2.0 / 48)), 0.0)
    c["invR"] = inv[:, None]
    c["sgnR"] = np.where(d < 24, -1.0, np.where(d < 48, 1.0, 0.0))[:, None]
    c["ramp"] = np.broadcast_to(np.arange(1, 257, dtype=np.float64)[None, :], (128, 256))
    log_g = np.log1p(-np.exp2(-5.0 - np.arange(4)))
    sc = 48 ** -0.5
    m = (p % 64)[:, None]
    cc = np.arange(64)[None, :]
    intra = np.zeros((128, 4, 64))
    qdec = np.zeros((128, 2, 64))
    kdec = np.zeros((128, 4))
    for h in range(4):
        intra[:, h, :] = np.where(cc >= m, np.exp(log_g[h] * np.maximum(cc - m, 0)), 0.0) * sc
        kdec[:, h] = np.exp(log_g[h] * (63.0 - (p % 64)))
    for pair in range(2):
        for half in range(2):
            h = 2 * pair + half
            qdec[64 * half:64 * half + 64, pair, :] = (np.exp(log_g[h] * (np.arange(64) + 1.0)) * sc)[None, :]
    c["intra"] = intra.reshape(128, 256)
    c["qdec"] = qdec.reshape(128, 128)
    c["kdec"] = kdec
    q = np.arange(128)[:, None]
    k = np.arange(128)[None, :]
    c["diag"] = np.where(k < (q // 64 + 1) * 64, 0.0, -BIG)
    c["cdec"] = [float(np.exp(log_g[h] * 64.0)) for h in range(4)]
    return c


CONST = _const_tables()


def _pack_params(inp, l, flag):
    P = np.zeros((128, NP_), np.float32)

    def put(name, arr):
        o, w = _pc[name]
        P[:, o:o + w] = np.asarray(arr, np.float32).reshape(128, w)
    put("g1", inp["norm1_g"][l].reshape(8, 128).T)
    put("g2", inp["norm2_g"][l].reshape(8, 128).T)
    put("gf", inp["final_norm_g"].reshape(8, 128).T)
    put("glub", inp["ssm_glu_b"][l].reshape(2, 128).T)
    put("ssd", inp["ssm_d"][l].reshape(2, 128).T)
    put("lre", inp["ssm_lambda_re"][l].reshape(8, 128).T)
    put("lim", inp["ssm_lambda_im"][l].reshape(8, 128).T)
    put("lst", np.repeat(inp["ssm_log_step"][l], 64).reshape(8, 128).T)
    put("bre", inp["ssm_b_re"][l].reshape(8, 128, 16).transpose(1, 0, 2))
    put("bim", inp["ssm_b_im"][l].reshape(8, 128, 16).transpose(1, 0, 2))
    put("cre", inp["ssm_c_re"][l].transpose(0, 2, 1).reshape(8, 128, 16).transpose(1, 0, 2))
    put("cim", inp["ssm_c_im"][l].transpose(0, 2, 1).reshape(8, 128, 16).transpose(1, 0, 2))
    put("flag", np.full((128, 1), flag))
    put("retg", np.broadcast_to(inp["ret_norm_g"][l][None, :], (128, 384)))
    for n in ["invD", "sgnD", "invI", "sgnI", "invR", "sgnR", "ramp", "intra", "qdec", "kdec", "diag"]:
        put(n, CONST[n])
    return P


def _arrange_w_in(w):
    wz = np.concatenate([w, np.zeros((w.shape[0], 1), w.dtype)], axis=1)
    return wz[:, WA_COLS]


def build_program(ntiles, debug=None):
    nc = bass.Bass("TRN2", target_bir_lowering=False)
    NTOK = ntiles * TT
    dr = lambda n, s, dt, kind: nc.dram_tensor(n, s, dt, kind=kind).ap()
    x_in = dr("x_in", [NTOK, D], F32, "ExternalInput")
    pos_in = dr("pos", [1, NTOK], I32, "ExternalInput")
    P_in = dr("P", [128, NP_], F32, "ExternalInput")
    wf_in = dr("wf", [128, NW], F32, "ExternalInput")
    y_out = dr("y", [NTOK, D], F32, "ExternalOutput")
    dbg_out = None
    if debug:
        dbg_out = dr("dbg", list(debug[1]), F32, "ExternalOutput")
    wf_b = dr("wf_b", [128, NW], BF16, "Internal")

    with ExitStack() as st:
        S = Sched(nc, st)

        def sb(name, shape, dt=F32):
            return T(st.enter_context(nc.sbuf_tensor(name, shape, dt))[:])

        print("sbuf at start:", nc.sbuf_bytes_remaining)
        CH = 2048
        wchunks = []
        for c0 in range(0, NW, CH):
            c1 = min(NW, c0 + CH)
            rg = Reg()
            S.dma("pool", T(wf_b[:, c0:c1], rg), T(wf_in[:, c0:c1]))
            wchunks.append((c0, c1, rg))

        Pt = sb("Pt", [128, NP_])
        S.dma("sp", Pt, T(P_in))

        def pc(name, a=None, b=None):
            o, w = _pc[name]
            a = 0 if a is None else a
            b = w if b is None else b
            return Pt[:, o + a:o + b]

        ident = sb("ident", [128, 128])
        S.memset("pool", ident, 0.0)
        S.op("pool", lambda g: g.affine_select(ident.ap, ident.ap, pattern=[[-1, 128]], compare_op=ALU.not_equal,
                                               fill=1.0, base=0, channel_multiplier=1), reads=[ident], writes=[ident])
        identb = sb("identb", [128, 128], BF16)
        S.copy("dve", identb, ident)
        onesb = sb("onesb", [128, 128], BF16)
        S.memset("pool", onesb, 1.0)

        xT = sb("xT", [128, 8, TT])
        hT = sb("hT", [128, 8, TT], BF16)
        sq = sb("sq", [128, 8, TT], BF16)
        rstd = sb("rstd", [128, TT])
        big = sb("big", [128, 8192])
        aT = big.v(lambda a: a.bitcast(BF16).rearrange("p (k t) -> p k t", t=TT)[:, 0:32, :])
        stg = big.v(lambda a: a[:, 0:4 * D].rearrange("p (j f) -> p j f", f=D))
        KT = sb("KT", [128, NTOK], BF16)
        VC = sb("VC", [128, NTOK // 128, 96], BF16)
        KTr = [Reg() for _ in range(ntiles)]
        VCr = [Reg() for _ in range(ntiles)]
        S.memset("pool", VC, 1.0)
        for r in VCr:
            r.w = VC.reg.w
        uT = sb("uT", [128, 2, TT])
        qT = sb("qT", [64, 6, TT], BF16)
        iqT = sb("iqT", [128, 4, TT], BF16)
        iw = sb("iw", [128, 4, 4])
        rqT = sb("rqT", [128, 2, TT], BF16)
        rkT = sb("rkT", [128, 2, TT], BF16)
        rv = sb("rv", [128, 4, 384], BF16)
        rgs = sb("rgs", [128, 4, 384], BF16)
        yaT = sb("yaT", [128, 2, TT], BF16)
        ybT = sb("ybT", [128, 3, TT], BF16)
        ycT = sb("ycT", [128, 3, TT], BF16)
        NSL = 11
        U = st.enter_context(nc.sbuf_tensor("U", [128, NSL * 512], F32))[:]
        Ureg = [Reg() for _ in range(NSL)]

        def carve(s0, ns, shape_fn=None, dt=F32):
            ap = U[:, s0 * 512:(s0 + ns) * 512]
            if dt == BF16:
                ap = ap.bitcast(BF16)
            elif dt == I32:
                ap = ap.bitcast(I32)
            if shape_fn is not None:
                ap = shape_fn(ap)
            return T(ap, tuple(Ureg[s0:s0 + ns]))
        posi = carve(0, 1, None, I32)
        posf = carve(1, 1)
        ang = carve(2, 1)
        rope = carve(3, 6, lambda a: a.rearrange("p (s t) -> p s t", t=TT))
        ropa = carve(9, 1)
        ropb = carve(10, 1)
        TC = 256
        r2 = lambda a: a.rearrange("p (r t) -> p r t", t=TC)
        w12 = carve(0, 1, r2)
        w43 = carve(1, 1, r2)
        vv = carve(2, 1, r2)
        xs_ = carve(3, 1, r2)
        xo = carve(4, 1, r2)
        ypre = carve(5, 2, lambda a: a.rearrange("p (r t) -> p r t", t=TT))
        ypb = carve(7, 1, lambda a: a.rearrange("p (r t) -> p r t", t=TT), BF16)
        gsig = carve(8, 1)
        qd = carve(0, 1, lambda a: a.rearrange("p (r t) -> p r t", t=TT), BF16)
        kd = carve(1, 1, lambda a: a[:, 0:768].rearrange("p (j h d) -> p j h d", j=4, h=4), BF16)
        yr = carve(2, 1, lambda a: a[:, 0:384])
        ysq = carve(3, 1, lambda a: a[:, 0:384])
        ycb = carve(4, 1, lambda a: a[:, 0:384], BF16)
        At = carve(5, 1, lambda a: a[:, 0:64], BF16)
        st4 = carve(6, 1, lambda a: a[:, 0:16].rearrange("p (a b) -> p a b", b=4))
        relu_t = carve(0, 1)
        pT = carve(1, 1, lambda a: a[:, 0:768].rearrange("p (g c t) -> p g c t", g=2, c=3), BF16)
        pvs = carve(2, 1, lambda a: a[:, 0:390].rearrange("p (h e) -> p h e", e=65))
        ybt = carve(3, 1, lambda a: a[:, 0:384], BF16)
        m01 = carve(4, 1, lambda a: a[:, 0:128], BF16)
        mT = carve(5, 1, lambda a: a[:, 0:128], BF16)
        bis = carve(6, 1, lambda a: a[:, 0:8])
        lo, mid, nmid, ssA, ssB, gcol, rden6 = bis[:, 0:1], bis[:, 1:2], bis[:, 2:3], bis[:, 3:4], bis[:, 4:5], bis[:, 5:6], None
        rden = carve(7, 1, lambda a: a[:, 0:6])
        ot = carve(8, 2, lambda a: a[:, 0:768])
        junk = sq.v(lambda a: a.rearrange("p k t -> p (k t)"))
        gA = carve(0, 1)
        gB = carve(1, 1)
        gC = carve(2, 1)
        rl = carve(3, 1, None, BF16)[:, 0:TT]
        NWB = 2
        WBW = 4096
        wbuf = [sb("wbuf%d" % i, [128, WBW], BF16) for i in range(NWB)]
        wctr = [0]
        wpos = [0]

        def wnext(name):
            nm, s_, nkt, cols = PLAN[wpos[0]]
            off = PLAN_OFF[wpos[0]]
            assert nm == name, (nm, name)
            wpos[0] += 1
            n = len(cols)
            b = wbuf[wctr[0] % NWB]
            wctr[0] += 1
            v = b.v(lambda a: a[:, 0:nkt * n])
            regs = tuple(rg for (c0, c1, rg) in wchunks if c0 < off + nkt * n and c1 > off)
            S.dma("sp", v, T(wf_b[:, off:off + nkt * n], regs))
            return v.v(lambda a: a.rearrange("p (k n) -> p k n", n=n))

        pbank = [T(st.enter_context(nc.psum_tensor("ps%d" % i, [128, 512], F32))[:]) for i in range(8)]
        prot = [0]
        for b_ in pbank:
            S.psum_regs.add(id(b_.reg))
        if os.environ.get('FENCE'):
            fnc = st.enter_context(nc.sbuf_tensor("fnc", [128, 4], F32))[:]
            S.fence = fnc[0:1, 0:1]
        if os.environ.get('DUMMY'):
            dmy = st.enter_context(nc.sbuf_tensor("dmy", [128, 4], F32))[:]
            S.memset("pool", T(dmy), 0.0)
            S.act_dummy = (dmy[:, 0:1], dmy[:, 2:3])

        def ps():
            b = pbank[prot[0] % 6]
            prot[0] += 1
            return b
        PD0, PD1 = pbank[6], pbank[7]

        TWO_PI = 2.0 * math.pi

        C1 = 6.28125
        C2 = 4058.0 / 2 ** 21
        C3 = TWO_PI - C1 - C2
        PI_LO = 3.1415925

        def sincos(out_sin, out_cos, angle, tmp_t, ki_t, kf_t):
            S.ts("dve", tmp_t, angle, 1.0 / TWO_PI, ALU.mult)
            _ce = "dve" if os.environ.get('CVT_DVE') else "pool"
            S.copy(_ce, ki_t, tmp_t)
            S.copy(_ce, kf_t, ki_t)
            S.stt("dve", tmp_t, kf_t, -C1, angle, ALU.mult, ALU.add)
            S.stt("dve", tmp_t, kf_t, -C2, tmp_t, ALU.mult, ALU.add)
            S.stt("dve", tmp_t, kf_t, -C3, tmp_t, ALU.mult, ALU.add)
            S.ts("dve", tmp_t, tmp_t, -PI_LO, ALU.max, PI_LO, ALU.min)
            S.actf(out_sin, tmp_t, AF.Sin)
            S.ts("dve", kf_t, tmp_t, 0.5 * math.pi, ALU.is_gt, -TWO_PI, ALU.mult)
            S.stt("dve", tmp_t, tmp_t, 0.5 * math.pi, kf_t, ALU.add, ALU.add)
            S.ts("dve", tmp_t, tmp_t, -PI_LO, ALU.max, PI_LO, ALU.min)
            S.actf(out_cos, tmp_t, AF.Sin)

        s5 = sb("s5p", [128, 12, 8])
        lre, lim, lst = pc("lre"), pc("lim"), pc("lst")
        stp, are, th, rr, ct_, st_, lbr, lbi, den, fre, fim, tmp = [s5[:, i, :] for i in range(12)]
        S.actf(stp, lst, AF.Exp)
        S.tt("dve", are, lre, stp, ALU.mult)
        S.tt("dve", th, lim, stp, ALU.mult)
        S.actf(rr, are, AF.Exp)
        s5i = sb("s5i", [128, 8], I32)
        s5f = sb("s5f", [128, 8])
        sincos(st_, ct_, th, tmp, s5i, s5f)
        S.tt("dve", lbr, rr, ct_, ALU.mult)
        S.tt("dve", lbi, rr, st_, ALU.mult)
        S.tt("dve", den, lre, lre, ALU.mult)
        S.tt("dve", tmp, lim, lim, ALU.mult)
        S.tt("dve", den, den, tmp, ALU.add)
        S.op("dve", lambda v: v.reciprocal(den.ap, den.ap), reads=[den], writes=[den])
        S.ts("dve", lbr, lbr, -1.0, ALU.add)
        S.tt("dve", fre, lbr, lre, ALU.mult)
        S.tt("dve", tmp, lbi, lim, ALU.mult)
        S.tt("dve", fre, fre, tmp, ALU.add)
        S.tt("dve", fre, fre, den, ALU.mult)
        S.tt("dve", fim, lbi, lre, ALU.mult)
        S.tt("dve", tmp, lbr, lim, ALU.mult)
        S.tt("dve", fim, fim, tmp, ALU.subtract)
        S.tt("dve", fim, fim, den, ALU.mult)
        bbr = sb("bbr", [128, 8, 16])
        bbi = sb("bbi", [128, 8, 16])
        tb = sb("tb", [128, 16])
        k16 = lambda a: a.rearrange("p (k c) -> p k c", c=16)
        bre, bim, cre, cim = pc("bre").v(k16), pc("bim").v(k16), pc("cre").v(k16), pc("cim").v(k16)
        BT = sb("BT", [128, 8, 2, 128])
        CT = sb("CT", [128, 8, 2, 128])
        S.memset("pool", CT, 0.0)
        pad = sb("pad", [128, 2, 128])
        for k in range(8):
            S.ts("dve", bbr[:, k, :], bre[:, k, :], fre[:, k:k + 1], ALU.mult)
            S.ts("dve", tb, bim[:, k, :], fim[:, k:k + 1], ALU.mult)
            S.tt("dve", bbr[:, k, :], bbr[:, k, :], tb, ALU.subtract)
            S.ts("dve", bbi[:, k, :], bim[:, k, :], fre[:, k:k + 1], ALU.mult)
            S.ts("dve", tb, bre[:, k, :], fim[:, k:k + 1], ALU.mult)
            S.tt("dve", bbi[:, k, :], bbi[:, k, :], tb, ALU.add)
            c0 = 32 * (k % 4)
            S.memset("pool", pad, 0.0)
            for ri, src in enumerate((bbr, bbi)):
                S.copy("pool", pad[0:64, ri, c0:c0 + 16], src[0:64, k, :])
                S.copy("pool", pad[64:128, ri, c0 + 16:c0 + 32], src[64:128, k, :])
            for ri in range(2):
                pb = ps()
                S.tr(pb[:, 0:128], pad[:, ri, :], ident)
                S.copy("dve", BT[:, k, ri, :], pb[:, 0:128])
            S.copy("pool", CT[0:64, k, 0, c0:c0 + 16], cre[0:64, k, :])
            S.copy("pool", CT[64:128, k, 0, c0 + 16:c0 + 32], cre[64:128, k, :])
            S.ts("dve", CT[0:64, k, 1, c0:c0 + 16], cim[0:64, k, :], -1.0, ALU.mult)
            S.ts("dve", CT[64:128, k, 1, c0 + 16:c0 + 32], cim[64:128, k, :], -1.0, ALU.mult)
        CS = sb("CS", [128, 8, 2, TC])
        for k in range(8):
            S.ts("dve", ropa[:, 0:TC], pc("ramp"), th[:, k:k + 1], ALU.mult)
            sincos(CS[:, k, 1, :], CS[:, k, 0, :], ropa[:, 0:TC], ropb[:, 0:TC], posi[:, 0:TC], posf[:, 0:TC])
        xprev = sb("xprev", [128, 8, 2])
        S.memset("pool", xprev, 0.0)
        Sst = sb("Sst", [128, 2, 96])
        Sbf = sb("Sbf", [128, 2, 96], BF16)
        S.memset("pool", Sst, 0.0)
        S.memset("pool", Sbf, 0.0)
        ogf = sb("ogf", [128, 8])
        omf = sb("omf", [128, 1])
        S.ts("dve", ogf, pc("gf"), pc("flag"), ALU.mult)
        S.ts("dve", omf, pc("flag"), -1.0, ALU.mult, 1.0, ALU.add)
        print("sbuf left after alloc:", nc.sbuf_bytes_remaining)

        def rms_stats():
            S.actf(sq, xT, AF.Square)
            pb = ps()
            for kt in range(8):
                S.mm(pb, onesb, sq[:, kt, :], kt == 0, kt == 7)
            S.actf(rstd, pb, AF.Sqrt, bias=EPS, scale=1.0 / D)
            S.op("dve", lambda v: v.reciprocal(rstd.ap, rstd.ap), reads=[rstd], writes=[rstd])

        def rmsnorm(gname):
            rms_stats()
            for kt in range(8):
                S.stt("dve" if kt % 2 == 0 else "pool", hT[:, kt, :], xT[:, kt, :], pc(gname, kt, kt + 1), rstd, ALU.mult, ALU.mult)

        evac_flip = [0]

        def evac_eng():
            evac_flip[0] += 1
            return "dve"

        hk = lambda kt: hT[:, kt, :]

        def fm_mm(wv, col0, m, rhs_of_kt, nkt=8, po=0):
            pb = ps()
            for kt in range(nkt):
                S.mm(pb[po:po + m, :], wv[:, kt, col0:col0 + m], rhs_of_kt(kt), kt == 0, kt == nkt - 1)
            return pb

        def roped(wv, zc, sc, m, ty, out_t, po=0):
            pz = fm_mm(wv, zc, m, hk, po=po)
            pz2 = fm_mm(wv, sc, m, hk, po=po)
            S.tt("dve", ropa[po:po + m, :], pz[po:po + m, :], rope[po:po + m, 2 * ty, :], ALU.mult)
            S.tt("dve", ropb[po:po + m, :], pz2[po:po + m, :], rope[po:po + m, 2 * ty + 1, :], ALU.mult)
            S.tt("pool", out_t, ropa[po:po + m, :], ropb[po:po + m, :], ALU.add)

        class _Stop(Exception):
            pass

        def kstop(tag):
            if os.environ.get('KSTOP') == tag:
                raise _Stop()
        for ti in range(ntiles):
          try:
                t0 = ti * TT
                wpos[0] = 0
                S.dma("sp", stg, T(x_in[t0:t0 + TT, :].rearrange("(j p) f -> p j f", p=128)))
                for j in range(4):
                    for kt in range(0, 8, 4):
                        pb = ps()
                        for q in range(4):
                            S.tr(pb[:, q * 128:(q + 1) * 128], stg[:, j, (kt + q) * 128:(kt + q + 1) * 128], ident)
                        S.copy(evac_eng(), xT[:, kt:kt + 4, j * 128:(j + 1) * 128],
                               pb.v(lambda a: a.rearrange("p (q t) -> p q t", t=128)))
                S.dma("sp", posi, T(pos_in[0:1, t0:t0 + TT].partition_broadcast(128)))
                S.copy("dve", posf, posi)
                for ty, (inv, sgn) in enumerate((("invD", "sgnD"), ("invI", "sgnI"), ("invR", "sgnR"))):
                    S.ts("dve", ang, posf, pc(inv), ALU.mult)
                    sincos(rope[:, 2 * ty + 1, :], rope[:, 2 * ty, :], ang, ropa, posi, ropb)
                    S.ts("dve", rope[:, 2 * ty + 1, :], rope[:, 2 * ty + 1, :], pc(sgn), ALU.mult)
                rmsnorm("g1")

                wv = wnext("u")
                for g in range(2):
                    pb = fm_mm(wv, 128 * g, 128, hk)
                    S.copy("dve", uT[:, g, :], pb)
                for i in range(3):
                    wv = wnext("dq%d" % i)
                    for hh in range(2):
                        roped(wv, 128 * hh, 128 * hh + 64, 64, 0, qT[:, 2 * i + hh, :])
                wv = wnext("dkik")
                roped(wv, 0, 64, 64, 0, KT[0:64, t0:t0 + TT].wr(KTr[ti]))
                roped(wv, 128, 160, 32, 1, KT[64:96, t0:t0 + TT].wr(KTr[ti]), po=64)
                wv = wnext("iq")
                for h in range(4):
                    roped(wv, 64 * h, 64 * h + 32, 32, 1, iqT[64:96, h, :], po=64)
                for i in range(2):
                    wv = wnext("rq%d" % i)
                    roped(wv, 0, 128, 128, 2, rqT[:, i, :])
                for i in range(2):
                    wv = wnext("rk%d" % i)
                    roped(wv, 0, 128, 128, 2, rkT[:, i, :])
                wv = wnext("vw")
                for j in range(4):
                    pb = ps()
                    for kt in range(8):
                        S.mm(pb[:, 0:68], hT[:, kt, j * 128:(j + 1) * 128], wv[:, kt, :], kt == 0, kt == 7)
                    S.copy("dve", VC[:, ti * 4 + j, 0:64].wr(VCr[ti]), pb[:, 0:64])
                    S.ts("dve", iw[:, j, :], pb[:, 64:68], 0.5 * 32 ** -0.5, ALU.mult)
                wv = wnext("rv")
                for j in range(4):
                    pb = ps()
                    for kt in range(8):
                        S.mm(pb[:, 0:384], hT[:, kt, j * 128:(j + 1) * 128], wv[:, kt, :], kt == 0, kt == 7)
                    S.copy("dve", rv[:, j, :], pb[:, 0:384])
                wv = wnext("rg")
                for j in range(4):
                    pb = ps()
                    for kt in range(8):
                        S.mm(pb[:, 0:384], hT[:, kt, j * 128:(j + 1) * 128], wv[:, kt, :], kt == 0, kt == 7)
                    S.actf(rgs[:, j, :], pb[:, 0:384], AF.Silu)

                for ch in range(TT // TC):
                    cs = slice(ch * TC, (ch + 1) * TC)
                    for ct in range(2):
                        yacc = PD0 if ct == 0 else PD1
                        for kk in range(4):
                            k = 4 * ct + kk
                            pb = ps()
                            for ri in range(2):
                                S.mm(pb[:, ri * TC:(ri + 1) * TC], BT[:, k, ri, :], uT[:, ct, cs], True, True)
                            bu = pb.v(r2)
                            S.tt("dve", w12, bu, CS[:, k, :, :], ALU.mult)
                            S.tt("dve", w43[:, 0, :], bu[:, 0, :], CS[:, k, 1, :], ALU.mult)
                            S.tt("dve", w43[:, 1, :], bu[:, 1, :], CS[:, k, 0, :], ALU.mult)
                            S.tt("pool", vv[:, 0, :], w12[:, 0, :], w12[:, 1, :], ALU.add)
                            S.tt("pool", vv[:, 1, :], w43[:, 1, :], w43[:, 0, :], ALU.subtract)
                            for ri in range(2):
                                S.op("dve", lambda v, ri=ri, k=k: v.tensor_tensor_scan(
                                    xs_.ap[:, ri, :], rr.ap[:, k:k + 1].to_broadcast([128, TC]), vv.ap[:, ri, :],
                                    xprev.ap[:, k, ri:ri + 1], ALU.mult, ALU.add),
                                    reads=[rr, vv, xprev], writes=[xs_])
                            S.tt("pool", w12, xs_, CS[:, k, :, :], ALU.mult)
                            S.tt("pool", w43[:, 0, :], xs_[:, 0, :], CS[:, k, 1, :], ALU.mult)
                            S.tt("pool", w43[:, 1, :], xs_[:, 1, :], CS[:, k, 0, :], ALU.mult)
                            S.tt("dve", xo[:, 0, :], w12[:, 0, :], w12[:, 1, :], ALU.subtract)
                            S.tt("dve", xo[:, 1, :], w43[:, 0, :], w43[:, 1, :], ALU.add)
                            S.copy("act", xprev[:, k, :], xo[:, :, TC - 1])
                            for ri in range(2):
                                S.mm(yacc[:, 0:TC], CT[:, k, ri, :], xo[:, ri, :], kk == 0 and ri == 0, kk == 3 and ri == 1)
                        yv = ypre[:, ct, cs]
                        S.stt("dve", yv, uT[:, ct, cs], pc("ssd", ct, ct + 1), yacc[:, 0:TC], ALU.mult, ALU.add)
                        S.tt("pool", w12[:, 0, :], yv, yv, ALU.mult)
                        S.ts("dve", w12[:, 0, :], w12[:, 0, :], 0.044715, ALU.mult, 1.0, ALU.add)
                        S.tt("pool", w12[:, 0, :], w12[:, 0, :], yv, ALU.mult)
                        S.actf(w12[:, 1, :], w12[:, 0, :], AF.Sigmoid, scale=2.0 * math.sqrt(2.0 / math.pi))
                        S.tt("dve", yv, yv, w12[:, 1, :], ALU.mult)
                S.copy("act", ypb, ypre)
                wv = wnext("glu")
                for ct in range(2):
                    pb = fm_mm(wv, 128 * ct, 128, lambda kt: ypb[:, kt, :], nkt=2)
                    S.actf(gsig, pb, AF.Sigmoid, bias=pc("glub", ct, ct + 1))
                    S.tt("dve", yaT[:, ct, :], ypre[:, ct, :], gsig, ALU.mult)
                if debug:
                    debug[0](S, locals(), dbg_out, ti, "s5")

                _skipret = ti >= 1 and 'ret' in os.environ.get('KSKIP', '')
                c64 = lambda a: a.rearrange("p (c t) -> p c t", t=64)
                for i in range(2):
                    S.tt("pool", qd[:, i, :].v(c64), rqT[:, i, :].v(c64),
                         pc("qdec", 64 * i, 64 * i + 64).v(lambda a: a.unsqueeze(1).to_broadcast([128, 8, 64])), ALU.mult)
                for j in range(4):
                    pb = ps()
                    pbv = pb.v(lambda a: a.bitcast(BF16))
                    for h in range(4):
                        base = 64 * (h % 2)
                        S.tr(pbv[:, 64 * h:64 * h + 48], rkT[base:base + 48, h // 2, j * 128:(j + 1) * 128], identb[base:base + 48, base:base + 48])
                    for h in range(4):
                        S.ts("dve", kd[:, j, h, :], pbv[:, 64 * h:64 * h + 48], pc("kdec", h, h + 1), ALU.mult)
                for j in range(4):
                    for half in range(2):
                        rb = 64 * half
                        cs0 = j * 128 + rb
                        pout = pbank[4 + half] if not os.environ.get('POUT_PD1') else (PD1 if os.environ.get('POUT_PD1') == '1' else pbank[4])
                        for h in range(4):
                            base = 64 * (h % 2)
                            pair = h // 2
                            pa = pbank[(2 * h) % 4]
                            S.mm(pa[rb:rb + 64, 0:64], rkT[base:base + 48, pair, cs0:cs0 + 64], rqT[base:base + 48, pair, cs0:cs0 + 64], True, True)
                            kstop('r%d%d_h%d_s0' % (j, half, h))
                            S.tt("dve", At[rb:rb + 64, :], pa[rb:rb + 64, 0:64], pc("intra", 64 * h, 64 * h + 64)[rb:rb + 64, :], ALU.mult)
                            kstop('r%d%d_h%d_s1' % (j, half, h))
                            if os.environ.get('FAKEDEP'):
                                S.op("pe", lambda p, h=h, rb=rb: p.matmul(pout.ap[rb:rb + 64, 96 * h:96 * h + 96], lhsT=At.ap[rb:rb + 64, :], rhs=rv.ap[rb:rb + 64, j, 96 * h:96 * h + 96], start=True, stop=False), reads=[At, rv, yr], writes=[pout])
                            else:
                                S.mm(pout[rb:rb + 64, 96 * h:96 * h + 96], At[rb:rb + 64, :], rv[rb:rb + 64, j, 96 * h:96 * h + 96], True, False)
                            kstop('r%d%d_h%d_s2' % (j, half, h))
                            S.mm(pout[rb:rb + 64, 96 * h:96 * h + 96], qd[base:base + 48, pair, cs0:cs0 + 64], Sbf[base:base + 48, pair, :], False, True)
                            kstop('r%d%d_h%d_s3' % (j, half, h))
                            pu = pbank[(2 * h + 1) % 4]
                            S.mm(pu[base:base + 48, 0:96], kd[rb:rb + 64, j, h, :], rv[rb:rb + 64, j, 96 * h:96 * h + 96], True, True)
                            kstop('r%d%d_h%d_s4' % (j, half, h))
                            S.stt("dve", Sst[base:base + 48, pair, :], Sst[base:base + 48, pair, :], CONST["cdec"][h], pu[base:base + 48, 0:96], ALU.mult, ALU.add)
                            kstop('r%d%d_h%d_s5' % (j, half, h))
                            S.copy("act", Sbf[base:base + 48, pair, :], Sst[base:base + 48, pair, :])
                            kstop('r%d%d_h%d_s6' % (j, half, h))
                        S.copy("dve", yr[rb:rb + 64, :], pout[rb:rb + 64, 0:384])
                        kstop('ret_%d_%d' % (j, half))
                    yr3 = yr.v(lambda a: a.rearrange("p (h e) -> p h e", e=96))
                    S.op("dve", lambda v: v.tensor_reduce(st4.ap[:, 0, :], yr3.ap, AX.X, ALU.add), reads=[yr], writes=[st4])
                    S.tt("pool", ysq, yr, yr, ALU.mult)
                    S.op("dve", lambda v: v.tensor_reduce(st4.ap[:, 1, :], ysq.ap.rearrange("p (h e) -> p h e", e=96), AX.X, ALU.add), reads=[ysq], writes=[st4])
                    S.ts("dve", st4[:, 2, :], st4[:, 0, :], 1.0 / 96, ALU.mult)
                    S.tt("dve", st4[:, 0, :], st4[:, 2, :], st4[:, 2, :], ALU.mult)
                    S.stt("dve", st4[:, 3, :], st4[:, 1, :], 1.0 / 96, st4[:, 0, :], ALU.mult, ALU.subtract)
                    S.actf(st4[:, 3, :], st4[:, 3, :], AF.Sqrt, bias=EPS)
                    S.op("dve", lambda v: v.reciprocal(st4.ap[:, 3, :], st4.ap[:, 3, :]), reads=[st4], writes=[st4])
                    bc = lambda c: st4[:, c, :].v(lambda a: a.unsqueeze(2).to_broadcast([128, 4, 96]))
                    S.tt("dve", yr3, yr3, bc(2), ALU.subtract)
                    S.tt("dve", yr3, yr3, bc(3), ALU.mult)
                    S.tt("pool", yr, yr, pc("retg"), ALU.mult)
                    S.tt("pool", ycb, yr, rgs[:, j, :], ALU.mult)
                    pb = ps()
                    pbv = pb.v(lambda a: a.bitcast(BF16))
                    for c3 in range(3):
                        S.tr(pbv[:, 128 * c3:128 * c3 + 128], ycb[:, 128 * c3:128 * c3 + 128], identb)
                    S.copy("dve", ycT[:, :, j * 128:(j + 1) * 128], pbv[:, 0:384].v(lambda a: a.rearrange("p (c t) -> p c t", t=128)))
                if debug:
                    debug[0](S, locals(), dbg_out, ti, "ret")

                for j in range(4 if ti == 0 else int(os.environ.get('KDSAJ', '4'))):
                    qb = ti * 4 + j
                    nkb = qb + 1
                    nk = nkb * 128
                    qs = slice(j * 128, (j + 1) * 128)
                    for k0 in range(0, nk, 512):
                        kw = min(512, nk - k0)
                        tl = [KTr[t] for t in range(k0 // TT, (k0 + kw - 1) // TT + 1)]
                        for h in range(4):
                            pb = ps()
                            S.op("pe", lambda p, pb=pb, h=h, k0=k0, kw=kw: p.matmul(
                                pb.ap[:, 0:kw], lhsT=iqT.ap[64:96, h, qs], rhs=KT.ap[64:96, k0:k0 + kw], start=True, stop=True),
                                reads=[iqT] + tl, writes=[pb])
                            if h == 0:
                                S.actf(big[:, k0:k0 + kw], pb[:, 0:kw], AF.Relu)
                                S.ts("dve", big[:, k0:k0 + kw], big[:, k0:k0 + kw], iw[:, j, 0:1], ALU.mult)
                            else:
                                S.actf(relu_t[:, 0:kw], pb[:, 0:kw], AF.Relu)
                                S.stt("dve" if h % 2 else "pool", big[:, k0:k0 + kw], relu_t[:, 0:kw], iw[:, j, h:h + 1], big[:, k0:k0 + kw], ALU.mult, ALU.add)
                    S.tt("pool", big[:, nk - 128:nk], big[:, nk - 128:nk], pc("diag"), ALU.add)
                    S.memset("dve", mid, BIS_LO + BIS_W / 2)
                    thr_s = float(2 * TOPK - nk)
                    w_i = BIS_W / 2
                    nA = min(nk, 4096)
                    for it in range(NBIS if ti == 0 else int(os.environ.get('KNBIS', NBIS))):
                        S.ts("dve", nmid, mid, -1.0, ALU.mult)
                        S.memset("dve", bis[:, 3:5], 0.0)
                        S.actf(junk[:, 0:nA], big[:, 0:nA], AF.Sign, bias=nmid, accum=ssA)
                        if nk > nA:
                            S.actf(junk[:, 0:nk - nA], big[:, nA:nk], AF.Sign, bias=nmid, accum=ssB)
                            S.tt("dve", ssA, ssA, ssB, ALU.add)
                        S.ts("dve", gcol, ssA, thr_s, ALU.is_ge, w_i, ALU.mult)
                        if it < NBIS - 1:
                            S.stt("dve", mid, gcol, -w_i / 2, mid, ALU.add, ALU.add)
                        else:
                            S.stt("dve", lo, gcol, -w_i, mid, ALU.add, ALU.add)
                        w_i = w_i / 2
                    if debug:
                        debug[0](S, locals(), dbg_out, ti, "bis")
                    for kb in range(nkb if ti == 0 else min(nkb, int(os.environ.get('KNKB', 99)))):
                        ks = slice(kb * 128, (kb + 1) * 128)
                        kreg = KTr[kb // 4]
                        S.ts("dve", m01, big[:, ks], lo, ALU.is_ge)
                        pm = ps()
                        pmv = pm.v(lambda a: a.bitcast(BF16))
                        S.tr(pmv[:, 0:128], m01, identb)
                        S.copy("dve", mT, pmv[:, 0:128])
                        for g in range(2):
                            pp = ps()
                            S.op("pe", lambda p, pp=pp, g=g: p.matmul(
                                pp.ap[:, 0:384], lhsT=KT.ap[0:64, ks], rhs=qT.ap[0:64, 3 * g:3 * g + 3, qs], start=True, stop=True),
                                reads=[qT, kreg], writes=[pp])
                            S.actf(pT[:, g, :, :], pp[:, 0:384].v(lambda a: a.rearrange("p (c t) -> p c t", t=128)), AF.Exp, scale=0.125)
                        p6 = pT.v(lambda a: a.rearrange("p g c t -> p (g c) t"))
                        S.tt("pool", p6, p6, mT.v(lambda a: a.unsqueeze(1).to_broadcast([128, 6, 128])), ALU.mult)
                        for g in range(2):
                            PDg = PD0 if g == 0 else PD1
                            S.op("pe", lambda p, g=g, kb=kb, PDg=PDg: p.matmul(
                                PDg.ap[0:96, 0:384], lhsT=VC.ap[:, kb, :], rhs=pT.ap[:, g, :, :],
                                start=(kb == 0), stop=(kb == nkb - 1)),
                                reads=[pT, VCr[kb // 4]], writes=[PDg], acc=(kb != 0))
                    S.copy("dve", ot[0:96, 0:384], PD0[0:96, 0:384])
                    S.copy("dve", ot[0:96, 384:768], PD1[0:96, 0:384])
                    po = [ps(), ps()]
                    for h in range(6):
                        S.tr(po[h // 3][:, 96 * (h % 3):96 * (h % 3) + 96], ot[0:96, 128 * h:128 * h + 128], ident[0:96, 0:96])
                    for g in range(2):
                        S.copy("dve", pvs[:, 3 * g:3 * g + 3, :], po[g][:, 0:288].v(lambda a: a.rearrange("p (c e) -> p c e", e=96))[:, :, 0:65])
                    S.op("dve", lambda v: v.reciprocal(rden.ap, pvs.ap[:, :, 64]), reads=[pvs], writes=[rden])
                    S.tt("dve", ybt.v(lambda a: a.rearrange("p (h e) -> p h e", e=64)), pvs[:, :, 0:64],
                         rden.v(lambda a: a.unsqueeze(2).to_broadcast([128, 6, 64])), ALU.mult)
                    pb = ps()
                    pbv = pb.v(lambda a: a.bitcast(BF16))
                    for c3 in range(3):
                        S.tr(pbv[:, 128 * c3:128 * c3 + 128], ybt[:, 128 * c3:128 * c3 + 128], identb)
                    S.copy("dve", ybT[:, :, qs], pbv[:, 0:384].v(lambda a: a.rearrange("p (c t) -> p c t", t=128)))
                if debug:
                    debug[0](S, locals(), dbg_out, ti, "dsa")

                mrg = sq
                srcs = [(yaT, 0, 2), (ybT, 2, 3), (ycT, 5, 3)]
                for ft in range(8):
                    wp = wnext("pr%d" % ft)
                    pP = []
                    for bi, (yt, k0, nk_) in enumerate(srcs):
                        pb = ps()
                        for kt in range(nk_):
                            S.mm(pb, wp[:, k0 + kt, :], yt[:, kt, :], kt == 0, kt == nk_ - 1)
                        pP.append(pb)
                    wg = wnext("gt%d" % ft)
                    for bi, gt in enumerate((gA, gB, gC)):
                        pb = fm_mm(wg, 128 * bi, 128, hk)
                        S.actf(gt, pb, AF.Sigmoid)
                    S.tt("dve", gA, gA, pP[0], ALU.mult)
                    S.tt("dve", gB, gB, pP[1], ALU.mult)
                    S.tt("dve", gC, gC, pP[2], ALU.mult)
                    S.tt("pool", gA, gA, gB, ALU.add)
                    S.tt("pool", mrg[:, ft, :], gA, gC, ALU.add)
                for ft in range(8):
                    wv = wnext("wo%d" % ft)
                    pb = fm_mm(wv, 0, 128, lambda kt: mrg[:, kt, :])
                    S.tt("dve", xT[:, ft, :], xT[:, ft, :], pb, ALU.add)
                if debug:
                    debug[0](S, locals(), dbg_out, ti, "mix")

                rmsnorm("g2")
                for f4 in range(8):
                    wv = wnext("w1_%d" % f4)
                    for q in range(4):
                        f = f4 * 4 + q
                        pb = fm_mm(wv, 128 * q, 128, hk)
                        S.actf(rl, pb, AF.Relu)
                        S.tt("pool" if f % 2 else "dve", aT[:, f, :], rl, rl, ALU.mult)
                for ft in range(8):
                    wv = wnext("w2_%d" % ft)
                    pb = fm_mm(wv, 0, 128, lambda kt: aT[:, kt, :], nkt=32)
                    S.tt("dve", xT[:, ft, :], xT[:, ft, :], pb, ALU.add)

                rms_stats()
                oT = xT
                tmpo = rope.v(lambda a: a[:, 0:1, :].rearrange("p s t -> p (s t)"))
                for kt in range(8):
                    S.stt("dve", tmpo, xT[:, kt, :], ogf[:, kt:kt + 1], rstd, ALU.mult, ALU.mult)
                    S.stt("pool", xT[:, kt, :], xT[:, kt, :], omf[:, 0:1], tmpo, ALU.mult, ALU.add)
                for j in range(4):
                    for kt in range(0, 8, 4):
                        pb = ps()
                        for q in range(4):
                            S.tr(pb[:, q * 128:(q + 1) * 128], oT[:, kt + q, j * 128:(j + 1) * 128], ident)
                        S.copy(evac_eng(), stg[:, j, kt * 128:(kt + 4) * 128], pb)
                S.dma("sp", T(y_out[t0:t0 + TT, :].rearrange("(j p) f -> p j f", p=128)), stg)
          except _Stop:
            break
        S.finish()
        print("instructions:", S.nins, "sbuf left:", nc.sbuf_bytes_remaining)
    return nc


_PROG = {}


def _layer_maps(inp, l, xs, flag, ntok):
    ws = {"wa": _arrange_w_in(np.asarray(inp["w_in"][l], np.float32)),
          "wglu": np.asarray(inp["ssm_glu_w"][l], np.float32),
          "wpr": np.concatenate([inp["w_proj_a"][l], inp["w_proj_b"][l], inp["w_proj_c"][l]], axis=0).astype(np.float32),
          "wo": np.asarray(inp["w_out"][l], np.float32),
          "w1": np.asarray(inp["w_ff1"][l], np.float32),
          "w2": np.asarray(inp["w_ff2"][l], np.float32)}
    wf = _flat_weights(ws)
    P = _pack_params(inp, l, flag)
    maps = []
    for b in range(len(xs)):
        maps.append({
            "x_in": np.ascontiguousarray(xs[b][:ntok], dtype=np.float32),
            "pos": np.ascontiguousarray(inp["positions"][b:b + 1, :ntok], dtype=np.int32),
            "P": P, "wf": wf,
        })
    return maps


def kernel(**inputs):
    inp = {k: np.asarray(v) for k, v in inputs.items()}
    ntiles = SEQ // TT
    if ntiles not in _PROG:
        _PROG[ntiles] = build_program(ntiles)
    nc = _PROG[ntiles]
    xs = [inp["x"][b] for b in range(BATCH)]
    for l in range(DEPTH):
        maps = _layer_maps(inp, l, xs, 1.0 if l == DEPTH - 1 else 0.0, SEQ)
        res = run_bass_kernel_spmd(nc, maps, core_ids=list(range(BATCH)))
        xs = [np.asarray(r["y"]) for r in res.results]
    return np.stack(xs, axis=0).astype(np.float32)
```

```python
import math
import os
from contextlib import ExitStack
import numpy as np
import concourse.bass as bass
import concourse.mybir as mybir
from concourse.bass_utils import run_bass_kernel_spmd

F32 = mybir.dt.float32
BF16 = mybir.dt.bfloat16
I32 = mybir.dt.int32
ALU = mybir.AluOpType
AF = mybir.ActivationFunctionType
AX = mybir.AxisListType

D = 1024
SEQ = 8192
BATCH = 4
DEPTH = 2
TT = 512
EPS = 1e-6
BIG = 30000.0
TOPK = 256
NBIS = 22
BIS_LO, BIS_W = -16.0, 32.0


class Reg:
    __slots__ = ("w", "r")

    def __init__(self):
        self.w = None
        self.r = {}


class T:
    __slots__ = ("ap", "reg")

    def __init__(self, ap, reg=None):
        self.ap = ap
        self.reg = reg if reg is not None else Reg()

    def __getitem__(self, idx):
        return T(self.ap[idx], self.reg)

    def v(self, fn):
        return T(fn(self.ap), self.reg)

    def wr(self, reg):
        return T(self.ap, reg)


def _regs(xs):
    out = []
    for x in xs:
        if isinstance(x, T):
            x = x.reg
        if isinstance(x, Reg):
            out.append(x)
        elif isinstance(x, (tuple, list)):
            out.extend(x)
    return out


def _ap(x):
    return x.ap if isinstance(x, T) else x


class Sched:
    def __init__(self, nc, stack):
        self.nc = nc
        self.eng = {"pe": nc.tensor, "act": nc.scalar, "dve": nc.vector, "pool": nc.gpsimd, "sp": nc.sync}
        self.sem, self.cnt, self.seen = {}, {}, {}
        for e in ["pe", "act", "dve", "pool"]:
            self.sem[e] = stack.enter_context(nc.semaphore("s_" + e))
        self.NDS = 4
        self.dcnt = {}
        for q in ["sp", "pool"]:
            self.dcnt[q] = 0
            for i in range(self.NDS):
                self.sem["dma_%s%d" % (q, i)] = stack.enter_context(nc.semaphore("s_dma_%s%d" % (q, i)))
        for k in self.sem:
            self.cnt[k] = 0
        self.names = list(self.sem.keys())
        for e in ["pe", "act", "dve", "pool", "sp"]:
            self.seen[e] = {k: 0 for k in self.names}
        self.nins = 0
        self.psum_regs = set()
        self.act_dummy = None
        self.fence = None

    def _waits(self, e, reads, writes, acc):
        deps = {}

        def add(d):
            if d is not None and deps.get(d[0], 0) < d[1]:
                deps[d[0]] = d[1]
        for r in reads:
            add(r.w)
        for w in writes:
            if not acc:
                add(w.w)
            for k, t in w.r.items():
                add((k, t))
        eh, seen = self.eng[e], self.seen[e]
        if e == "pe" and self.fence is not None and seen["act"] < deps.get("act", 0):
            ta = deps.pop("act")
            pseen = self.seen["pool"]
            if pseen["act"] < ta:
                self.eng["pool"].wait_ge(self.sem["act"], ta)
                pseen["act"] = ta
            self.eng["pool"].memset(self.fence, 0.0).then_inc(self.sem["pool"], 1)
            self.cnt["pool"] += 1
            self.nins += 1
            seen["act"] = ta
            if deps.get("pool", 0) < self.cnt["pool"]:
                deps["pool"] = self.cnt["pool"]
        for k, t in deps.items():
            if seen[k] < t:
                eh.wait_ge(self.sem[k], t)
                seen[k] = t

    def op(self, e, fn, reads=(), writes=(), acc=False):
        reads, writes = _regs(reads), _regs(writes)
        self._waits(e, reads, writes, acc)
        ins = fn(self.eng[e])
        self.cnt[e] += 1
        t = self.cnt[e]
        ins.then_inc(self.sem[e], 1)
        self.nins += 1
        if e == "act" and self.act_dummy is not None and any(id(r) in self.psum_regs for r in reads):
            d0, d1 = self.act_dummy
            self.eng["act"].copy(d0, d1).then_inc(self.sem[e], 1)
            self.cnt[e] += 1
            t = self.cnt[e]
            self.nins += 1
        for r in reads:
            if r.r.get(e, 0) < t:
                r.r[e] = t
        for w in writes:
            w.w = (e, t)
            if not acc:
                w.r = {}
        return ins

    def dma(self, q, out, in_, **kw):
        reads, writes = _regs([in_]), _regs([out])
        self._waits(q, reads, writes, False)
        k = "dma_%s%d" % (q, self.dcnt[q] % self.NDS)
        self.dcnt[q] += 1
        if self.seen[q][k] < self.cnt[k]:
            self.eng[q].wait_ge(self.sem[k], self.cnt[k])
            self.seen[q][k] = self.cnt[k]
        ins = self.eng[q].dma_start(out=_ap(out), in_=_ap(in_), **kw)
        self.cnt[k] += 16
        t = self.cnt[k]
        ins.then_inc(self.sem[k], 16)
        self.nins += 1
        for r in reads:
            if r.r.get(k, 0) < t:
                r.r[k] = t
        for w in writes:
            w.w = (k, t)
            w.r = {}
        return ins

    def finish(self):
        for e in ["sp", "act", "pool", "dve", "pe"]:
            for k in self.names:
                if self.cnt[k] > self.seen[e][k]:
                    self.eng[e].wait_ge(self.sem[k], self.cnt[k])
                    self.seen[e][k] = self.cnt[k]

    def mm(self, out, lhsT, rhs, start, stop):
        return self.op("pe", lambda p: p.matmul(out.ap, lhsT=lhsT.ap, rhs=rhs.ap, start=start, stop=stop),
                       reads=[lhsT, rhs], writes=[out], acc=not start)

    def tr(self, out, in_, ident):
        return self.op("pe", lambda p: p.transpose(out.ap, in_.ap, ident.ap), reads=[in_, ident], writes=[out])

    def actf(self, out, in_, func, bias=None, scale=1.0, accum=None, eng="act"):
        kw = {}
        if bias is not None:
            kw["bias"] = _ap(bias)
        if accum is not None:
            kw["accum_out"] = accum.ap
        wr = [out] + ([accum] if accum is not None else [])
        return self.op(eng, lambda a: a.activation(out.ap, in_.ap, func, scale=_ap(scale), **kw),
                       reads=[in_, bias, scale], writes=wr)

    def tt(self, eng, out, a, b, op):
        return self.op(eng, lambda v: v.tensor_tensor(out.ap, a.ap, b.ap, op), reads=[a, b], writes=[out])

    def ts(self, eng, out, a, s1, op0, s2=None, op1=None, accum=None):
        kw = {}
        if op1 is not None:
            kw["op1"] = op1
        if accum is not None:
            kw["accum_out"] = accum.ap
        wr = [out] + ([accum] if accum is not None else [])
        return self.op(eng, lambda v: v.tensor_scalar(out.ap, a.ap, _ap(s1), _ap(s2) if s2 is not None else None, op0, **kw),
                       reads=[a, s1, s2], writes=wr)

    def stt(self, eng, out, a, s, b, op0, op1):
        eng = "dve"
        return self.op(eng, lambda v: v.scalar_tensor_tensor(out.ap, a.ap, _ap(s), b.ap, op0, op1),
                       reads=[a, s, b], writes=[out])

    def copy(self, eng, out, in_):
        if eng == "act":
            return self.op("act", lambda a: a.copy(out.ap, in_.ap), reads=[in_], writes=[out])
        return self.op(eng, lambda v: v.tensor_copy(out.ap, in_.ap), reads=[in_], writes=[out])

    def memset(self, eng, out, val):
        return self.op(eng, lambda v: v.memset(out.ap, val), writes=[out])


IN_SIZES = (256, 384, 64, 64, 128, 32, 4, 192, 192, 384, 384, 3072)
IN_OFF = np.concatenate([[0], np.cumsum(IN_SIZES)]).astype(int)
(O_U, O_DQ, O_DK, O_DV, O_IQ, O_IK, O_IW, O_RQ, O_RK, O_RV, O_RG, O_GT) = [int(v) for v in IN_OFF[:12]]


def _swap_cols(base, hd, rot):
    half = rot // 2
    idx = list(range(hd))
    for i in range(half):
        idx[i], idx[i + half] = i + half, i
    return [base + i for i in idx]


def _wa_columns():
    fm = list(range(O_U, O_U + 256))
    for h in range(6):
        fm += list(range(O_DQ + 64 * h, O_DQ + 64 * h + 64)) + _swap_cols(O_DQ + 64 * h, 64, 16)
    fm += list(range(O_DK, O_DK + 64)) + _swap_cols(O_DK, 64, 16)
    for h in range(4):
        fm += list(range(O_IQ + 32 * h, O_IQ + 32 * h + 32)) + _swap_cols(O_IQ + 32 * h, 32, 8)
    fm += list(range(O_IK, O_IK + 32)) + _swap_cols(O_IK, 32, 8)
    for off in (O_RQ, O_RK):
        for i in range(2):
            z, s = [], []
            for h in (2 * i, 2 * i + 1):
                z += list(range(off + 48 * h, off + 48 * h + 48)) + [-1] * 16
                s += _swap_cols(off + 48 * h, 48, 48) + [-1] * 16
            fm += z + s
    tm = list(range(O_DV, O_DV + 64)) + list(range(O_IW, O_IW + 4))
    tm += list(range(O_RV, O_RV + 384)) + list(range(O_RG, O_RG + 384))
    gt = list(range(O_GT, O_GT + 3072))
    return fm + tm + gt


WA_COLS = _wa_columns()
NA = len(WA_COLS)
A_U = 0
A_DQ = 256
A_DK = A_DQ + 768
A_IQ = A_DK + 128
A_IK = A_IQ + 256
A_RQ = A_IK + 64
A_RK = A_RQ + 512
A_VW = A_RK + 512
A_RV = A_VW + 68
A_RG = A_RV + 384
A_GT = A_RG + 384
assert A_GT + 3072 == NA


def _plan():
    r = lambda a, n: list(range(a, a + n))
    pl = [("u", "wa", 8, r(A_U, 256))]
    for i in range(3):
        pl.append(("dq%d" % i, "wa", 8, r(A_DQ + 256 * i, 256)))
    pl.append(("dkik", "wa", 8, r(A_DK, 128) + r(A_IK, 64)))
    pl.append(("iq", "wa", 8, r(A_IQ, 256)))
    for i in range(2):
        pl.append(("rq%d" % i, "wa", 8, r(A_RQ + 256 * i, 256)))
    for i in range(2):
        pl.append(("rk%d" % i, "wa", 8, r(A_RK + 256 * i, 256)))
    pl.append(("vw", "wa", 8, r(A_VW, 68)))
    pl.append(("rv", "wa", 8, r(A_RV, 384)))
    pl.append(("rg", "wa", 8, r(A_RG, 384)))
    pl.append(("glu", "wglu", 2, r(0, 256)))
    for ft in range(8):
        pl.append(("pr%d" % ft, "wpr", 8, r(128 * ft, 128)))
        pl.append(("gt%d" % ft, "wa", 8, r(A_GT + 128 * ft, 128) + r(A_GT + 1024 + 128 * ft, 128) + r(A_GT + 2048 + 128 * ft, 128)))
    for ft in range(8):
        pl.append(("wo%d" % ft, "wo", 8, r(128 * ft, 128)))
    for f4 in range(8):
        pl.append(("w1_%d" % f4, "w1", 8, r(512 * f4, 512)))
    for ft in range(8):
        pl.append(("w2_%d" % ft, "w2", 32, r(128 * ft, 128)))
    return pl


PLAN = _plan()
PLAN_OFF = []
_o = 0
for _nm, _s, _k, _c in PLAN:
    PLAN_OFF.append(_o)
    _o += _k * len(_c)
NW = _o


def _flat_weights(ws):
    out = np.empty((128, NW), np.float32)
    for (nm, s, nkt, cols), o in zip(PLAN, PLAN_OFF):
        w = ws[s][:, cols]
        n = len(cols)
        out[:, o:o + nkt * n] = w.reshape(nkt, 128, n).transpose(1, 0, 2).reshape(128, nkt * n)
    return out


_pc = {}
_off = 0
for _n, _w in [("g1", 8), ("g2", 8), ("gf", 8), ("glub", 2), ("ssd", 2), ("lre", 8), ("lim", 8), ("lst", 8),
               ("bre", 128), ("bim", 128), ("cre", 128), ("cim", 128), ("flag", 1), ("retg", 384),
               ("invD", 1), ("sgnD", 1), ("invI", 1), ("sgnI", 1), ("invR", 1), ("sgnR", 1),
               ("ramp", 256), ("intra", 256), ("qdec", 128), ("kdec", 4), ("diag", 128)]:
    _pc[_n] = (_off, _w)
    _off += _w
NP_ = _off


def _const_tables():
    c = {}
    p = np.arange(128)
    d = p % 64
    inv = np.where(d < 16, np.exp(-math.log(500000.0) * (d % 8) * (2.0 / 16)), 0.0)
    c["invD"] = inv[:, None]
    c["sgnD"] = np.where(d < 8, -1.0, np.where(d < 16, 1.0, 0.0))[:, None]
    d = p % 32
    inv = np.where(d < 8, np.exp(-math.log(500000.0) * (d % 4) * (2.0 / 8)), 0.0)
    c["invI"] = inv[:, None]
    c["sgnI"] = np.where(d < 4, -1.0, np.where(d < 8, 1.0, 0.0))[:, None]
    d = p % 64
    inv = np.where(d < 48, np.exp(-math.log(10000.0) * (d % 24) * (2.0 / 48)), 0.0)
    c["invR"] = inv[:, None]
    c["sgnR"] = np.where(d < 24, -1.0, np.where(d < 48, 1.0, 0.0))[:, None]
    c["ramp"] = np.broadcast_to(np.arange(1, 257, dtype=np.float64)[None, :], (128, 256))
    log_g = np.log1p(-np.exp2(-5.0 - np.arange(4)))
    sc = 48 ** -0.5
    m = (p % 64)[:, None]
    cc = np.arange(64)[None, :]
    intra = np.zeros((128, 4, 64))
    qdec = np.zeros((128, 2, 64))
    kdec = np.zeros((128, 4))
    for h in range(4):
        intra[:, h, :] = np.where(cc >= m, np.exp(log_g[h] * np.maximum(cc - m, 0)), 0.0) * sc
        kdec[:, h] = np.exp(log_g[h] * (63.0 - (p % 64)))
    for pair in range(2):
        for half in range(2):
            h = 2 * pair + half
            qdec[64 * half:64 * half + 64, pair, :] = (np.exp(log_g[h] * (np.arange(64) + 1.0)) * sc)[None, :]
    c["intra"] = intra.reshape(128, 256)
    c["qdec"] = qdec.reshape(128, 128)
    c["kdec"] = kdec
    q = np.arange(128)[:, None]
    k = np.arange(128)[None, :]
    c["diag"] = np.where(k < (q // 64 + 1) * 64, 0.0, -BIG)
    c["cdec"] = [float(np.exp(log_g[h] * 64.0)) for h in range(4)]
    return c


CONST = _const_tables()


def _pack_params(inp, l, flag):
    P = np.zeros((128, NP_), np.float32)

    def put(name, arr):
        o, w = _pc[name]
        P[:, o:o + w] = np.asarray(arr, np.float32).reshape(128, w)
    put("g1", inp["norm1_g"][l].reshape(8, 128).T)
    put("g2", inp["norm2_g"][l].reshape(8, 128).T)
    put("gf", inp["final_norm_g"].reshape(8, 128).T)
    put("glub", inp["ssm_glu_b"][l].reshape(2, 128).T)
    put("ssd", inp["ssm_d"][l].reshape(2, 128).T)
    put("lre", inp["ssm_lambda_re"][l].reshape(8, 128).T)
    put("lim", inp["ssm_lambda_im"][l].reshape(8, 128).T)
    put("lst", np.repeat(inp["ssm_log_step"][l], 64).reshape(8, 128).T)
    put("bre", inp["ssm_b_re"][l].reshape(8, 128, 16).transpose(1, 0, 2))
    put("bim", inp["ssm_b_im"][l].reshape(8, 128, 16).transpose(1, 0, 2))
    put("cre", inp["ssm_c_re"][l].transpose(0, 2, 1).reshape(8, 128, 16).transpose(1, 0, 2))
    put("cim", inp["ssm_c_im"][l].transpose(0, 2, 1).reshape(8, 128, 16).transpose(1, 0, 2))
    put("flag", np.full((128, 1), flag))
    put("retg", np.broadcast_to(inp["ret_norm_g"][l][None, :], (128, 384)))
    for n in ["invD", "sgnD", "invI", "sgnI", "invR", "sgnR", "ramp", "intra", "qdec", "kdec", "diag"]:
        put(n, CONST[n])
    return P


def _arrange_w_in(w):
    wz = np.concatenate([w, np.zeros((w.shape[0], 1), w.dtype)], axis=1)
    return wz[:, WA_COLS]


def build_program(ntiles, nlayers=1, debug=None):
    nc = bass.Bass("TRN2", target_bir_lowering=False)
    NTOK = ntiles * TT
    dr = lambda n, s, dt, kind: nc.dram_tensor(n, s, dt, kind=kind).ap()
    x_in = dr("x_in", [NTOK, D], F32, "ExternalInput")
    pos_in = dr("pos", [1, NTOK], I32, "ExternalInput")
    P_in = dr("P", [nlayers * 128, NP_], F32, "ExternalInput")
    wf_in = dr("wf", [nlayers * 128, NW], F32, "ExternalInput")
    y_out = dr("y", [NTOK, D], F32, "ExternalOutput")
    dbg_out = None
    if debug:
        dbg_out = dr("dbg", list(debug[1]), F32, "ExternalOutput")
    wf_b = dr("wf_b", [nlayers * 128, NW], BF16, "Internal")
    xmid = dr("xmid", [NTOK, D], F32, "Internal") if nlayers > 1 else None
    xmr = [Reg() for _ in range(ntiles)]

    with ExitStack() as st:
        S = Sched(nc, st)

        _sbc = {}

        def sb(name, shape, dt=F32):
            if name not in _sbc:
                _sbc[name] = T(st.enter_context(nc.sbuf_tensor(name, shape, dt))[:])
            return _sbc[name]

        print("sbuf at start:", nc.sbuf_bytes_remaining)
        CH = 2048
        wchunks = [[] for _ in range(nlayers)]
        for l_ in range(nlayers):
            for c0 in range(0, NW, CH):
                c1 = min(NW, c0 + CH)
                rg = Reg()
                S.dma("pool", T(wf_b[l_ * 128:(l_ + 1) * 128, c0:c1], rg), T(wf_in[l_ * 128:(l_ + 1) * 128, c0:c1]))
                wchunks[l_].append((c0, c1, rg))
        cur = [0]

        Pt = sb("Pt", [128, NP_])

        def pc(name, a=None, b=None):
            o, w = _pc[name]
            a = 0 if a is None else a
            b = w if b is None else b
            return Pt[:, o + a:o + b]

        ident = sb("ident", [128, 128])
        S.memset("pool", ident, 0.0)
        S.op("pool", lambda g: g.affine_select(ident.ap, ident.ap, pattern=[[-1, 128]], compare_op=ALU.not_equal,
                                               fill=1.0, base=0, channel_multiplier=1), reads=[ident], writes=[ident])
        identb = sb("identb", [128, 128], BF16)
        S.copy("dve", identb, ident)
        onesb = sb("onesb", [128, 128], BF16)
        S.memset("pool", onesb, 1.0)

        xT = sb("xT", [128, 8, TT])
        hT = sb("hT", [128, 8, TT], BF16)
        sq = sb("sq", [128, 8, TT], BF16)
        rstd = sb("rstd", [128, TT])
        big = sb("big", [128, 8192])
        aT = big.v(lambda a: a.bitcast(BF16).rearrange("p (k t) -> p k t", t=TT)[:, 0:32, :])
        stg = big.v(lambda a: a[:, 0:4 * D].rearrange("p (j f) -> p j f", f=D))
        KT = sb("KT", [128, NTOK], BF16)
        VC = sb("VC", [128, NTOK // 128, 96], BF16)
        KTr = [Reg() for _ in range(ntiles)]
        VCr = [Reg() for _ in range(ntiles)]
        S.memset("pool", VC, 1.0)
        for r in VCr:
            r.w = VC.reg.w
        uT = sb("uT", [128, 2, TT])
        qT = sb("qT", [64, 6, TT], BF16)
        iqT = sb("iqT", [128, 4, TT], BF16)
        iw = sb("iw", [128, 4, 4])
        rqT = sb("rqT", [128, 2, TT], BF16)
        rkT = sb("rkT", [128, 2, TT], BF16)
        rv = sb("rv", [128, 4, 384], BF16)
        rgs = sb("rgs", [128, 4, 384], BF16)
        yaT = sb("yaT", [128, 2, TT], BF16)
        ybT = sb("ybT", [128, 3, TT], BF16)
        ycT = sb("ycT", [128, 3, TT], BF16)
        NSL = 11
        U = st.enter_context(nc.sbuf_tensor("U", [128, NSL * 512], F32))[:]
        Ureg = [Reg() for _ in range(NSL)]

        def carve(s0, ns, shape_fn=None, dt=F32):
            ap = U[:, s0 * 512:(s0 + ns) * 512]
            if dt == BF16:
                ap = ap.bitcast(BF16)
            elif dt == I32:
                ap = ap.bitcast(I32)
            if shape_fn is not None:
                ap = shape_fn(ap)
            return T(ap, tuple(Ureg[s0:s0 + ns]))
        posi = carve(0, 1, None, I32)
        posf = carve(1, 1)
        ang = carve(2, 1)
        rope = carve(3, 6, lambda a: a.rearrange("p (s t) -> p s t", t=TT))
        ropa = carve(9, 1)
        ropb = carve(10, 1)
        TC = 256
        r2 = lambda a: a.rearrange("p (r t) -> p r t", t=TC)
        w12 = carve(0, 1, r2)
        w43 = carve(1, 1, r2)
        vv = carve(2, 1, r2)
        xs_ = carve(3, 1, r2)
        xo = carve(4, 1, r2)
        ypre = carve(5, 2, lambda a: a.rearrange("p (r t) -> p r t", t=TT))
        ypb = carve(7, 1, lambda a: a.rearrange("p (r t) -> p r t", t=TT), BF16)
        gsig = carve(8, 1)
        qd = carve(0, 1, lambda a: a.rearrange("p (r t) -> p r t", t=TT), BF16)
        kd = carve(1, 1, lambda a: a[:, 0:768].rearrange("p (j h d) -> p j h d", j=4, h=4), BF16)
        yr = carve(2, 1, lambda a: a[:, 0:384])
        ysq = carve(3, 1, lambda a: a[:, 0:384])
        ycb = carve(4, 1, lambda a: a[:, 0:384], BF16)
        At = carve(5, 1, lambda a: a[:, 0:64], BF16)
        st4 = carve(6, 1, lambda a: a[:, 0:16].rearrange("p (a b) -> p a b", b=4))
        relu_t = carve(0, 1)
        pT = carve(1, 1, lambda a: a[:, 0:768].rearrange("p (g c t) -> p g c t", g=2, c=3), BF16)
        pvs = carve(2, 1, lambda a: a[:, 0:390].rearrange("p (h e) -> p h e", e=65))
        ybt = carve(3, 1, lambda a: a[:, 0:384], BF16)
        m01 = carve(4, 1, lambda a: a[:, 0:128], BF16)
        mT = carve(5, 1, lambda a: a[:, 0:128], BF16)
        bis = carve(6, 1, lambda a: a[:, 0:8])
        lo, mid, nmid, ssA, ssB, gcol, rden6 = bis[:, 0:1], bis[:, 1:2], bis[:, 2:3], bis[:, 3:4], bis[:, 4:5], bis[:, 5:6], None
        rden = carve(7, 1, lambda a: a[:, 0:6])
        ot = carve(8, 2, lambda a: a[:, 0:768])
        junk = sq.v(lambda a: a.rearrange("p k t -> p (k t)"))
        gA = carve(0, 1)
        gB = carve(1, 1)
        gC = carve(2, 1)
        rl = carve(3, 1, None, BF16)[:, 0:TT]
        NWB = 2
        WBW = 4096
        wbuf = [sb("wbuf%d" % i, [128, WBW], BF16) for i in range(NWB)]
        wctr = [0]
        wpos = [0]

        def wnext(name):
            nm, s_, nkt, cols = PLAN[wpos[0]]
            off = PLAN_OFF[wpos[0]]
            assert nm == name, (nm, name)
            wpos[0] += 1
            n = len(cols)
            b = wbuf[wctr[0] % NWB]
            wctr[0] += 1
            v = b.v(lambda a: a[:, 0:nkt * n])
            regs = tuple(rg for (c0, c1, rg) in wchunks[cur[0]] if c0 < off + nkt * n and c1 > off)
            S.dma("sp", v, T(wf_b[cur[0] * 128:(cur[0] + 1) * 128, off:off + nkt * n], regs))
            return v.v(lambda a: a.rearrange("p (k n) -> p k n", n=n))

        pbank = [T(st.enter_context(nc.psum_tensor("ps%d" % i, [128, 512], F32))[:]) for i in range(8)]
        prot = [0]
        for b_ in pbank:
            S.psum_regs.add(id(b_.reg))
        if os.environ.get('FENCE'):
            fnc = st.enter_context(nc.sbuf_tensor("fnc", [128, 4], F32))[:]
            S.fence = fnc[0:1, 0:1]
        if os.environ.get('DUMMY'):
            dmy = st.enter_context(nc.sbuf_tensor("dmy", [128, 4], F32))[:]
            S.memset("pool", T(dmy), 0.0)
            S.act_dummy = (dmy[:, 0:1], dmy[:, 2:3])

        def ps():
            b = pbank[prot[0] % 6]
            prot[0] += 1
            return b
        PD0, PD1 = pbank[6], pbank[7]

        TWO_PI = 2.0 * math.pi

        C1 = 6.28125
        C2 = 4058.0 / 2 ** 21
        C3 = TWO_PI - C1 - C2
        PI_LO = 3.1415925

        def sincos(out_sin, out_cos, angle, tmp_t, ki_t, kf_t):
            S.ts("dve", tmp_t, angle, 1.0 / TWO_PI, ALU.mult)
            _ce = "dve" if os.environ.get('CVT_DVE') else "pool"
            S.copy(_ce, ki_t, tmp_t)
            S.copy(_ce, kf_t, ki_t)
            S.stt("dve", tmp_t, kf_t, -C1, angle, ALU.mult, ALU.add)
            S.stt("dve", tmp_t, kf_t, -C2, tmp_t, ALU.mult, ALU.add)
            S.stt("dve", tmp_t, kf_t, -C3, tmp_t, ALU.mult, ALU.add)
            S.ts("dve", tmp_t, tmp_t, -PI_LO, ALU.max, PI_LO, ALU.min)
            S.actf(out_sin, tmp_t, AF.Sin)
            S.ts("dve", kf_t, tmp_t, 0.5 * math.pi, ALU.is_gt, -TWO_PI, ALU.mult)
            S.stt("dve", tmp_t, tmp_t, 0.5 * math.pi, kf_t, ALU.add, ALU.add)
            S.ts("dve", tmp_t, tmp_t, -PI_LO, ALU.max, PI_LO, ALU.min)
            S.actf(out_cos, tmp_t, AF.Sin)

        for l in range(nlayers):
            cur[0] = l
            S.dma("sp", Pt, T(P_in[l * 128:(l + 1) * 128, :]))
            x_src = x_in if l == 0 else xmid
            y_dst = y_out if l == nlayers - 1 else xmid
            s5 = sb("s5p", [128, 12, 8])
            lre, lim, lst = pc("lre"), pc("lim"), pc("lst")
            stp, are, th, rr, ct_, st_, lbr, lbi, den, fre, fim, tmp = [s5[:, i, :] for i in range(12)]
            S.actf(stp, lst, AF.Exp)
            S.tt("dve", are, lre, stp, ALU.mult)
            S.tt("dve", th, lim, stp, ALU.mult)
            S.actf(rr, are, AF.Exp)
            s5i = sb("s5i", [128, 8], I32)
            s5f = sb("s5f", [128, 8])
            sincos(st_, ct_, th, tmp, s5i, s5f)
            S.tt("dve", lbr, rr, ct_, ALU.mult)
            S.tt("dve", lbi, rr, st_, ALU.mult)
            S.tt("dve", den, lre, lre, ALU.mult)
            S.tt("dve", tmp, lim, lim, ALU.mult)
            S.tt("dve", den, den, tmp, ALU.add)
            S.op("dve", lambda v: v.reciprocal(den.ap, den.ap), reads=[den], writes=[den])
            S.ts("dve", lbr, lbr, -1.0, ALU.add)
            S.tt("dve", fre, lbr, lre, ALU.mult)
            S.tt("dve", tmp, lbi, lim, ALU.mult)
            S.tt("dve", fre, fre, tmp, ALU.add)
            S.tt("dve", fre, fre, den, ALU.mult)
            S.tt("dve", fim, lbi, lre, ALU.mult)
            S.tt("dve", tmp, lbr, lim, ALU.mult)
            S.tt("dve", fim, fim, tmp, ALU.subtract)
            S.tt("dve", fim, fim, den, ALU.mult)
            bbr = sb("bbr", [128, 8, 16])
            bbi = sb("bbi", [128, 8, 16])
            tb = sb("tb", [128, 16])
            k16 = lambda a: a.rearrange("p (k c) -> p k c", c=16)
            bre, bim, cre, cim = pc("bre").v(k16), pc("bim").v(k16), pc("cre").v(k16), pc("cim").v(k16)
            BT = sb("BT", [128, 8, 2, 128])
            CT = sb("CT", [128, 8, 2, 128])
            S.memset("pool", CT, 0.0)
            pad = sb("pad", [128, 2, 128])
            for k in range(8):
                S.ts("dve", bbr[:, k, :], bre[:, k, :], fre[:, k:k + 1], ALU.mult)
                S.ts("dve", tb, bim[:, k, :], fim[:, k:k + 1], ALU.mult)
                S.tt("dve", bbr[:, k, :], bbr[:, k, :], tb, ALU.subtract)
                S.ts("dve", bbi[:, k, :], bim[:, k, :], fre[:, k:k + 1], ALU.mult)
                S.ts("dve", tb, bre[:, k, :], fim[:, k:k + 1], ALU.mult)
                S.tt("dve", bbi[:, k, :], bbi[:, k, :], tb, ALU.add)
                c0 = 32 * (k % 4)
                S.memset("pool", pad, 0.0)
                for ri, src in enumerate((bbr, bbi)):
                    S.copy("pool", pad[0:64, ri, c0:c0 + 16], src[0:64, k, :])
                    S.copy("pool", pad[64:128, ri, c0 + 16:c0 + 32], src[64:128, k, :])
                for ri in range(2):
                    pb = ps()
                    S.tr(pb[:, 0:128], pad[:, ri, :], ident)
                    S.copy("dve", BT[:, k, ri, :], pb[:, 0:128])
                S.copy("pool", CT[0:64, k, 0, c0:c0 + 16], cre[0:64, k, :])
                S.copy("pool", CT[64:128, k, 0, c0 + 16:c0 + 32], cre[64:128, k, :])
                S.ts("dve", CT[0:64, k, 1, c0:c0 + 16], cim[0:64, k, :], -1.0, ALU.mult)
                S.ts("dve", CT[64:128, k, 1, c0 + 16:c0 + 32], cim[64:128, k, :], -1.0, ALU.mult)
            CS = sb("CS", [128, 8, 2, TC])
            for k in range(8):
                S.ts("dve", ropa[:, 0:TC], pc("ramp"), th[:, k:k + 1], ALU.mult)
                sincos(CS[:, k, 1, :], CS[:, k, 0, :], ropa[:, 0:TC], ropb[:, 0:TC], posi[:, 0:TC], posf[:, 0:TC])
            xprev = sb("xprev", [128, 8, 2])
            S.memset("pool", xprev, 0.0)
            Sst = sb("Sst", [128, 2, 96])
            Sbf = sb("Sbf", [128, 2, 96], BF16)
            S.memset("pool", Sst, 0.0)
            S.memset("pool", Sbf, 0.0)
            ogf = sb("ogf", [128, 8])
            omf = sb("omf", [128, 1])
            S.ts("dve", ogf, pc("gf"), pc("flag"), ALU.mult)
            S.ts("dve", omf, pc("flag"), -1.0, ALU.mult, 1.0, ALU.add)
            print("sbuf left after alloc:", nc.sbuf_bytes_remaining)

            def rms_stats():
                S.actf(sq, xT, AF.Square)
                pb = ps()
                for kt in range(8):
                    S.mm(pb, onesb, sq[:, kt, :], kt == 0, kt == 7)
                S.actf(rstd, pb, AF.Sqrt, bias=EPS, scale=1.0 / D)
                S.op("dve", lambda v: v.reciprocal(rstd.ap, rstd.ap), reads=[rstd], writes=[rstd])

            def rmsnorm(gname):
                rms_stats()
                for kt in range(8):
                    S.stt("dve" if kt % 2 == 0 else "pool", hT[:, kt, :], xT[:, kt, :], pc(gname, kt, kt + 1), rstd, ALU.mult, ALU.mult)

            evac_flip = [0]

            def evac_eng():
                evac_flip[0] += 1
                return "dve"

            hk = lambda kt: hT[:, kt, :]

            def fm_mm(wv, col0, m, rhs_of_kt, nkt=8, po=0):
                pb = ps()
                for kt in range(nkt):
                    S.mm(pb[po:po + m, :], wv[:, kt, col0:col0 + m], rhs_of_kt(kt), kt == 0, kt == nkt - 1)
                return pb

            def roped(wv, zc, sc, m, ty, out_t, po=0):
                pz = fm_mm(wv, zc, m, hk, po=po)
                pz2 = fm_mm(wv, sc, m, hk, po=po)
                S.tt("dve", ropa[po:po + m, :], pz[po:po + m, :], rope[po:po + m, 2 * ty, :], ALU.mult)
                S.tt("dve", ropb[po:po + m, :], pz2[po:po + m, :], rope[po:po + m, 2 * ty + 1, :], ALU.mult)
                S.tt("pool", out_t, ropa[po:po + m, :], ropb[po:po + m, :], ALU.add)

            class _Stop(Exception):
                pass

            def kstop(tag):
                if os.environ.get('KSTOP') == tag:
                    raise _Stop()
            for ti in range(ntiles):
              try:
                    t0 = ti * TT
                    wpos[0] = 0
                    S.dma("sp", stg, T(x_src[t0:t0 + TT, :].rearrange("(j p) f -> p j f", p=128), xmr[ti] if l > 0 else None))
                    for j in range(4):
                        for kt in range(0, 8, 4):
                            pb = ps()
                            for q in range(4):
                                S.tr(pb[:, q * 128:(q + 1) * 128], stg[:, j, (kt + q) * 128:(kt + q + 1) * 128], ident)
                            S.copy(evac_eng(), xT[:, kt:kt + 4, j * 128:(j + 1) * 128],
                                   pb.v(lambda a: a.rearrange("p (q t) -> p q t", t=128)))
                    S.dma("sp", posi, T(pos_in[0:1, t0:t0 + TT].partition_broadcast(128)))
                    S.copy("dve", posf, posi)
                    for ty, (inv, sgn) in enumerate((("invD", "sgnD"), ("invI", "sgnI"), ("invR", "sgnR"))):
                        S.ts("dve", ang, posf, pc(inv), ALU.mult)
                        sincos(rope[:, 2 * ty + 1, :], rope[:, 2 * ty, :], ang, ropa, posi, ropb)
                        S.ts("dve", rope[:, 2 * ty + 1, :], rope[:, 2 * ty + 1, :], pc(sgn), ALU.mult)
                    rmsnorm("g1")

                    wv = wnext("u")
                    for g in range(2):
                        pb = fm_mm(wv, 128 * g, 128, hk)
                        S.copy("dve", uT[:, g, :], pb)
                    for i in range(3):
                        wv = wnext("dq%d" % i)
                        for hh in range(2):
                            roped(wv, 128 * hh, 128 * hh + 64, 64, 0, qT[:, 2 * i + hh, :])
                    wv = wnext("dkik")
                    roped(wv, 0, 64, 64, 0, KT[0:64, t0:t0 + TT].wr(KTr[ti]))
                    roped(wv, 128, 160, 32, 1, KT[64:96, t0:t0 + TT].wr(KTr[ti]), po=64)
                    wv = wnext("iq")
                    for h in range(4):
                        roped(wv, 64 * h, 64 * h + 32, 32, 1, iqT[64:96, h, :], po=64)
                    for i in range(2):
                        wv = wnext("rq%d" % i)
                        roped(wv, 0, 128, 128, 2, rqT[:, i, :])
                    for i in range(2):
                        wv = wnext("rk%d" % i)
                        roped(wv, 0, 128, 128, 2, rkT[:, i, :])
                    wv = wnext("vw")
                    for j in range(4):
                        pb = ps()
                        for kt in range(8):
                            S.mm(pb[:, 0:68], hT[:, kt, j * 128:(j + 1) * 128], wv[:, kt, :], kt == 0, kt == 7)
                        S.copy("dve", VC[:, ti * 4 + j, 0:64].wr(VCr[ti]), pb[:, 0:64])
                        S.ts("dve", iw[:, j, :], pb[:, 64:68], 0.5 * 32 ** -0.5, ALU.mult)
                    wv = wnext("rv")
                    for j in range(4):
                        pb = ps()
                        for kt in range(8):
                            S.mm(pb[:, 0:384], hT[:, kt, j * 128:(j + 1) * 128], wv[:, kt, :], kt == 0, kt == 7)
                        S.copy("dve", rv[:, j, :], pb[:, 0:384])
                    wv = wnext("rg")
                    for j in range(4):
                        pb = ps()
                        for kt in range(8):
                            S.mm(pb[:, 0:384], hT[:, kt, j * 128:(j + 1) * 128], wv[:, kt, :], kt == 0, kt == 7)
                        S.actf(rgs[:, j, :], pb[:, 0:384], AF.Silu)

                    for ch in range(TT // TC):
                        cs = slice(ch * TC, (ch + 1) * TC)
                        for ct in range(2):
                            yacc = PD0 if ct == 0 else PD1
                            for kk in range(4):
                                k = 4 * ct + kk
                                pb = ps()
                                for ri in range(2):
                                    S.mm(pb[:, ri * TC:(ri + 1) * TC], BT[:, k, ri, :], uT[:, ct, cs], True, True)
                                bu = pb.v(r2)
                                S.tt("dve", w12, bu, CS[:, k, :, :], ALU.mult)
                                S.tt("dve", w43[:, 0, :], bu[:, 0, :], CS[:, k, 1, :], ALU.mult)
                                S.tt("dve", w43[:, 1, :], bu[:, 1, :], CS[:, k, 0, :], ALU.mult)
                                S.tt("pool", vv[:, 0, :], w12[:, 0, :], w12[:, 1, :], ALU.add)
                                S.tt("pool", vv[:, 1, :], w43[:, 1, :], w43[:, 0, :], ALU.subtract)
                                for ri in range(2):
                                    S.op("dve", lambda v, ri=ri, k=k: v.tensor_tensor_scan(
                                        xs_.ap[:, ri, :], rr.ap[:, k:k + 1].to_broadcast([128, TC]), vv.ap[:, ri, :],
                                        xprev.ap[:, k, ri:ri + 1], ALU.mult, ALU.add),
                                        reads=[rr, vv, xprev], writes=[xs_])
                                S.tt("pool", w12, xs_, CS[:, k, :, :], ALU.mult)
                                S.tt("pool", w43[:, 0, :], xs_[:, 0, :], CS[:, k, 1, :], ALU.mult)
                                S.tt("pool", w43[:, 1, :], xs_[:, 1, :], CS[:, k, 0, :], ALU.mult)
                                S.tt("dve", xo[:, 0, :], w12[:, 0, :], w12[:, 1, :], ALU.subtract)
                                S.tt("dve", xo[:, 1, :], w43[:, 0, :], w43[:, 1, :], ALU.add)
                                S.copy("act", xprev[:, k, :], xo[:, :, TC - 1])
                                for ri in range(2):
                                    S.mm(yacc[:, 0:TC], CT[:, k, ri, :], xo[:, ri, :], kk == 0 and ri == 0, kk == 3 and ri == 1)
                            yv = ypre[:, ct, cs]
                            S.stt("dve", yv, uT[:, ct, cs], pc("ssd", ct, ct + 1), yacc[:, 0:TC], ALU.mult, ALU.add)
                            S.tt("pool", w12[:, 0, :], yv, yv, ALU.mult)
                            S.ts("dve", w12[:, 0, :], w12[:, 0, :], 0.044715, ALU.mult, 1.0, ALU.add)
                            S.tt("pool", w12[:, 0, :], w12[:, 0, :], yv, ALU.mult)
                            S.actf(w12[:, 1, :], w12[:, 0, :], AF.Sigmoid, scale=2.0 * math.sqrt(2.0 / math.pi))
                            S.tt("dve", yv, yv, w12[:, 1, :], ALU.mult)
                    S.copy("act", ypb, ypre)
                    wv = wnext("glu")
                    for ct in range(2):
                        pb = fm_mm(wv, 128 * ct, 128, lambda kt: ypb[:, kt, :], nkt=2)
                        S.actf(gsig, pb, AF.Sigmoid, bias=pc("glub", ct, ct + 1))
                        S.tt("dve", yaT[:, ct, :], ypre[:, ct, :], gsig, ALU.mult)
                    if debug:
                        debug[0](S, locals(), dbg_out, ti, "s5")

                    _skipret = ti >= 1 and 'ret' in os.environ.get('KSKIP', '')
                    c64 = lambda a: a.rearrange("p (c t) -> p c t", t=64)
                    for i in range(2):
                        S.tt("pool", qd[:, i, :].v(c64), rqT[:, i, :].v(c64),
                             pc("qdec", 64 * i, 64 * i + 64).v(lambda a: a.unsqueeze(1).to_broadcast([128, 8, 64])), ALU.mult)
                    for j in range(4):
                        pb = ps()
                        pbv = pb.v(lambda a: a.bitcast(BF16))
                        for h in range(4):
                            base = 64 * (h % 2)
                            S.tr(pbv[:, 64 * h:64 * h + 48], rkT[base:base + 48, h // 2, j * 128:(j + 1) * 128], identb[base:base + 48, base:base + 48])
                        for h in range(4):
                            S.ts("dve", kd[:, j, h, :], pbv[:, 64 * h:64 * h + 48], pc("kdec", h, h + 1), ALU.mult)
                    for j in range(4):
                        for half in range(2):
                            rb = 64 * half
                            cs0 = j * 128 + rb
                            pout = pbank[4 + half] if not os.environ.get('POUT_PD1') else (PD1 if os.environ.get('POUT_PD1') == '1' else pbank[4])
                            for h in range(4):
                                base = 64 * (h % 2)
                                pair = h // 2
                                pa = pbank[(2 * h) % 4]
                                S.mm(pa[rb:rb + 64, 0:64], rkT[base:base + 48, pair, cs0:cs0 + 64], rqT[base:base + 48, pair, cs0:cs0 + 64], True, True)
                                kstop('r%d%d_h%d_s0' % (j, half, h))
                                S.tt("dve", At[rb:rb + 64, :], pa[rb:rb + 64, 0:64], pc("intra", 64 * h, 64 * h + 64)[rb:rb + 64, :], ALU.mult)
                                kstop('r%d%d_h%d_s1' % (j, half, h))
                                if os.environ.get('FAKEDEP'):
                                    S.op("pe", lambda p, h=h, rb=rb: p.matmul(pout.ap[rb:rb + 64, 96 * h:96 * h + 96], lhsT=At.ap[rb:rb + 64, :], rhs=rv.ap[rb:rb + 64, j, 96 * h:96 * h + 96], start=True, stop=False), reads=[At, rv, yr], writes=[pout])
                                else:
                                    S.mm(pout[rb:rb + 64, 96 * h:96 * h + 96], At[rb:rb + 64, :], rv[rb:rb + 64, j, 96 * h:96 * h + 96], True, False)
                                kstop('r%d%d_h%d_s2' % (j, half, h))
                                S.mm(pout[rb:rb + 64, 96 * h:96 * h + 96], qd[base:base + 48, pair, cs0:cs0 + 64], Sbf[base:base + 48, pair, :], False, True)
                                kstop('r%d%d_h%d_s3' % (j, half, h))
                                pu = pbank[(2 * h + 1) % 4]
                                S.mm(pu[base:base + 48, 0:96], kd[rb:rb + 64, j, h, :], rv[rb:rb + 64, j, 96 * h:96 * h + 96], True, True)
                                kstop('r%d%d_h%d_s4' % (j, half, h))
                                S.stt("dve", Sst[base:base + 48, pair, :], Sst[base:base + 48, pair, :], CONST["cdec"][h], pu[base:base + 48, 0:96], ALU.mult, ALU.add)
                                kstop('r%d%d_h%d_s5' % (j, half, h))
                                S.copy("act", Sbf[base:base + 48, pair, :], Sst[base:base + 48, pair, :])
                                kstop('r%d%d_h%d_s6' % (j, half, h))
                            S.copy("dve", yr[rb:rb + 64, :], pout[rb:rb + 64, 0:384])
                            kstop('ret_%d_%d' % (j, half))
                        yr3 = yr.v(lambda a: a.rearrange("p (h e) -> p h e", e=96))
                        S.op("dve", lambda v: v.tensor_reduce(st4.ap[:, 0, :], yr3.ap, AX.X, ALU.add), reads=[yr], writes=[st4])
                        S.tt("pool", ysq, yr, yr, ALU.mult)
                        S.op("dve", lambda v: v.tensor_reduce(st4.ap[:, 1, :], ysq.ap.rearrange("p (h e) -> p h e", e=96), AX.X, ALU.add), reads=[ysq], writes=[st4])
                        S.ts("dve", st4[:, 2, :], st4[:, 0, :], 1.0 / 96, ALU.mult)
                        S.tt("dve", st4[:, 0, :], st4[:, 2, :], st4[:, 2, :], ALU.mult)
                        S.stt("dve", st4[:, 3, :], st4[:, 1, :], 1.0 / 96, st4[:, 0, :], ALU.mult, ALU.subtract)
                        S.actf(st4[:, 3, :], st4[:, 3, :], AF.Sqrt, bias=EPS)
                        S.op("dve", lambda v: v.reciprocal(st4.ap[:, 3, :], st4.ap[:, 3, :]), reads=[st4], writes=[st4])
                        bc = lambda c: st4[:, c, :].v(lambda a: a.unsqueeze(2).to_broadcast([128, 4, 96]))
                        S.tt("dve", yr3, yr3, bc(2), ALU.subtract)
                        S.tt("dve", yr3, yr3, bc(3), ALU.mult)
                        S.tt("pool", yr, yr, pc("retg"), ALU.mult)
                        S.tt("pool", ycb, yr, rgs[:, j, :], ALU.mult)
                        pb = ps()
                        pbv = pb.v(lambda a: a.bitcast(BF16))
                        for c3 in range(3):
                            S.tr(pbv[:, 128 * c3:128 * c3 + 128], ycb[:, 128 * c3:128 * c3 + 128], identb)
                        S.copy("dve", ycT[:, :, j * 128:(j + 1) * 128], pbv[:, 0:384].v(lambda a: a.rearrange("p (c t) -> p c t", t=128)))
                    if debug:
                        debug[0](S, locals(), dbg_out, ti, "ret")

                    for j in range(4 if ti == 0 else int(os.environ.get('KDSAJ', '4'))):
                        qb = ti * 4 + j
                        nkb = qb + 1
                        nk = nkb * 128
                        qs = slice(j * 128, (j + 1) * 128)
                        for k0 in range(0, nk, 512):
                            kw = min(512, nk - k0)
                            tl = [KTr[t] for t in range(k0 // TT, (k0 + kw - 1) // TT + 1)]
                            for h in range(4):
                                pb = ps()
                                S.op("pe", lambda p, pb=pb, h=h, k0=k0, kw=kw: p.matmul(
                                    pb.ap[:, 0:kw], lhsT=iqT.ap[64:96, h, qs], rhs=KT.ap[64:96, k0:k0 + kw], start=True, stop=True),
                                    reads=[iqT] + tl, writes=[pb])
                                if h == 0:
                                    S.actf(big[:, k0:k0 + kw], pb[:, 0:kw], AF.Relu)
                                    S.ts("dve", big[:, k0:k0 + kw], big[:, k0:k0 + kw], iw[:, j, 0:1], ALU.mult)
                                else:
                                    S.actf(relu_t[:, 0:kw], pb[:, 0:kw], AF.Relu)
                                    S.stt("dve" if h % 2 else "pool", big[:, k0:k0 + kw], relu_t[:, 0:kw], iw[:, j, h:h + 1], big[:, k0:k0 + kw], ALU.mult, ALU.add)
                        S.tt("pool", big[:, nk - 128:nk], big[:, nk - 128:nk], pc("diag"), ALU.add)
                        S.memset("dve", mid, BIS_LO + BIS_W / 2)
                        thr_s = float(2 * TOPK - nk)
                        w_i = BIS_W / 2
                        nA = min(nk, 4096)
                        for it in range(NBIS if ti == 0 else int(os.environ.get('KNBIS', NBIS))):
                            S.ts("dve", nmid, mid, -1.0, ALU.mult)
                            S.memset("dve", bis[:, 3:5], 0.0)
                            S.actf(junk[:, 0:nA], big[:, 0:nA], AF.Sign, bias=nmid, accum=ssA)
                            if nk > nA:
                                S.actf(junk[:, 0:nk - nA], big[:, nA:nk], AF.Sign, bias=nmid, accum=ssB)
                                S.tt("dve", ssA, ssA, ssB, ALU.add)
                            S.ts("dve", gcol, ssA, thr_s, ALU.is_ge, w_i, ALU.mult)
                            if it < NBIS - 1:
                                S.stt("dve", mid, gcol, -w_i / 2, mid, ALU.add, ALU.add)
                            else:
                                S.stt("dve", lo, gcol, -w_i, mid, ALU.add, ALU.add)
                            w_i = w_i / 2
                        if debug:
                            debug[0](S, locals(), dbg_out, ti, "bis")
                        for kb in range(nkb if ti == 0 else min(nkb, int(os.environ.get('KNKB', 99)))):
                            ks = slice(kb * 128, (kb + 1) * 128)
                            kreg = KTr[kb // 4]
                            S.ts("dve", m01, big[:, ks], lo, ALU.is_ge)
                            pm = ps()
                            pmv = pm.v(lambda a: a.bitcast(BF16))
                            S.tr(pmv[:, 0:128], m01, identb)
                            S.copy("dve", mT, pmv[:, 0:128])
                            for g in range(2):
                                pp = ps()
                                S.op("pe", lambda p, pp=pp, g=g: p.matmul(
                                    pp.ap[:, 0:384], lhsT=KT.ap[0:64, ks], rhs=qT.ap[0:64, 3 * g:3 * g + 3, qs], start=True, stop=True),
                                    reads=[qT, kreg], writes=[pp])
                                S.actf(pT[:, g, :, :], pp[:, 0:384].v(lambda a: a.rearrange("p (c t) -> p c t", t=128)), AF.Exp, scale=0.125)
                            p6 = pT.v(lambda a: a.rearrange("p g c t -> p (g c) t"))
                            S.tt("pool", p6, p6, mT.v(lambda a: a.unsqueeze(1).to_broadcast([128, 6, 128])), ALU.mult)
                            for g in range(2):
                                PDg = PD0 if g == 0 else PD1
                                S.op("pe", lambda p, g=g, kb=kb, PDg=PDg: p.matmul(
                                    PDg.ap[0:96, 0:384], lhsT=VC.ap[:, kb, :], rhs=pT.ap[:, g, :, :],
                                    start=(kb == 0), stop=(kb == nkb - 1)),
                                    reads=[pT, VCr[kb // 4]], writes=[PDg], acc=(kb != 0))
                        S.copy("dve", ot[0:96, 0:384], PD0[0:96, 0:384])
                        S.copy("dve", ot[0:96, 384:768], PD1[0:96, 0:384])
                        po = [ps(), ps()]
                        for h in range(6):
                            S.tr(po[h // 3][:, 96 * (h % 3):96 * (h % 3) + 96], ot[0:96, 128 * h:128 * h + 128], ident[0:96, 0:96])
                        for g in range(2):
                            S.copy("dve", pvs[:, 3 * g:3 * g + 3, :], po[g][:, 0:288].v(lambda a: a.rearrange("p (c e) -> p c e", e=96))[:, :, 0:65])
                        S.op("dve", lambda v: v.reciprocal(rden.ap, pvs.ap[:, :, 64]), reads=[pvs], writes=[rden])
                        S.tt("dve", ybt.v(lambda a: a.rearrange("p (h e) -> p h e", e=64)), pvs[:, :, 0:64],
                             rden.v(lambda a: a.unsqueeze(2).to_broadcast([128, 6, 64])), ALU.mult)
                        pb = ps()
                        pbv = pb.v(lambda a: a.bitcast(BF16))
                        for c3 in range(3):
                            S.tr(pbv[:, 128 * c3:128 * c3 + 128], ybt[:, 128 * c3:128 * c3 + 128], identb)
                        S.copy("dve", ybT[:, :, qs], pbv[:, 0:384].v(lambda a: a.rearrange("p (c t) -> p c t", t=128)))
                    if debug:
                        debug[0](S, locals(), dbg_out, ti, "dsa")

                    mrg = sq
                    srcs = [(yaT, 0, 2), (ybT, 2, 3), (ycT, 5, 3)]
                    for ft in range(8):
                        wp = wnext("pr%d" % ft)
                        pP = []
                        for bi, (yt, k0, nk_) in enumerate(srcs):
                            pb = ps()
                            for kt in range(nk_):
                                S.mm(pb, wp[:, k0 + kt, :], yt[:, kt, :], kt == 0, kt == nk_ - 1)
                            pP.append(pb)
                        wg = wnext("gt%d" % ft)
                        for bi, gt in enumerate((gA, gB, gC)):
                            pb = fm_mm(wg, 128 * bi, 128, hk)
                            S.actf(gt, pb, AF.Sigmoid)
                        S.tt("dve", gA, gA, pP[0], ALU.mult)
                        S.tt("dve", gB, gB, pP[1], ALU.mult)
                        S.tt("dve", gC, gC, pP[2], ALU.mult)
                        S.tt("pool", gA, gA, gB, ALU.add)
                        S.tt("pool", mrg[:, ft, :], gA, gC, ALU.add)
                    for ft in range(8):
                        wv = wnext("wo%d" % ft)
                        pb = fm_mm(wv, 0, 128, lambda kt: mrg[:, kt, :])
                        S.tt("dve", xT[:, ft, :], xT[:, ft, :], pb, ALU.add)
                    if debug:
                        debug[0](S, locals(), dbg_out, ti, "mix")

                    rmsnorm("g2")
                    for f4 in range(8):
                        wv = wnext("w1_%d" % f4)
                        for q in range(4):
                            f = f4 * 4 + q
                            pb = fm_mm(wv, 128 * q, 128, hk)
                            S.actf(rl, pb, AF.Relu)
                            S.tt("pool" if f % 2 else "dve", aT[:, f, :], rl, rl, ALU.mult)
                    for ft in range(8):
                        wv = wnext("w2_%d" % ft)
                        pb = fm_mm(wv, 0, 128, lambda kt: aT[:, kt, :], nkt=32)
                        S.tt("dve", xT[:, ft, :], xT[:, ft, :], pb, ALU.add)

                    rms_stats()
                    oT = xT
                    tmpo = rope.v(lambda a: a[:, 0:1, :].rearrange("p s t -> p (s t)"))
                    for kt in range(8):
                        S.stt("dve", tmpo, xT[:, kt, :], ogf[:, kt:kt + 1], rstd, ALU.mult, ALU.mult)
                        S.stt("pool", xT[:, kt, :], xT[:, kt, :], omf[:, 0:1], tmpo, ALU.mult, ALU.add)
                    for j in range(4):
                        for kt in range(0, 8, 4):
                            pb = ps()
                            for q in range(4):
                                S.tr(pb[:, q * 128:(q + 1) * 128], oT[:, kt + q, j * 128:(j + 1) * 128], ident)
                            S.copy(evac_eng(), stg[:, j, kt * 128:(kt + 4) * 128], pb)
                    S.dma("sp", T(y_dst[t0:t0 + TT, :].rearrange("(j p) f -> p j f", p=128), xmr[ti] if l < nlayers - 1 else None), stg)
              except _Stop:
                break
        S.finish()
        print("instructions:", S.nins, "sbuf left:", nc.sbuf_bytes_remaining)
    return nc


_PROG = {}


def _layer_arrays(inp, l, flag):
    ws = {"wa": _arrange_w_in(np.asarray(inp["w_in"][l], np.float32)),
          "wglu": np.asarray(inp["ssm_glu_w"][l], np.float32),
          "wpr": np.concatenate([inp["w_proj_a"][l], inp["w_proj_b"][l], inp["w_proj_c"][l]], axis=0).astype(np.float32),
          "wo": np.asarray(inp["w_out"][l], np.float32),
          "w1": np.asarray(inp["w_ff1"][l], np.float32),
          "w2": np.asarray(inp["w_ff2"][l], np.float32)}
    return _flat_weights(ws), _pack_params(inp, l, flag)


def _maps(inp, layers, xs, ntok, last_is_final=True):
    wfs, Ps = [], []
    for i, l in enumerate(layers):
        flag = 1.0 if (last_is_final and i == len(layers) - 1) else 0.0
        wf, P = _layer_arrays(inp, l, flag)
        wfs.append(wf)
        Ps.append(P)
    wf = np.concatenate(wfs, axis=0)
    P = np.concatenate(Ps, axis=0)
    return [{"x_in": np.ascontiguousarray(xs[b][:ntok], dtype=np.float32),
             "pos": np.ascontiguousarray(inp["positions"][b:b + 1, :ntok], dtype=np.int32),
             "P": P, "wf": wf} for b in range(len(xs))]


def kernel(**inputs):
    inp = {k: np.asarray(v) for k, v in inputs.items()}
    ntiles = SEQ // TT
    key = (ntiles, DEPTH)
    if key not in _PROG:
        _PROG[key] = build_program(ntiles, DEPTH)
    nc = _PROG[key]
    xs = [inp["x"][b] for b in range(BATCH)]
    maps = _maps(inp, list(range(DEPTH)), xs, SEQ)
    res = run_bass_kernel_spmd(nc, maps, core_ids=list(range(BATCH)))
    return np.stack([np.asarray(r["y"]) for r in res.results], axis=0).astype(np.float32)
```

```python
import math
import os
from contextlib import ExitStack
import numpy as np
import concourse.bass as bass
import concourse.mybir as mybir
from concourse.bass_utils import run_bass_kernel_spmd

F32 = mybir.dt.float32
BF16 = mybir.dt.bfloat16
I32 = mybir.dt.int32
ALU = mybir.AluOpType
AF = mybir.ActivationFunctionType
AX = mybir.AxisListType

D = 1024
SEQ = 8192
BATCH = 4
DEPTH = 2
TT = 512
EPS = 1e-6
BIG = 30000.0
TOPK = 256
NBIS = 22
BIS_LO, BIS_W = -16.0, 32.0


class Reg:
    __slots__ = ("w", "r")

    def __init__(self):
        self.w = None
        self.r = {}


class T:
    __slots__ = ("ap", "reg")

    def __init__(self, ap, reg=None):
        self.ap = ap
        self.reg = reg if reg is not None else Reg()

    def __getitem__(self, idx):
        return T(self.ap[idx], self.reg)

    def v(self, fn):
        return T(fn(self.ap), self.reg)

    def wr(self, reg):
        return T(self.ap, reg)


def _regs(xs):
    out = []
    for x in xs:
        if isinstance(x, T):
            x = x.reg
        if isinstance(x, Reg):
            out.append(x)
        elif isinstance(x, (tuple, list)):
            out.extend(x)
    return out


def _ap(x):
    return x.ap if isinstance(x, T) else x


class Sched:
    def __init__(self, nc, stack):
        self.nc = nc
        self.eng = {"pe": nc.tensor, "act": nc.scalar, "dve": nc.vector, "pool": nc.gpsimd, "sp": nc.sync}
        self.sem, self.cnt, self.seen = {}, {}, {}
        for e in ["pe", "act", "dve", "pool"]:
            self.sem[e] = stack.enter_context(nc.semaphore("s_" + e))
        self.NDS = 4
        self.dcnt = {}
        for q in ["sp", "pool"]:
            self.dcnt[q] = 0
            for i in range(self.NDS):
                self.sem["dma_%s%d" % (q, i)] = stack.enter_context(nc.semaphore("s_dma_%s%d" % (q, i)))
        for k in self.sem:
            self.cnt[k] = 0
        self.names = list(self.sem.keys())
        for e in ["pe", "act", "dve", "pool", "sp"]:
            self.seen[e] = {k: 0 for k in self.names}
        self.nins = 0
        self.psum_regs = set()
        self.act_dummy = None
        self.fence = None

    def _waits(self, e, reads, writes, acc):
        deps = {}

        def add(d):
            if d is not None and deps.get(d[0], 0) < d[1]:
                deps[d[0]] = d[1]
        for r in reads:
            add(r.w)
        for w in writes:
            if not acc:
                add(w.w)
            for k, t in w.r.items():
                add((k, t))
        eh, seen = self.eng[e], self.seen[e]
        if e == "pe" and self.fence is not None and seen["act"] < deps.get("act", 0):
            ta = deps.pop("act")
            pseen = self.seen["pool"]
            if pseen["act"] < ta:
                self.eng["pool"].wait_ge(self.sem["act"], ta)
                pseen["act"] = ta
            self.eng["pool"].memset(self.fence, 0.0).then_inc(self.sem["pool"], 1)
            self.cnt["pool"] += 1
            self.nins += 1
            seen["act"] = ta
            if deps.get("pool", 0) < self.cnt["pool"]:
                deps["pool"] = self.cnt["pool"]
        for k, t in deps.items():
            if seen[k] < t:
                eh.wait_ge(self.sem[k], t)
                seen[k] = t

    def op(self, e, fn, reads=(), writes=(), acc=False):
        reads, writes = _regs(reads), _regs(writes)
        self._waits(e, reads, writes, acc)
        ins = fn(self.eng[e])
        self.cnt[e] += 1
        t = self.cnt[e]
        ins.then_inc(self.sem[e], 1)
        self.nins += 1
        if e == "act" and self.act_dummy is not None and any(id(r) in self.psum_regs for r in reads):
            d0, d1 = self.act_dummy
            self.eng["act"].copy(d0, d1).then_inc(self.sem[e], 1)
            self.cnt[e] += 1
            t = self.cnt[e]
            self.nins += 1
        for r in reads:
            if r.r.get(e, 0) < t:
                r.r[e] = t
        for w in writes:
            w.w = (e, t)
            if not acc:
                w.r = {}
        return ins

    def dma(self, q, out, in_, **kw):
        reads, writes = _regs([in_]), _regs([out])
        self._waits(q, reads, writes, False)
        k = "dma_%s%d" % (q, self.dcnt[q] % self.NDS)
        self.dcnt[q] += 1
        if self.seen[q][k] < self.cnt[k]:
            self.eng[q].wait_ge(self.sem[k], self.cnt[k])
            self.seen[q][k] = self.cnt[k]
        ins = self.eng[q].dma_start(out=_ap(out), in_=_ap(in_), **kw)
        self.cnt[k] += 16
        t = self.cnt[k]
        ins.then_inc(self.sem[k], 16)
        self.nins += 1
        for r in reads:
            if r.r.get(k, 0) < t:
                r.r[k] = t
        for w in writes:
            w.w = (k, t)
            w.r = {}
        return ins

    def finish(self):
        for e in ["sp", "act", "pool", "dve", "pe"]:
            for k in self.names:
                if self.cnt[k] > self.seen[e][k]:
                    self.eng[e].wait_ge(self.sem[k], self.cnt[k])
                    self.seen[e][k] = self.cnt[k]

    def mm(self, out, lhsT, rhs, start, stop):
        return self.op("pe", lambda p: p.matmul(out.ap, lhsT=lhsT.ap, rhs=rhs.ap, start=start, stop=stop),
                       reads=[lhsT, rhs], writes=[out], acc=not start)

    def tr(self, out, in_, ident):
        return self.op("pe", lambda p: p.transpose(out.ap, in_.ap, ident.ap), reads=[in_, ident], writes=[out])

    def actf(self, out, in_, func, bias=None, scale=1.0, accum=None, eng="act"):
        kw = {}
        if bias is not None:
            kw["bias"] = _ap(bias)
        if accum is not None:
            kw["accum_out"] = accum.ap
        wr = [out] + ([accum] if accum is not None else [])
        return self.op(eng, lambda a: a.activation(out.ap, in_.ap, func, scale=_ap(scale), **kw),
                       reads=[in_, bias, scale], writes=wr)

    def tt(self, eng, out, a, b, op):
        return self.op(eng, lambda v: v.tensor_tensor(out.ap, a.ap, b.ap, op), reads=[a, b], writes=[out])

    def ts(self, eng, out, a, s1, op0, s2=None, op1=None, accum=None):
        kw = {}
        if op1 is not None:
            kw["op1"] = op1
        if accum is not None:
            kw["accum_out"] = accum.ap
        wr = [out] + ([accum] if accum is not None else [])
        return self.op(eng, lambda v: v.tensor_scalar(out.ap, a.ap, _ap(s1), _ap(s2) if s2 is not None else None, op0, **kw),
                       reads=[a, s1, s2], writes=wr)

    def stt(self, eng, out, a, s, b, op0, op1):
        eng = "dve"
        return self.op(eng, lambda v: v.scalar_tensor_tensor(out.ap, a.ap, _ap(s), b.ap, op0, op1),
                       reads=[a, s, b], writes=[out])

    def copy(self, eng, out, in_):
        if eng == "act":
            return self.op("act", lambda a: a.copy(out.ap, in_.ap), reads=[in_], writes=[out])
        return self.op(eng, lambda v: v.tensor_copy(out.ap, in_.ap), reads=[in_], writes=[out])

    def memset(self, eng, out, val):
        return self.op(eng, lambda v: v.memset(out.ap, val), writes=[out])


IN_SIZES = (256, 384, 64, 64, 128, 32, 4, 192, 192, 384, 384, 3072)
IN_OFF = np.concatenate([[0], np.cumsum(IN_SIZES)]).astype(int)
(O_U, O_DQ, O_DK, O_DV, O_IQ, O_IK, O_IW, O_RQ, O_RK, O_RV, O_RG, O_GT) = [int(v) for v in IN_OFF[:12]]


def _swap_cols(base, hd, rot):
    half = rot // 2
    idx = list(range(hd))
    for i in range(half):
        idx[i], idx[i + half] = i + half, i
    return [base + i for i in idx]


def _wa_columns():
    fm = list(range(O_U, O_U + 256))
    for h in range(6):
        fm += list(range(O_DQ + 64 * h, O_DQ + 64 * h + 64)) + _swap_cols(O_DQ + 64 * h, 64, 16)
    fm += list(range(O_DK, O_DK + 64)) + _swap_cols(O_DK, 64, 16)
    for h in range(4):
        fm += list(range(O_IQ + 32 * h, O_IQ + 32 * h + 32)) + _swap_cols(O_IQ + 32 * h, 32, 8)
    fm += list(range(O_IK, O_IK + 32)) + _swap_cols(O_IK, 32, 8)
    for off in (O_RQ, O_RK):
        for i in range(2):
            z, s = [], []
            for h in (2 * i, 2 * i + 1):
                z += list(range(off + 48 * h, off + 48 * h + 48)) + [-1] * 16
                s += _swap_cols(off + 48 * h, 48, 48) + [-1] * 16
            fm += z + s
    tm = list(range(O_DV, O_DV + 64)) + list(range(O_IW, O_IW + 4))
    tm += list(range(O_RV, O_RV + 384)) + list(range(O_RG, O_RG + 384))
    gt = list(range(O_GT, O_GT + 3072))
    return fm + tm + gt


WA_COLS = _wa_columns()
NA = len(WA_COLS)
A_U = 0
A_DQ = 256
A_DK = A_DQ + 768
A_IQ = A_DK + 128
A_IK = A_IQ + 256
A_RQ = A_IK + 64
A_RK = A_RQ + 512
A_VW = A_RK + 512
A_RV = A_VW + 68
A_RG = A_RV + 384
A_GT = A_RG + 384
assert A_GT + 3072 == NA


def _plan():
    r = lambda a, n: list(range(a, a + n))
    pl = [("u", "wa", 8, r(A_U, 256))]
    for i in range(3):
        pl.append(("dq%d" % i, "wa", 8, r(A_DQ + 256 * i, 256)))
    pl.append(("dkik", "wa", 8, r(A_DK, 128) + r(A_IK, 64)))
    pl.append(("iq", "wa", 8, r(A_IQ, 256)))
    for i in range(2):
        pl.append(("rq%d" % i, "wa", 8, r(A_RQ + 256 * i, 256)))
    for i in range(2):
        pl.append(("rk%d" % i, "wa", 8, r(A_RK + 256 * i, 256)))
    pl.append(("vw", "wa", 8, r(A_VW, 68)))
    pl.append(("rv", "wa", 8, r(A_RV, 384)))
    pl.append(("rg", "wa", 8, r(A_RG, 384)))
    pl.append(("glu", "wglu", 2, r(0, 256)))
    for ft in range(8):
        pl.append(("pr%d" % ft, "wpr", 8, r(128 * ft, 128)))
        pl.append(("gt%d" % ft, "wa", 8, r(A_GT + 128 * ft, 128) + r(A_GT + 1024 + 128 * ft, 128) + r(A_GT + 2048 + 128 * ft, 128)))
    for ft in range(8):
        pl.append(("wo%d" % ft, "wo", 8, r(128 * ft, 128)))
    for f4 in range(8):
        pl.append(("w1_%d" % f4, "w1", 8, r(512 * f4, 512)))
    for ft in range(8):
        pl.append(("w2_%d" % ft, "w2", 32, r(128 * ft, 128)))
    return pl


PLAN = _plan()
PLAN_OFF = []
_o = 0
for _nm, _s, _k, _c in PLAN:
    PLAN_OFF.append(_o)
    _o += _k * len(_c)
NW = _o


def _flat_weights(ws):
    out = np.empty((128, NW), np.float32)
    for (nm, s, nkt, cols), o in zip(PLAN, PLAN_OFF):
        w = ws[s][:, cols]
        n = len(cols)
        out[:, o:o + nkt * n] = w.reshape(nkt, 128, n).transpose(1, 0, 2).reshape(128, nkt * n)
    return out


_pc = {}
_off = 0
for _n, _w in [("g1", 8), ("g2", 8), ("gf", 8), ("glub", 2), ("ssd", 2), ("lre", 8), ("lim", 8), ("lst", 8),
               ("bre", 128), ("bim", 128), ("cre", 128), ("cim", 128), ("flag", 1), ("retg", 384),
               ("invD", 1), ("sgnD", 1), ("invI", 1), ("sgnI", 1), ("invR", 1), ("sgnR", 1),
               ("ramp", 256), ("intra", 256), ("qdec", 128), ("kdec", 4), ("diag", 128)]:
    _pc[_n] = (_off, _w)
    _off += _w
NP_ = _off


def _const_tables():
    c = {}
    p = np.arange(128)
    d = p % 64
    inv = np.where(d < 16, np.exp(-math.log(500000.0) * (d % 8) * (2.0 / 16)), 0.0)
    c["invD"] = inv[:, None]
    c["sgnD"] = np.where(d < 8, -1.0, np.where(d < 16, 1.0, 0.0))[:, None]
    d = p % 32
    inv = np.where(d < 8, np.exp(-math.log(500000.0) * (d % 4) * (2.0 / 8)), 0.0)
    c["invI"] = inv[:, None]
    c["sgnI"] = np.where(d < 4, -1.0, np.where(d < 8, 1.0, 0.0))[:, None]
    d = p % 64
    inv = np.where(d < 48, np.exp(-math.log(10000.0) * (d % 24) * (2.0 / 48)), 0.0)
    c["invR"] = inv[:, None]
    c["sgnR"] = np.where(d < 24, -1.0, np.where(d < 48, 1.0, 0.0))[:, None]
    c["ramp"] = np.broadcast_to(np.arange(1, 257, dtype=np.float64)[None, :], (128, 256))
    log_g = np.log1p(-np.exp2(-5.0 - np.arange(4)))
    sc = 48 ** -0.5
    m = (p % 64)[:, None]
    cc = np.arange(64)[None, :]
    intra = np.zeros((128, 4, 64))
    qdec = np.zeros((128, 2, 64))
    kdec = np.zeros((128, 4))
    for h in range(4):
        intra[:, h, :] = np.where(cc >= m, np.exp(log_g[h] * np.maximum(cc - m, 0)), 0.0) * sc
        kdec[:, h] = np.exp(log_g[h] * (63.0 - (p % 64)))
    for pair in range(2):
        for half in range(2):
            h = 2 * pair + half
            qdec[64 * half:64 * half + 64, pair, :] = (np.exp(log_g[h] * (np.arange(64) + 1.0)) * sc)[None, :]
    c["intra"] = intra.reshape(128, 256)
    c["qdec"] = qdec.reshape(128, 128)
    c["kdec"] = kdec
    q = np.arange(128)[:, None]
    k = np.arange(128)[None, :]
    c["diag"] = np.where(k < (q // 64 + 1) * 64, 0.0, -BIG)
    c["cdec"] = [float(np.exp(log_g[h] * 64.0)) for h in range(4)]
    return c


CONST = _const_tables()


def _pack_params(inp, l, flag):
    P = np.zeros((128, NP_), np.float32)

    def put(name, arr):
        o, w = _pc[name]
        P[:, o:o + w] = np.asarray(arr, np.float32).reshape(128, w)
    put("g1", inp["norm1_g"][l].reshape(8, 128).T)
    put("g2", inp["norm2_g"][l].reshape(8, 128).T)
    put("gf", inp["final_norm_g"].reshape(8, 128).T)
    put("glub", inp["ssm_glu_b"][l].reshape(2, 128).T)
    put("ssd", inp["ssm_d"][l].reshape(2, 128).T)
    put("lre", inp["ssm_lambda_re"][l].reshape(8, 128).T)
    put("lim", inp["ssm_lambda_im"][l].reshape(8, 128).T)
    put("lst", np.repeat(inp["ssm_log_step"][l], 64).reshape(8, 128).T)
    put("bre", inp["ssm_b_re"][l].reshape(8, 128, 16).transpose(1, 0, 2))
    put("bim", inp["ssm_b_im"][l].reshape(8, 128, 16).transpose(1, 0, 2))
    put("cre", inp["ssm_c_re"][l].transpose(0, 2, 1).reshape(8, 128, 16).transpose(1, 0, 2))
    put("cim", inp["ssm_c_im"][l].transpose(0, 2, 1).reshape(8, 128, 16).transpose(1, 0, 2))
    put("flag", np.full((128, 1), flag))
    put("retg", np.broadcast_to(inp["ret_norm_g"][l][None, :], (128, 384)))
    for n in ["invD", "sgnD", "invI", "sgnI", "invR", "sgnR", "ramp", "intra", "qdec", "kdec", "diag"]:
        put(n, CONST[n])
    return P


def _arrange_w_in(w):
    wz = np.concatenate([w, np.zeros((w.shape[0], 1), w.dtype)], axis=1)
    return wz[:, WA_COLS]


def build_program(ntiles, nlayers=1, debug=None):
    nc = bass.Bass("TRN2", target_bir_lowering=False)
    NTOK = ntiles * TT
    dr = lambda n, s, dt, kind: nc.dram_tensor(n, s, dt, kind=kind).ap()
    x_in = dr("x_in", [NTOK, D], F32, "ExternalInput")
    pos_in = dr("pos", [1, NTOK], I32, "ExternalInput")
    P_in = dr("P", [nlayers * 128, NP_], F32, "ExternalInput")
    wf_in = dr("wf", [nlayers * 128, NW], F32, "ExternalInput")
    y_out = dr("y", [NTOK, D], F32, "ExternalOutput")
    dbg_out = None
    if debug:
        dbg_out = dr("dbg", list(debug[1]), F32, "ExternalOutput")
    wf_b = dr("wf_b", [nlayers * 128, NW], BF16, "Internal")
    xmid = dr("xmid", [NTOK, D], F32, "Internal") if nlayers > 1 else None
    xmr = [Reg() for _ in range(ntiles)]

    with ExitStack() as st:
        S = Sched(nc, st)

        _sbc = {}

        def sb(name, shape, dt=F32):
            if name not in _sbc:
                _sbc[name] = T(st.enter_context(nc.sbuf_tensor(name, shape, dt))[:])
            return _sbc[name]

        print("sbuf at start:", nc.sbuf_bytes_remaining)
        CH = 2048
        wchunks = [[] for _ in range(nlayers)]
        for l_ in range(nlayers):
            for c0 in range(0, NW, CH):
                c1 = min(NW, c0 + CH)
                rg = Reg()
                S.dma("pool", T(wf_b[l_ * 128:(l_ + 1) * 128, c0:c1], rg), T(wf_in[l_ * 128:(l_ + 1) * 128, c0:c1]))
                wchunks[l_].append((c0, c1, rg))
        cur = [0]

        Pt = sb("Pt", [128, NP_])

        def pc(name, a=None, b=None):
            o, w = _pc[name]
            a = 0 if a is None else a
            b = w if b is None else b
            return Pt[:, o + a:o + b]

        ident = sb("ident", [128, 128])
        S.memset("pool", ident, 0.0)
        S.op("pool", lambda g: g.affine_select(ident.ap, ident.ap, pattern=[[-1, 128]], compare_op=ALU.not_equal,
                                               fill=1.0, base=0, channel_multiplier=1), reads=[ident], writes=[ident])
        identb = sb("identb", [128, 128], BF16)
        S.copy("dve", identb, ident)
        onesb = sb("onesb", [128, 128], BF16)
        S.memset("pool", onesb, 1.0)

        xT = sb("xT", [128, 8, TT])
        hT = sb("hT", [128, 8, TT], BF16)
        sq = sb("sq", [128, 8, TT], BF16)
        rstd = sb("rstd", [128, TT])
        big = sb("big", [128, 8192])
        aT = big.v(lambda a: a.bitcast(BF16).rearrange("p (k t) -> p k t", t=TT)[:, 0:32, :])
        stg = big.v(lambda a: a[:, 0:4 * D].rearrange("p (j f) -> p j f", f=D))
        KT = sb("KT", [128, NTOK], BF16)
        VC = sb("VC", [128, NTOK // 128, 96], BF16)
        KTr = [Reg() for _ in range(ntiles)]
        VCr = [Reg() for _ in range(ntiles)]
        S.memset("pool", VC, 1.0)
        for r in VCr:
            r.w = VC.reg.w
        uT = sb("uT", [128, 2, TT])
        qT = sb("qT", [64, 6, TT], BF16)
        iqT = sb("iqT", [128, 4, TT], BF16)
        iw = sb("iw", [128, 4, 4])
        rqT = sb("rqT", [128, 2, TT], BF16)
        rkT = sb("rkT", [128, 2, TT], BF16)
        rv = sb("rv", [128, 4, 384], BF16)
        rgs = sb("rgs", [128, 4, 384], BF16)
        yaT = sb("yaT", [128, 2, TT], BF16)
        ybT = sb("ybT", [128, 3, TT], BF16)
        ycT = sb("ycT", [128, 3, TT], BF16)
        NSL = 11
        U = st.enter_context(nc.sbuf_tensor("U", [128, NSL * 512], F32))[:]
        Ureg = [Reg() for _ in range(NSL)]

        def carve(s0, ns, shape_fn=None, dt=F32):
            ap = U[:, s0 * 512:(s0 + ns) * 512]
            if dt == BF16:
                ap = ap.bitcast(BF16)
            elif dt == I32:
                ap = ap.bitcast(I32)
            if shape_fn is not None:
                ap = shape_fn(ap)
            return T(ap, tuple(Ureg[s0:s0 + ns]))
        posi = carve(0, 1, None, I32)
        posf = carve(1, 1)
        ang = carve(2, 1)
        rope = carve(3, 6, lambda a: a.rearrange("p (s t) -> p s t", t=TT))
        ropa = carve(9, 1)
        ropb = carve(10, 1)
        TC = 256
        r2 = lambda a: a.rearrange("p (r t) -> p r t", t=TC)
        w12 = carve(0, 1, r2)
        w43 = carve(1, 1, r2)
        vv = carve(2, 1, r2)
        xs_ = carve(3, 1, r2)
        xo = carve(4, 1, r2)
        ypre = carve(5, 2, lambda a: a.rearrange("p (r t) -> p r t", t=TT))
        ypb = carve(7, 1, lambda a: a.rearrange("p (r t) -> p r t", t=TT), BF16)
        gsig = carve(8, 1)
        qd = carve(0, 1, lambda a: a.rearrange("p (r t) -> p r t", t=TT), BF16)
        kd = carve(1, 1, lambda a: a[:, 0:768].rearrange("p (j h d) -> p j h d", j=4, h=4), BF16)
        yr = carve(2, 1, lambda a: a[:, 0:384])
        ysq = carve(3, 1, lambda a: a[:, 0:384])
        ycb = carve(4, 1, lambda a: a[:, 0:384], BF16)
        At = carve(5, 1, lambda a: a[:, 0:64], BF16)
        st4 = carve(6, 1, lambda a: a[:, 0:16].rearrange("p (a b) -> p a b", b=4))
        relu_t = carve(0, 1)
        pT = carve(1, 1, lambda a: a[:, 0:768].rearrange("p (g c t) -> p g c t", g=2, c=3), BF16)
        pvs = carve(2, 1, lambda a: a[:, 0:390].rearrange("p (h e) -> p h e", e=65))
        ybt = carve(3, 1, lambda a: a[:, 0:384], BF16)
        m01 = carve(4, 1, lambda a: a[:, 0:128], BF16)
        mT = carve(5, 1, lambda a: a[:, 0:128], BF16)
        bis = carve(6, 1, lambda a: a[:, 0:8])
        lo, mid, nmid, ssA, ssB, gcol, rden6 = bis[:, 0:1], bis[:, 1:2], bis[:, 2:3], bis[:, 3:4], bis[:, 4:5], bis[:, 5:6], None
        rden = carve(7, 1, lambda a: a[:, 0:6])
        ot = carve(8, 2, lambda a: a[:, 0:768])
        junk = sq.v(lambda a: a.rearrange("p k t -> p (k t)"))
        pT2 = sb("pT2", [128, 768], BF16).v(lambda a: a.rearrange("p (g c t) -> p g c t", g=2, c=3))
        mT2 = sb("mT2", [128, 128], BF16)
        pTs, mTs = [pT, pT2], [mT, mT2]
        bsx = st.enter_context(nc.sbuf_tensor("bsx", [128, 8], F32))[:] if "bsx" not in _sbc else None
        if bsx is not None:
            _sbc["bsx"] = bsx
        bsx = _sbc["bsx"]
        nmid, nlo, gcol = T(bsx[:, 0:1]), T(bsx[:, 1:2]), T(bsx[:, 2:3])
        accs = [T(bsx[:, 3:5]), T(bsx[:, 5:7])]
        gA = carve(0, 1)
        gB = carve(1, 1)
        gC = carve(2, 1)
        rl = carve(3, 1, None, BF16)[:, 0:TT]
        NWB = 2
        WBW = 4096
        wbuf = [sb("wbuf%d" % i, [128, WBW], BF16) for i in range(NWB)]
        wctr = [0]
        wpos = [0]

        def wnext(name):
            nm, s_, nkt, cols = PLAN[wpos[0]]
            off = PLAN_OFF[wpos[0]]
            assert nm == name, (nm, name)
            wpos[0] += 1
            n = len(cols)
            b = wbuf[wctr[0] % NWB]
            wctr[0] += 1
            v = b.v(lambda a: a[:, 0:nkt * n])
            regs = tuple(rg for (c0, c1, rg) in wchunks[cur[0]] if c0 < off + nkt * n and c1 > off)
            S.dma("sp", v, T(wf_b[cur[0] * 128:(cur[0] + 1) * 128, off:off + nkt * n], regs))
            return v.v(lambda a: a.rearrange("p (k n) -> p k n", n=n))

        pbank = [T(st.enter_context(nc.psum_tensor("ps%d" % i, [128, 512], F32))[:]) for i in range(8)]
        prot = [0]
        for b_ in pbank:
            S.psum_regs.add(id(b_.reg))
        if os.environ.get('FENCE'):
            fnc = st.enter_context(nc.sbuf_tensor("fnc", [128, 4], F32))[:]
            S.fence = fnc[0:1, 0:1]
        if os.environ.get('DUMMY'):
            dmy = st.enter_context(nc.sbuf_tensor("dmy", [128, 4], F32))[:]
            S.memset("pool", T(dmy), 0.0)
            S.act_dummy = (dmy[:, 0:1], dmy[:, 2:3])

        def ps():
            b = pbank[prot[0] % 6]
            prot[0] += 1
            return b
        PD0, PD1 = pbank[6], pbank[7]

        TWO_PI = 2.0 * math.pi

        C1 = 6.28125
        C2 = 4058.0 / 2 ** 21
        C3 = TWO_PI - C1 - C2
        PI_LO = 3.1415925

        def sincos(out_sin, out_cos, angle, tmp_t, ki_t, kf_t):
            S.ts("dve", tmp_t, angle, 1.0 / TWO_PI, ALU.mult)
            _ce = "dve" if os.environ.get('CVT_DVE') else "pool"
            S.copy(_ce, ki_t, tmp_t)
            S.copy(_ce, kf_t, ki_t)
            S.stt("dve", tmp_t, kf_t, -C1, angle, ALU.mult, ALU.add)
            S.stt("dve", tmp_t, kf_t, -C2, tmp_t, ALU.mult, ALU.add)
            S.stt("dve", tmp_t, kf_t, -C3, tmp_t, ALU.mult, ALU.add)
            S.ts("dve", tmp_t, tmp_t, -PI_LO, ALU.max, PI_LO, ALU.min)
            S.actf(out_sin, tmp_t, AF.Sin)
            S.ts("dve", kf_t, tmp_t, 0.5 * math.pi, ALU.is_gt, -TWO_PI, ALU.mult)
            S.stt("dve", tmp_t, tmp_t, 0.5 * math.pi, kf_t, ALU.add, ALU.add)
            S.ts("dve", tmp_t, tmp_t, -PI_LO, ALU.max, PI_LO, ALU.min)
            S.actf(out_cos, tmp_t, AF.Sin)

        for l in range(nlayers):
            cur[0] = l
            S.dma("sp", Pt, T(P_in[l * 128:(l + 1) * 128, :]))
            x_src = x_in if l == 0 else xmid
            y_dst = y_out if l == nlayers - 1 else xmid
            s5 = sb("s5p", [128, 12, 8])
            lre, lim, lst = pc("lre"), pc("lim"), pc("lst")
            stp, are, th, rr, ct_, st_, lbr, lbi, den, fre, fim, tmp = [s5[:, i, :] for i in range(12)]
            S.actf(stp, lst, AF.Exp)
            S.tt("dve", are, lre, stp, ALU.mult)
            S.tt("dve", th, lim, stp, ALU.mult)
            S.actf(rr, are, AF.Exp)
            s5i = sb("s5i", [128, 8], I32)
            s5f = sb("s5f", [128, 8])
            sincos(st_, ct_, th, tmp, s5i, s5f)
            S.tt("dve", lbr, rr, ct_, ALU.mult)
            S.tt("dve", lbi, rr, st_, ALU.mult)
            S.tt("dve", den, lre, lre, ALU.mult)
            S.tt("dve", tmp, lim, lim, ALU.mult)
            S.tt("dve", den, den, tmp, ALU.add)
            S.op("dve", lambda v: v.reciprocal(den.ap, den.ap), reads=[den], writes=[den])
            S.ts("dve", lbr, lbr, -1.0, ALU.add)
            S.tt("dve", fre, lbr, lre, ALU.mult)
            S.tt("dve", tmp, lbi, lim, ALU.mult)
            S.tt("dve", fre, fre, tmp, ALU.add)
            S.tt("dve", fre, fre, den, ALU.mult)
            S.tt("dve", fim, lbi, lre, ALU.mult)
            S.tt("dve", tmp, lbr, lim, ALU.mult)
            S.tt("dve", fim, fim, tmp, ALU.subtract)
            S.tt("dve", fim, fim, den, ALU.mult)
            bbr = carve(4, 1, lambda a: a[:, 0:128].rearrange("p (k c) -> p k c", c=16))
            bbi = carve(5, 1, lambda a: a[:, 0:128].rearrange("p (k c) -> p k c", c=16))
            tb = carve(6, 1, lambda a: a[:, 0:16])
            k16 = lambda a: a.rearrange("p (k c) -> p k c", c=16)
            bre, bim, cre, cim = pc("bre").v(k16), pc("bim").v(k16), pc("cre").v(k16), pc("cim").v(k16)
            BT = sb("BT", [128, 8, 2, 128])
            CT = sb("CT", [128, 8, 2, 128])
            S.memset("pool", CT, 0.0)
            pad = carve(7, 1, lambda a: a[:, 0:256].rearrange("p (r c) -> p r c", c=128))
            for k in range(8):
                S.ts("dve", bbr[:, k, :], bre[:, k, :], fre[:, k:k + 1], ALU.mult)
                S.ts("dve", tb, bim[:, k, :], fim[:, k:k + 1], ALU.mult)
                S.tt("dve", bbr[:, k, :], bbr[:, k, :], tb, ALU.subtract)
                S.ts("dve", bbi[:, k, :], bim[:, k, :], fre[:, k:k + 1], ALU.mult)
                S.ts("dve", tb, bre[:, k, :], fim[:, k:k + 1], ALU.mult)
                S.tt("dve", bbi[:, k, :], bbi[:, k, :], tb, ALU.add)
                c0 = 32 * (k % 4)
                S.memset("pool", pad, 0.0)
                for ri, src in enumerate((bbr, bbi)):
                    S.copy("pool", pad[0:64, ri, c0:c0 + 16], src[0:64, k, :])
                    S.copy("pool", pad[64:128, ri, c0 + 16:c0 + 32], src[64:128, k, :])
                for ri in range(2):
                    pb = ps()
                    S.tr(pb[:, 0:128], pad[:, ri, :], ident)
                    S.copy("dve", BT[:, k, ri, :], pb[:, 0:128])
                S.copy("pool", CT[0:64, k, 0, c0:c0 + 16], cre[0:64, k, :])
                S.copy("pool", CT[64:128, k, 0, c0 + 16:c0 + 32], cre[64:128, k, :])
                S.ts("dve", CT[0:64, k, 1, c0:c0 + 16], cim[0:64, k, :], -1.0, ALU.mult)
                S.ts("dve", CT[64:128, k, 1, c0 + 16:c0 + 32], cim[64:128, k, :], -1.0, ALU.mult)
            CS = sb("CS", [128, 8, 2, TC])
            for k in range(8):
                S.ts("dve", ropa[:, 0:TC], pc("ramp"), th[:, k:k + 1], ALU.mult)
                sincos(CS[:, k, 1, :], CS[:, k, 0, :], ropa[:, 0:TC], ropb[:, 0:TC], posi[:, 0:TC], posf[:, 0:TC])
            xprev = sb("xprev", [128, 8, 2])
            S.memset("pool", xprev, 0.0)
            Sst = sb("Sst", [128, 2, 96])
            Sbf = sb("Sbf", [128, 2, 96], BF16)
            S.memset("pool", Sst, 0.0)
            S.memset("pool", Sbf, 0.0)
            ogf = sb("ogf", [128, 8])
            omf = sb("omf", [128, 1])
            S.ts("dve", ogf, pc("gf"), pc("flag"), ALU.mult)
            S.ts("dve", omf, pc("flag"), -1.0, ALU.mult, 1.0, ALU.add)
            print("sbuf left after alloc:", nc.sbuf_bytes_remaining)

            def rms_stats():
                S.actf(sq, xT, AF.Square)
                pb = ps()
                for kt in range(8):
                    S.mm(pb, onesb, sq[:, kt, :], kt == 0, kt == 7)
                S.actf(rstd, pb, AF.Sqrt, bias=EPS, scale=1.0 / D)
                S.op("dve", lambda v: v.reciprocal(rstd.ap, rstd.ap), reads=[rstd], writes=[rstd])

            def rmsnorm(gname):
                rms_stats()
                for kt in range(8):
                    S.stt("dve" if kt % 2 == 0 else "pool", hT[:, kt, :], xT[:, kt, :], pc(gname, kt, kt + 1), rstd, ALU.mult, ALU.mult)

            evac_flip = [0]

            def evac_eng():
                evac_flip[0] += 1
                return "dve"

            hk = lambda kt: hT[:, kt, :]

            def fm_mm(wv, col0, m, rhs_of_kt, nkt=8, po=0):
                pb = ps()
                for kt in range(nkt):
                    S.mm(pb[po:po + m, :], wv[:, kt, col0:col0 + m], rhs_of_kt(kt), kt == 0, kt == nkt - 1)
                return pb

            def roped(wv, zc, sc, m, ty, out_t, po=0):
                pz = fm_mm(wv, zc, m, hk, po=po)
                pz2 = fm_mm(wv, sc, m, hk, po=po)
                S.tt("dve", ropa[po:po + m, :], pz[po:po + m, :], rope[po:po + m, 2 * ty, :], ALU.mult)
                S.tt("dve", ropb[po:po + m, :], pz2[po:po + m, :], rope[po:po + m, 2 * ty + 1, :], ALU.mult)
                S.tt("pool", out_t, ropa[po:po + m, :], ropb[po:po + m, :], ALU.add)

            class _Stop(Exception):
                pass

            def kstop(tag):
                if os.environ.get('KSTOP') == tag:
                    raise _Stop()
            for ti in range(ntiles):
              try:
                    t0 = ti * TT
                    wpos[0] = 0
                    S.dma("sp", stg, T(x_src[t0:t0 + TT, :].rearrange("(j p) f -> p j f", p=128), xmr[ti] if l > 0 else None))
                    for j in range(4):
                        for kt in range(0, 8, 4):
                            pb = ps()
                            for q in range(4):
                                S.tr(pb[:, q * 128:(q + 1) * 128], stg[:, j, (kt + q) * 128:(kt + q + 1) * 128], ident)
                            S.copy(evac_eng(), xT[:, kt:kt + 4, j * 128:(j + 1) * 128],
                                   pb.v(lambda a: a.rearrange("p (q t) -> p q t", t=128)))
                    S.dma("sp", posi, T(pos_in[0:1, t0:t0 + TT].partition_broadcast(128)))
                    S.copy("dve", posf, posi)
                    for ty, (inv, sgn) in enumerate((("invD", "sgnD"), ("invI", "sgnI"), ("invR", "sgnR"))):
                        S.ts("dve", ang, posf, pc(inv), ALU.mult)
                        sincos(rope[:, 2 * ty + 1, :], rope[:, 2 * ty, :], ang, ropa, posi, ropb)
                        S.ts("dve", rope[:, 2 * ty + 1, :], rope[:, 2 * ty + 1, :], pc(sgn), ALU.mult)
                    rmsnorm("g1")

                    wv = wnext("u")
                    for g in range(2):
                        pb = fm_mm(wv, 128 * g, 128, hk)
                        S.copy("dve", uT[:, g, :], pb)
                    for i in range(3):
                        wv = wnext("dq%d" % i)
                        for hh in range(2):
                            roped(wv, 128 * hh, 128 * hh + 64, 64, 0, qT[:, 2 * i + hh, :])
                    wv = wnext("dkik")
                    roped(wv, 0, 64, 64, 0, KT[0:64, t0:t0 + TT].wr(KTr[ti]))
                    roped(wv, 128, 160, 32, 1, KT[64:96, t0:t0 + TT].wr(KTr[ti]), po=64)
                    wv = wnext("iq")
                    for h in range(4):
                        roped(wv, 64 * h, 64 * h + 32, 32, 1, iqT[64:96, h, :], po=64)
                    for i in range(2):
                        wv = wnext("rq%d" % i)
                        roped(wv, 0, 128, 128, 2, rqT[:, i, :])
                    for i in range(2):
                        wv = wnext("rk%d" % i)
                        roped(wv, 0, 128, 128, 2, rkT[:, i, :])
                    wv = wnext("vw")
                    for j in range(4):
                        pb = ps()
                        for kt in range(8):
                            S.mm(pb[:, 0:68], hT[:, kt, j * 128:(j + 1) * 128], wv[:, kt, :], kt == 0, kt == 7)
                        S.copy("dve", VC[:, ti * 4 + j, 0:64].wr(VCr[ti]), pb[:, 0:64])
                        S.ts("dve", iw[:, j, :], pb[:, 64:68], 0.5 * 32 ** -0.5, ALU.mult)
                    wv = wnext("rv")
                    for j in range(4):
                        pb = ps()
                        for kt in range(8):
                            S.mm(pb[:, 0:384], hT[:, kt, j * 128:(j + 1) * 128], wv[:, kt, :], kt == 0, kt == 7)
                        S.copy("dve", rv[:, j, :], pb[:, 0:384])
                    wv = wnext("rg")
                    for j in range(4):
                        pb = ps()
                        for kt in range(8):
                            S.mm(pb[:, 0:384], hT[:, kt, j * 128:(j + 1) * 128], wv[:, kt, :], kt == 0, kt == 7)
                        S.actf(rgs[:, j, :], pb[:, 0:384], AF.Silu)

                    for ch in range(TT // TC):
                        cs = slice(ch * TC, (ch + 1) * TC)
                        for ct in range(2):
                            yacc = PD0 if ct == 0 else PD1
                            for kk in range(4):
                                k = 4 * ct + kk
                                pb = ps()
                                for ri in range(2):
                                    S.mm(pb[:, ri * TC:(ri + 1) * TC], BT[:, k, ri, :], uT[:, ct, cs], True, True)
                                bu = pb.v(r2)
                                S.tt("dve", w12, bu, CS[:, k, :, :], ALU.mult)
                                S.tt("dve", w43[:, 0, :], bu[:, 0, :], CS[:, k, 1, :], ALU.mult)
                                S.tt("dve", w43[:, 1, :], bu[:, 1, :], CS[:, k, 0, :], ALU.mult)
                                S.tt("pool", vv[:, 0, :], w12[:, 0, :], w12[:, 1, :], ALU.add)
                                S.tt("pool", vv[:, 1, :], w43[:, 1, :], w43[:, 0, :], ALU.subtract)
                                for ri in range(2):
                                    S.op("dve", lambda v, ri=ri, k=k: v.tensor_tensor_scan(
                                        xs_.ap[:, ri, :], rr.ap[:, k:k + 1].to_broadcast([128, TC]), vv.ap[:, ri, :],
                                        xprev.ap[:, k, ri:ri + 1], ALU.mult, ALU.add),
                                        reads=[rr, vv, xprev], writes=[xs_])
                                S.tt("pool", w12, xs_, CS[:, k, :, :], ALU.mult)
                                S.tt("pool", w43[:, 0, :], xs_[:, 0, :], CS[:, k, 1, :], ALU.mult)
                                S.tt("pool", w43[:, 1, :], xs_[:, 1, :], CS[:, k, 0, :], ALU.mult)
                                S.tt("dve", xo[:, 0, :], w12[:, 0, :], w12[:, 1, :], ALU.subtract)
                                S.tt("dve", xo[:, 1, :], w43[:, 0, :], w43[:, 1, :], ALU.add)
                                S.copy("act", xprev[:, k, :], xo[:, :, TC - 1])
                                for ri in range(2):
                                    S.mm(yacc[:, 0:TC], CT[:, k, ri, :], xo[:, ri, :], kk == 0 and ri == 0, kk == 3 and ri == 1)
                            yv = ypre[:, ct, cs]
                            S.stt("dve", yv, uT[:, ct, cs], pc("ssd", ct, ct + 1), yacc[:, 0:TC], ALU.mult, ALU.add)
                            S.tt("pool", w12[:, 0, :], yv, yv, ALU.mult)
                            S.ts("dve", w12[:, 0, :], w12[:, 0, :], 0.044715, ALU.mult, 1.0, ALU.add)
                            S.tt("pool", w12[:, 0, :], w12[:, 0, :], yv, ALU.mult)
                            S.actf(w12[:, 1, :], w12[:, 0, :], AF.Sigmoid, scale=2.0 * math.sqrt(2.0 / math.pi))
                            S.tt("dve", yv, yv, w12[:, 1, :], ALU.mult)
                    S.copy("act", ypb, ypre)
                    wv = wnext("glu")
                    for ct in range(2):
                        pb = fm_mm(wv, 128 * ct, 128, lambda kt: ypb[:, kt, :], nkt=2)
                        S.actf(gsig, pb, AF.Sigmoid, bias=pc("glub", ct, ct + 1))
                        S.tt("dve", yaT[:, ct, :], ypre[:, ct, :], gsig, ALU.mult)
                    if debug:
                        debug[0](S, locals(), dbg_out, ti, "s5")

                    _skipret = ti >= 1 and 'ret' in os.environ.get('KSKIP', '')
                    c64 = lambda a: a.rearrange("p (c t) -> p c t", t=64)
                    for i in range(2):
                        S.tt("pool", qd[:, i, :].v(c64), rqT[:, i, :].v(c64),
                             pc("qdec", 64 * i, 64 * i + 64).v(lambda a: a.unsqueeze(1).to_broadcast([128, 8, 64])), ALU.mult)
                    for j in range(4):
                        pb = ps()
                        pbv = pb.v(lambda a: a.bitcast(BF16))
                        for h in range(4):
                            base = 64 * (h % 2)
                            S.tr(pbv[:, 64 * h:64 * h + 48], rkT[base:base + 48, h // 2, j * 128:(j + 1) * 128], identb[base:base + 48, base:base + 48])
                        for h in range(4):
                            S.ts("dve", kd[:, j, h, :], pbv[:, 64 * h:64 * h + 48], pc("kdec", h, h + 1), ALU.mult)
                    for j in range(4):
                        for half in range(2):
                            rb = 64 * half
                            cs0 = j * 128 + rb
                            pout = pbank[4 + half] if not os.environ.get('POUT_PD1') else (PD1 if os.environ.get('POUT_PD1') == '1' else pbank[4])
                            for h in range(4):
                                base = 64 * (h % 2)
                                pair = h // 2
                                pa = pbank[(2 * h) % 4]
                                S.mm(pa[rb:rb + 64, 0:64], rkT[base:base + 48, pair, cs0:cs0 + 64], rqT[base:base + 48, pair, cs0:cs0 + 64], True, True)
                                kstop('r%d%d_h%d_s0' % (j, half, h))
                                S.tt("dve", At[rb:rb + 64, :], pa[rb:rb + 64, 0:64], pc("intra", 64 * h, 64 * h + 64)[rb:rb + 64, :], ALU.mult)
                                kstop('r%d%d_h%d_s1' % (j, half, h))
                                if os.environ.get('FAKEDEP'):
                                    S.op("pe", lambda p, h=h, rb=rb: p.matmul(pout.ap[rb:rb + 64, 96 * h:96 * h + 96], lhsT=At.ap[rb:rb + 64, :], rhs=rv.ap[rb:rb + 64, j, 96 * h:96 * h + 96], start=True, stop=False), reads=[At, rv, yr], writes=[pout])
                                else:
                                    S.mm(pout[rb:rb + 64, 96 * h:96 * h + 96], At[rb:rb + 64, :], rv[rb:rb + 64, j, 96 * h:96 * h + 96], True, False)
                                kstop('r%d%d_h%d_s2' % (j, half, h))
                                S.mm(pout[rb:rb + 64, 96 * h:96 * h + 96], qd[base:base + 48, pair, cs0:cs0 + 64], Sbf[base:base + 48, pair, :], False, True)
                                kstop('r%d%d_h%d_s3' % (j, half, h))
                                pu = pbank[(2 * h + 1) % 4]
                                S.mm(pu[base:base + 48, 0:96], kd[rb:rb + 64, j, h, :], rv[rb:rb + 64, j, 96 * h:96 * h + 96], True, True)
                                kstop('r%d%d_h%d_s4' % (j, half, h))
                                S.stt("dve", Sst[base:base + 48, pair, :], Sst[base:base + 48, pair, :], CONST["cdec"][h], pu[base:base + 48, 0:96], ALU.mult, ALU.add)
                                kstop('r%d%d_h%d_s5' % (j, half, h))
                                S.copy("act", Sbf[base:base + 48, pair, :], Sst[base:base + 48, pair, :])
                                kstop('r%d%d_h%d_s6' % (j, half, h))
                            S.copy("dve", yr[rb:rb + 64, :], pout[rb:rb + 64, 0:384])
                            kstop('ret_%d_%d' % (j, half))
                        yr3 = yr.v(lambda a: a.rearrange("p (h e) -> p h e", e=96))
                        S.op("dve", lambda v: v.tensor_reduce(st4.ap[:, 0, :], yr3.ap, AX.X, ALU.add), reads=[yr], writes=[st4])
                        S.tt("pool", ysq, yr, yr, ALU.mult)
                        S.op("dve", lambda v: v.tensor_reduce(st4.ap[:, 1, :], ysq.ap.rearrange("p (h e) -> p h e", e=96), AX.X, ALU.add), reads=[ysq], writes=[st4])
                        S.ts("dve", st4[:, 2, :], st4[:, 0, :], 1.0 / 96, ALU.mult)
                        S.tt("dve", st4[:, 0, :], st4[:, 2, :], st4[:, 2, :], ALU.mult)
                        S.stt("dve", st4[:, 3, :], st4[:, 1, :], 1.0 / 96, st4[:, 0, :], ALU.mult, ALU.subtract)
                        S.actf(st4[:, 3, :], st4[:, 3, :], AF.Sqrt, bias=EPS)
                        S.op("dve", lambda v: v.reciprocal(st4.ap[:, 3, :], st4.ap[:, 3, :]), reads=[st4], writes=[st4])
                        bc = lambda c: st4[:, c, :].v(lambda a: a.unsqueeze(2).to_broadcast([128, 4, 96]))
                        S.tt("dve", yr3, yr3, bc(2), ALU.subtract)
                        S.tt("dve", yr3, yr3, bc(3), ALU.mult)
                        S.tt("pool", yr, yr, pc("retg"), ALU.mult)
                        S.tt("pool", ycb, yr, rgs[:, j, :], ALU.mult)
                        pb = ps()
                        pbv = pb.v(lambda a: a.bitcast(BF16))
                        for c3 in range(3):
                            S.tr(pbv[:, 128 * c3:128 * c3 + 128], ycb[:, 128 * c3:128 * c3 + 128], identb)
                        S.copy("dve", ycT[:, :, j * 128:(j + 1) * 128], pbv[:, 0:384].v(lambda a: a.rearrange("p (c t) -> p c t", t=128)))
                    if debug:
                        debug[0](S, locals(), dbg_out, ti, "ret")

                    for j in range(4 if ti == 0 else int(os.environ.get('KDSAJ', '4'))):
                        qb = ti * 4 + j
                        nkb = qb + 1
                        nk = nkb * 128
                        qs = slice(j * 128, (j + 1) * 128)
                        for k0 in range(0, nk, 512):
                            kw = min(512, nk - k0)
                            tl = [KTr[t] for t in range(k0 // TT, (k0 + kw - 1) // TT + 1)]
                            for h in range(4):
                                pb = ps()
                                S.op("pe", lambda p, pb=pb, h=h, k0=k0, kw=kw: p.matmul(
                                    pb.ap[:, 0:kw], lhsT=iqT.ap[64:96, h, qs], rhs=KT.ap[64:96, k0:k0 + kw], start=True, stop=True),
                                    reads=[iqT] + tl, writes=[pb])
                                if h == 0:
                                    S.actf(big[:, k0:k0 + kw], pb[:, 0:kw], AF.Relu)
                                    S.ts("dve", big[:, k0:k0 + kw], big[:, k0:k0 + kw], iw[:, j, 0:1], ALU.mult)
                                else:
                                    S.actf(relu_t[:, 0:kw], pb[:, 0:kw], AF.Relu)
                                    S.stt("dve" if h % 2 else "pool", big[:, k0:k0 + kw], relu_t[:, 0:kw], iw[:, j, h:h + 1], big[:, k0:k0 + kw], ALU.mult, ALU.add)
                        S.tt("pool", big[:, nk - 128:nk], big[:, nk - 128:nk], pc("diag"), ALU.add)
                        S.memset("dve", nmid, -(BIS_LO + BIS_W / 2))
                        S.memset("dve", accs[0], 0.0)
                        thr_s = float(2 * TOPK - nk)
                        w_i = BIS_W / 2
                        nA = min(nk, 4096)
                        for it in range(NBIS):
                            acc = accs[it % 2]
                            S.actf(junk[:, 0:nA], big[:, 0:nA], AF.Sign, bias=nmid, accum=acc[:, 0:1])
                            if nk > nA:
                                S.actf(junk[:, 0:nk - nA], big[:, nA:nk], AF.Sign, bias=nmid, accum=acc[:, 1:2])
                            S.memset("dve", accs[(it + 1) % 2], 0.0)
                            if nk > nA:
                                S.tt("dve", acc[:, 0:1], acc[:, 0:1], acc[:, 1:2], ALU.add)
                            S.ts("dve", gcol, acc[:, 0:1], thr_s, ALU.is_ge, -w_i, ALU.mult)
                            if it < NBIS - 1:
                                S.stt("dve", nmid, gcol, w_i / 2, nmid, ALU.add, ALU.add)
                            else:
                                S.stt("dve", nlo, gcol, w_i, nmid, ALU.add, ALU.add)
                            w_i = w_i / 2
                        for kb in range(nkb + 1):
                            if kb < nkb:
                                ks = slice(kb * 128, (kb + 1) * 128)
                                kreg = KTr[kb // 4]
                                pTc, mTc = pTs[kb % 2], mTs[kb % 2]
                                S.ts("dve", m01, big[:, ks], nlo, ALU.add, 0.0, ALU.is_ge)
                                pm = ps()
                                pmv = pm.v(lambda a: a.bitcast(BF16))
                                S.tr(pmv[:, 0:128], m01, identb)
                                S.copy("dve", mTc, pmv[:, 0:128])
                                for g in range(2):
                                    pp = ps()
                                    S.op("pe", lambda p, pp=pp, g=g, ks=ks: p.matmul(
                                        pp.ap[:, 0:384], lhsT=KT.ap[0:64, ks], rhs=qT.ap[0:64, 3 * g:3 * g + 3, qs], start=True, stop=True),
                                        reads=[qT, kreg], writes=[pp])
                                    S.actf(pTc[:, g, :, :], pp[:, 0:384].v(lambda a: a.rearrange("p (c t) -> p c t", t=128)), AF.Exp, scale=0.125)
                                p6 = pTc.v(lambda a: a.rearrange("p g c t -> p (g c) t"))
                                S.tt("pool", p6, p6, mTc.v(lambda a: a.unsqueeze(1).to_broadcast([128, 6, 128])), ALU.mult)
                            if kb >= 1:
                                kp = kb - 1
                                pTp = pTs[kp % 2]
                                for g in range(2):
                                    PDg = PD0 if g == 0 else PD1
                                    S.op("pe", lambda p, g=g, kp=kp, PDg=PDg, pTp=pTp: p.matmul(
                                        PDg.ap[0:96, 0:384], lhsT=VC.ap[:, kp, :], rhs=pTp.ap[:, g, :, :],
                                        start=(kp == 0), stop=(kp == nkb - 1)),
                                        reads=[pTp, VCr[kp // 4]], writes=[PDg], acc=(kp != 0))
                        S.copy("dve", ot[0:96, 0:384], PD0[0:96, 0:384])
                        S.copy("dve", ot[0:96, 384:768], PD1[0:96, 0:384])
                        po = [ps(), ps()]
                        for h in range(6):
                            S.tr(po[h // 3][:, 96 * (h % 3):96 * (h % 3) + 96], ot[0:96, 128 * h:128 * h + 128], ident[0:96, 0:96])
                        for g in range(2):
                            S.copy("dve", pvs[:, 3 * g:3 * g + 3, :], po[g][:, 0:288].v(lambda a: a.rearrange("p (c e) -> p c e", e=96))[:, :, 0:65])
                        S.op("dve", lambda v: v.reciprocal(rden.ap, pvs.ap[:, :, 64]), reads=[pvs], writes=[rden])
                        S.tt("dve", ybt.v(lambda a: a.rearrange("p (h e) -> p h e", e=64)), pvs[:, :, 0:64],
                             rden.v(lambda a: a.unsqueeze(2).to_broadcast([128, 6, 64])), ALU.mult)
                        pb = ps()
                        pbv = pb.v(lambda a: a.bitcast(BF16))
                        for c3 in range(3):
                            S.tr(pbv[:, 128 * c3:128 * c3 + 128], ybt[:, 128 * c3:128 * c3 + 128], identb)
                        S.copy("dve", ybT[:, :, qs], pbv[:, 0:384].v(lambda a: a.rearrange("p (c t) -> p c t", t=128)))
                    if debug:
                        debug[0](S, locals(), dbg_out, ti, "dsa")

                    mrg = sq
                    srcs = [(yaT, 0, 2), (ybT, 2, 3), (ycT, 5, 3)]
                    for ft in range(8):
                        wp = wnext("pr%d" % ft)
                        pP = []
                        for bi, (yt, k0, nk_) in enumerate(srcs):
                            pb = ps()
                            for kt in range(nk_):
                                S.mm(pb, wp[:, k0 + kt, :], yt[:, kt, :], kt == 0, kt == nk_ - 1)
                            pP.append(pb)
                        wg = wnext("gt%d" % ft)
                        for bi, gt in enumerate((gA, gB, gC)):
                            pb = fm_mm(wg, 128 * bi, 128, hk)
                            S.actf(gt, pb, AF.Sigmoid)
                        S.tt("dve", gA, gA, pP[0], ALU.mult)
                        S.tt("dve", gB, gB, pP[1], ALU.mult)
                        S.tt("dve", gC, gC, pP[2], ALU.mult)
                        S.tt("pool", gA, gA, gB, ALU.add)
                        S.tt("pool", mrg[:, ft, :], gA, gC, ALU.add)
                    for ft in range(8):
                        wv = wnext("wo%d" % ft)
                        pb = fm_mm(wv, 0, 128, lambda kt: mrg[:, kt, :])
                        S.tt("dve", xT[:, ft, :], xT[:, ft, :], pb, ALU.add)
                    if debug:
                        debug[0](S, locals(), dbg_out, ti, "mix")

                    rmsnorm("g2")
                    for f4 in range(8):
                        wv = wnext("w1_%d" % f4)
                        for q in range(4):
                            f = f4 * 4 + q
                            pb = fm_mm(wv, 128 * q, 128, hk)
                            S.actf(rl, pb, AF.Relu)
                            S.tt("pool" if f % 2 else "dve", aT[:, f, :], rl, rl, ALU.mult)
                    for ft in range(8):
                        wv = wnext("w2_%d" % ft)
                        pb = fm_mm(wv, 0, 128, lambda kt: aT[:, kt, :], nkt=32)
                        S.tt("dve", xT[:, ft, :], xT[:, ft, :], pb, ALU.add)

                    rms_stats()
                    oT = xT
                    tmpo = rope.v(lambda a: a[:, 0:1, :].rearrange("p s t -> p (s t)"))
                    for kt in range(8):
                        S.stt("dve", tmpo, xT[:, kt, :], ogf[:, kt:kt + 1], rstd, ALU.mult, ALU.mult)
                        S.stt("pool", xT[:, kt, :], xT[:, kt, :], omf[:, 0:1], tmpo, ALU.mult, ALU.add)
                    for j in range(4):
                        for kt in range(0, 8, 4):
                            pb = ps()
                            for q in range(4):
                                S.tr(pb[:, q * 128:(q + 1) * 128], oT[:, kt + q, j * 128:(j + 1) * 128], ident)
                            S.copy(evac_eng(), stg[:, j, kt * 128:(kt + 4) * 128], pb)
                    S.dma("sp", T(y_dst[t0:t0 + TT, :].rearrange("(j p) f -> p j f", p=128), xmr[ti] if l < nlayers - 1 else None), stg)
              except _Stop:
                break
        S.finish()
        print("instructions:", S.nins, "sbuf left:", nc.sbuf_bytes_remaining)
    return nc


_PROG = {}


def _layer_arrays(inp, l, flag):
    ws = {"wa": _arrange_w_in(np.asarray(inp["w_in"][l], np.float32)),
          "wglu": np.asarray(inp["ssm_glu_w"][l], np.float32),
          "wpr": np.concatenate([inp["w_proj_a"][l], inp["w_proj_b"][l], inp["w_proj_c"][l]], axis=0).astype(np.float32),
          "wo": np.asarray(inp["w_out"][l], np.float32),
          "w1": np.asarray(inp["w_ff1"][l], np.float32),
          "w2": np.asarray(inp["w_ff2"][l], np.float32)}
    return _flat_weights(ws), _pack_params(inp, l, flag)


def _maps(inp, layers, xs, ntok, last_is_final=True):
    wfs, Ps = [], []
    for i, l in enumerate(layers):
        flag = 1.0 if (last_is_final and i == len(layers) - 1) else 0.0
        wf, P = _layer_arrays(inp, l, flag)
        wfs.append(wf)
        Ps.append(P)
    wf = np.concatenate(wfs, axis=0)
    P = np.concatenate(Ps, axis=0)
    return [{"x_in": np.ascontiguousarray(xs[b][:ntok], dtype=np.float32),
             "pos": np.ascontiguousarray(inp["positions"][b:b + 1, :ntok], dtype=np.int32),
             "P": P, "wf": wf} for b in range(len(xs))]


def kernel(**inputs):
    inp = {k: np.asarray(v) for k, v in inputs.items()}
    ntiles = SEQ // TT
    key = (ntiles, DEPTH)
    if key not in _PROG:
        _PROG[key] = build_program(ntiles, DEPTH)
    nc = _PROG[key]
    xs = [inp["x"][b] for b in range(BATCH)]
    maps = _maps(inp, list(range(DEPTH)), xs, SEQ)
    res = run_bass_kernel_spmd(nc, maps, core_ids=list(range(BATCH)))
    return np.stack([np.asarray(r["y"]) for r in res.results], axis=0).astype(np.float32)
```

```python
import math
import os
from contextlib import ExitStack
import numpy as np
import concourse.bass as bass
import concourse.mybir as mybir
from concourse.bass_utils import run_bass_kernel_spmd

F32 = mybir.dt.float32
BF16 = mybir.dt.bfloat16
I32 = mybir.dt.int32
ALU = mybir.AluOpType
AF = mybir.ActivationFunctionType
AX = mybir.AxisListType

D = 1024
SEQ = 8192
BATCH = 4
DEPTH = 2
TT = 512
EPS = 1e-6
BIG = 30000.0
TOPK = 256
NBIS = 22
BIS_LO, BIS_W = -16.0, 32.0


class Reg:
    __slots__ = ("w", "r")

    def __init__(self):
        self.w = None
        self.r = {}


class T:
    __slots__ = ("ap", "reg")

    def __init__(self, ap, reg=None):
        self.ap = ap
        self.reg = reg if reg is not None else Reg()

    def __getitem__(self, idx):
        return T(self.ap[idx], self.reg)

    def v(self, fn):
        return T(fn(self.ap), self.reg)

    def wr(self, reg):
        return T(self.ap, reg)


def _regs(xs):
    out = []
    for x in xs:
        if isinstance(x, T):
            x = x.reg
        if isinstance(x, Reg):
            out.append(x)
        elif isinstance(x, (tuple, list)):
            out.extend(x)
    return out


def _ap(x):
    return x.ap if isinstance(x, T) else x


class Sched:
    def __init__(self, nc, stack):
        self.nc = nc
        self.eng = {"pe": nc.tensor, "act": nc.scalar, "dve": nc.vector, "pool": nc.gpsimd, "sp": nc.sync}
        self.sem, self.cnt, self.seen = {}, {}, {}
        for e in ["pe", "act", "dve", "pool"]:
            self.sem[e] = stack.enter_context(nc.semaphore("s_" + e))
        self.NDS = 4
        self.dcnt = {}
        for q in ["sp", "pool"]:
            self.dcnt[q] = 0
            for i in range(self.NDS):
                self.sem["dma_%s%d" % (q, i)] = stack.enter_context(nc.semaphore("s_dma_%s%d" % (q, i)))
        for k in self.sem:
            self.cnt[k] = 0
        self.names = list(self.sem.keys())
        for e in ["pe", "act", "dve", "pool", "sp"]:
            self.seen[e] = {k: 0 for k in self.names}
        self.nins = 0
        self.psum_regs = set()
        self.act_dummy = None
        self.fence = None

    def _waits(self, e, reads, writes, acc):
        deps = {}

        def add(d):
            if d is not None and deps.get(d[0], 0) < d[1]:
                deps[d[0]] = d[1]
        for r in reads:
            add(r.w)
        for w in writes:
            if not acc:
                add(w.w)
            for k, t in w.r.items():
                add((k, t))
        eh, seen = self.eng[e], self.seen[e]
        if e == "pe" and self.fence is not None and seen["act"] < deps.get("act", 0):
            ta = deps.pop("act")
            pseen = self.seen["pool"]
            if pseen["act"] < ta:
                self.eng["pool"].wait_ge(self.sem["act"], ta)
                pseen["act"] = ta
            self.eng["pool"].memset(self.fence, 0.0).then_inc(self.sem["pool"], 1)
            self.cnt["pool"] += 1
            self.nins += 1
            seen["act"] = ta
            if deps.get("pool", 0) < self.cnt["pool"]:
                deps["pool"] = self.cnt["pool"]
        for k, t in deps.items():
            if seen[k] < t:
                eh.wait_ge(self.sem[k], t)
                seen[k] = t

    def op(self, e, fn, reads=(), writes=(), acc=False):
        reads, writes = _regs(reads), _regs(writes)
        self._waits(e, reads, writes, acc)
        ins = fn(self.eng[e])
        self.cnt[e] += 1
        t = self.cnt[e]
        ins.then_inc(self.sem[e], 1)
        self.nins += 1
        if e == "act" and self.act_dummy is not None and any(id(r) in self.psum_regs for r in reads):
            d0, d1 = self.act_dummy
            self.eng["act"].copy(d0, d1).then_inc(self.sem[e], 1)
            self.cnt[e] += 1
            t = self.cnt[e]
            self.nins += 1
        for r in reads:
            if r.r.get(e, 0) < t:
                r.r[e] = t
        for w in writes:
            w.w = (e, t)
            if not acc:
                w.r = {}
        return ins

    def dma(self, q, out, in_, **kw):
        reads, writes = _regs([in_]), _regs([out])
        self._waits(q, reads, writes, False)
        k = "dma_%s%d" % (q, self.dcnt[q] % self.NDS)
        self.dcnt[q] += 1
        if self.seen[q][k] < self.cnt[k]:
            self.eng[q].wait_ge(self.sem[k], self.cnt[k])
            self.seen[q][k] = self.cnt[k]
        ins = self.eng[q].dma_start(out=_ap(out), in_=_ap(in_), **kw)
        self.cnt[k] += 16
        t = self.cnt[k]
        ins.then_inc(self.sem[k], 16)
        self.nins += 1
        for r in reads:
            if r.r.get(k, 0) < t:
                r.r[k] = t
        for w in writes:
            w.w = (k, t)
            w.r = {}
        return ins

    def finish(self):
        for e in ["sp", "act", "pool", "dve", "pe"]:
            for k in self.names:
                if self.cnt[k] > self.seen[e][k]:
                    self.eng[e].wait_ge(self.sem[k], self.cnt[k])
                    self.seen[e][k] = self.cnt[k]

    def mm(self, out, lhsT, rhs, start, stop):
        return self.op("pe", lambda p: p.matmul(out.ap, lhsT=lhsT.ap, rhs=rhs.ap, start=start, stop=stop),
                       reads=[lhsT, rhs], writes=[out], acc=not start)

    def tr(self, out, in_, ident):
        return self.op("pe", lambda p: p.transpose(out.ap, in_.ap, ident.ap), reads=[in_, ident], writes=[out])

    def actf(self, out, in_, func, bias=None, scale=1.0, accum=None, eng="act"):
        kw = {}
        if bias is not None:
            kw["bias"] = _ap(bias)
        if accum is not None:
            kw["accum_out"] = accum.ap
        wr = [out] + ([accum] if accum is not None else [])
        return self.op(eng, lambda a: a.activation(out.ap, in_.ap, func, scale=_ap(scale), **kw),
                       reads=[in_, bias, scale], writes=wr)

    def tt(self, eng, out, a, b, op):
        return self.op(eng, lambda v: v.tensor_tensor(out.ap, a.ap, b.ap, op), reads=[a, b], writes=[out])

    def ts(self, eng, out, a, s1, op0, s2=None, op1=None, accum=None):
        kw = {}
        if op1 is not None:
            kw["op1"] = op1
        if accum is not None:
            kw["accum_out"] = accum.ap
        wr = [out] + ([accum] if accum is not None else [])
        return self.op(eng, lambda v: v.tensor_scalar(out.ap, a.ap, _ap(s1), _ap(s2) if s2 is not None else None, op0, **kw),
                       reads=[a, s1, s2], writes=wr)

    def stt(self, eng, out, a, s, b, op0, op1):
        eng = "dve"
        return self.op(eng, lambda v: v.scalar_tensor_tensor(out.ap, a.ap, _ap(s), b.ap, op0, op1),
                       reads=[a, s, b], writes=[out])

    def copy(self, eng, out, in_):
        if eng == "act":
            return self.op("act", lambda a: a.copy(out.ap, in_.ap), reads=[in_], writes=[out])
        return self.op(eng, lambda v: v.tensor_copy(out.ap, in_.ap), reads=[in_], writes=[out])

    def memset(self, eng, out, val):
        return self.op(eng, lambda v: v.memset(out.ap, val), writes=[out])


IN_SIZES = (256, 384, 64, 64, 128, 32, 4, 192, 192, 384, 384, 3072)
IN_OFF = np.concatenate([[0], np.cumsum(IN_SIZES)]).astype(int)
(O_U, O_DQ, O_DK, O_DV, O_IQ, O_IK, O_IW, O_RQ, O_RK, O_RV, O_RG, O_GT) = [int(v) for v in IN_OFF[:12]]


def _swap_cols(base, hd, rot):
    half = rot // 2
    idx = list(range(hd))
    for i in range(half):
        idx[i], idx[i + half] = i + half, i
    return [base + i for i in idx]


def _wa_columns():
    fm = list(range(O_U, O_U + 256))
    for h in range(6):
        fm += list(range(O_DQ + 64 * h, O_DQ + 64 * h + 64)) + _swap_cols(O_DQ + 64 * h, 64, 16)
    fm += list(range(O_DK, O_DK + 64)) + _swap_cols(O_DK, 64, 16)
    for h in range(4):
        fm += list(range(O_IQ + 32 * h, O_IQ + 32 * h + 32)) + _swap_cols(O_IQ + 32 * h, 32, 8)
    fm += list(range(O_IK, O_IK + 32)) + _swap_cols(O_IK, 32, 8)
    for off in (O_RQ, O_RK):
        for i in range(2):
            z, s = [], []
            for h in (2 * i, 2 * i + 1):
                z += list(range(off + 48 * h, off + 48 * h + 48)) + [-1] * 16
                s += _swap_cols(off + 48 * h, 48, 48) + [-1] * 16
            fm += z + s
    tm = list(range(O_DV, O_DV + 64)) + list(range(O_IW, O_IW + 4))
    tm += list(range(O_RV, O_RV + 384)) + list(range(O_RG, O_RG + 384))
    gt = list(range(O_GT, O_GT + 3072))
    return fm + tm + gt


WA_COLS = _wa_columns()
NA = len(WA_COLS)
A_U = 0
A_DQ = 256
A_DK = A_DQ + 768
A_IQ = A_DK + 128
A_IK = A_IQ + 256
A_RQ = A_IK + 64
A_RK = A_RQ + 512
A_VW = A_RK + 512
A_RV = A_VW + 68
A_RG = A_RV + 384
A_GT = A_RG + 384
assert A_GT + 3072 == NA


def _plan():
    r = lambda a, n: list(range(a, a + n))
    pl = [("u", "wa", 8, r(A_U, 256))]
    for i in range(3):
        pl.append(("dq%d" % i, "wa", 8, r(A_DQ + 256 * i, 256)))
    pl.append(("dkik", "wa", 8, r(A_DK, 128) + r(A_IK, 64)))
    pl.append(("iq", "wa", 8, r(A_IQ, 256)))
    for i in range(2):
        pl.append(("rq%d" % i, "wa", 8, r(A_RQ + 256 * i, 256)))
    for i in range(2):
        pl.append(("rk%d" % i, "wa", 8, r(A_RK + 256 * i, 256)))
    pl.append(("vw", "wa", 8, r(A_VW, 68)))
    pl.append(("rv", "wa", 8, r(A_RV, 384)))
    pl.append(("rg", "wa", 8, r(A_RG, 384)))
    pl.append(("glu", "wglu", 2, r(0, 256)))
    for ft in range(8):
        pl.append(("pr%d" % ft, "wpr", 8, r(128 * ft, 128)))
        pl.append(("gt%d" % ft, "wa", 8, r(A_GT + 128 * ft, 128) + r(A_GT + 1024 + 128 * ft, 128) + r(A_GT + 2048 + 128 * ft, 128)))
    for ft in range(8):
        pl.append(("wo%d" % ft, "wo", 8, r(128 * ft, 128)))
    for f4 in range(8):
        pl.append(("w1_%d" % f4, "w1", 8, r(512 * f4, 512)))
    for ft in range(8):
        pl.append(("w2_%d" % ft, "w2", 32, r(128 * ft, 128)))
    return pl


PLAN = _plan()
PLAN_OFF = []
_o = 0
for _nm, _s, _k, _c in PLAN:
    PLAN_OFF.append(_o)
    _o += _k * len(_c)
NW = _o


def _flat_weights(ws):
    out = np.empty((128, NW), np.float32)
    for (nm, s, nkt, cols), o in zip(PLAN, PLAN_OFF):
        w = ws[s][:, cols]
        n = len(cols)
        out[:, o:o + nkt * n] = w.reshape(nkt, 128, n).transpose(1, 0, 2).reshape(128, nkt * n)
    return out


_pc = {}
_off = 0
for _n, _w in [("g1", 8), ("g2", 8), ("gf", 8), ("glub", 2), ("ssd", 2), ("lre", 8), ("lim", 8), ("lst", 8),
               ("bre", 128), ("bim", 128), ("cre", 128), ("cim", 128), ("flag", 1), ("retg", 384),
               ("invD", 1), ("sgnD", 1), ("invI", 1), ("sgnI", 1), ("invR", 1), ("sgnR", 1),
               ("ramp", 256), ("intra", 256), ("qdec", 128), ("kdec", 4), ("diag", 128)]:
    _pc[_n] = (_off, _w)
    _off += _w
NP_ = _off


def _const_tables():
    c = {}
    p = np.arange(128)
    d = p % 64
    inv = np.where(d < 16, np.exp(-math.log(500000.0) * (d % 8) * (2.0 / 16)), 0.0)
    c["invD"] = inv[:, None]
    c["sgnD"] = np.where(d < 8, -1.0, np.where(d < 16, 1.0, 0.0))[:, None]
    d = p % 32
    inv = np.where(d < 8, np.exp(-math.log(500000.0) * (d % 4) * (2.0 / 8)), 0.0)
    c["invI"] = inv[:, None]
    c["sgnI"] = np.where(d < 4, -1.0, np.where(d < 8, 1.0, 0.0))[:, None]
    d = p % 64
    inv = np.where(d < 48, np.exp(-math.log(10000.0) * (d % 24) * (2.0 / 48)), 0.0)
    c["invR"] = inv[:, None]
    c["sgnR"] = np.where(d < 24, -1.0, np.where(d < 48, 1.0, 0.0))[:, None]
    c["ramp"] = np.broadcast_to(np.arange(1, 257, dtype=np.float64)[None, :], (128, 256))
    log_g = np.log1p(-np.exp2(-5.0 - np.arange(4)))
    sc = 48 ** -0.5
    m = (p % 64)[:, None]
    cc = np.arange(64)[None, :]
    intra = np.zeros((128, 4, 64))
    qdec = np.zeros((128, 2, 64))
    kdec = np.zeros((128, 4))
    for h in range(4):
        intra[:, h, :] = np.where(cc >= m, np.exp(log_g[h] * np.maximum(cc - m, 0)), 0.0) * sc
        kdec[:, h] = np.exp(log_g[h] * (63.0 - (p % 64)))
    for pair in range(2):
        for half in range(2):
            h = 2 * pair + half
            qdec[64 * half:64 * half + 64, pair, :] = (np.exp(log_g[h] * (np.arange(64) + 1.0)) * sc)[None, :]
    c["intra"] = intra.reshape(128, 256)
    c["qdec"] = qdec.reshape(128, 128)
    c["kdec"] = kdec
    q = np.arange(128)[:, None]
    k = np.arange(128)[None, :]
    c["diag"] = np.where(k < (q // 64 + 1) * 64, 0.0, -BIG)
    c["cdec"] = [float(np.exp(log_g[h] * 64.0)) for h in range(4)]
    return c


CONST = _const_tables()


def _pack_params(inp, l, flag):
    P = np.zeros((128, NP_), np.float32)

    def put(name, arr):
        o, w = _pc[name]
        P[:, o:o + w] = np.asarray(arr, np.float32).reshape(128, w)
    put("g1", inp["norm1_g"][l].reshape(8, 128).T)
    put("g2", inp["norm2_g"][l].reshape(8, 128).T)
    put("gf", inp["final_norm_g"].reshape(8, 128).T)
    put("glub", inp["ssm_glu_b"][l].reshape(2, 128).T)
    put("ssd", inp["ssm_d"][l].reshape(2, 128).T)
    put("lre", inp["ssm_lambda_re"][l].reshape(8, 128).T)
    put("lim", inp["ssm_lambda_im"][l].reshape(8, 128).T)
    put("lst", np.repeat(inp["ssm_log_step"][l], 64).reshape(8, 128).T)
    put("bre", inp["ssm_b_re"][l].reshape(8, 128, 16).transpose(1, 0, 2))
    put("bim", inp["ssm_b_im"][l].reshape(8, 128, 16).transpose(1, 0, 2))
    put("cre", inp["ssm_c_re"][l].transpose(0, 2, 1).reshape(8, 128, 16).transpose(1, 0, 2))
    put("cim", inp["ssm_c_im"][l].transpose(0, 2, 1).reshape(8, 128, 16).transpose(1, 0, 2))
    put("flag", np.full((128, 1), flag))
    put("retg", np.broadcast_to(inp["ret_norm_g"][l][None, :], (128, 384)))
    for n in ["invD", "sgnD", "invI", "sgnI", "invR", "sgnR", "ramp", "intra", "qdec", "kdec", "diag"]:
        put(n, CONST[n])
    return P


def _arrange_w_in(w):
    wz = np.concatenate([w, np.zeros((w.shape[0], 1), w.dtype)], axis=1)
    return wz[:, WA_COLS]


def build_program(ntiles, nlayers=1, debug=None):
    nc = bass.Bass("TRN2", target_bir_lowering=False)
    NTOK = ntiles * TT
    dr = lambda n, s, dt, kind: nc.dram_tensor(n, s, dt, kind=kind).ap()
    x_in = dr("x_in", [NTOK, D], F32, "ExternalInput")
    pos_in = dr("pos", [1, NTOK], I32, "ExternalInput")
    P_in = dr("P", [nlayers * 128, NP_], F32, "ExternalInput")
    wf_in = dr("wf", [nlayers * 128, NW], F32, "ExternalInput")
    y_out = dr("y", [NTOK, D], F32, "ExternalOutput")
    dbg_out = None
    if debug:
        dbg_out = dr("dbg", list(debug[1]), F32, "ExternalOutput")
    wf_b = dr("wf_b", [nlayers * 128, NW], BF16, "Internal")
    xmid = dr("xmid", [NTOK, D], F32, "Internal") if nlayers > 1 else None
    xmr = [Reg() for _ in range(ntiles)]

    with ExitStack() as st:
        S = Sched(nc, st)

        _sbc = {}

        def sb(name, shape, dt=F32):
            if name not in _sbc:
                _sbc[name] = T(st.enter_context(nc.sbuf_tensor(name, shape, dt))[:])
            return _sbc[name]

        print("sbuf at start:", nc.sbuf_bytes_remaining)
        CH = 2048
        wchunks = [[] for _ in range(nlayers)]
        for l_ in range(nlayers):
            for c0 in range(0, NW, CH):
                c1 = min(NW, c0 + CH)
                rg = Reg()
                S.dma("pool", T(wf_b[l_ * 128:(l_ + 1) * 128, c0:c1], rg), T(wf_in[l_ * 128:(l_ + 1) * 128, c0:c1]))
                wchunks[l_].append((c0, c1, rg))
        cur = [0]

        Pt = sb("Pt", [128, NP_])

        def pc(name, a=None, b=None):
            o, w = _pc[name]
            a = 0 if a is None else a
            b = w if b is None else b
            return Pt[:, o + a:o + b]

        ident = sb("ident", [128, 128])
        S.memset("pool", ident, 0.0)
        S.op("pool", lambda g: g.affine_select(ident.ap, ident.ap, pattern=[[-1, 128]], compare_op=ALU.not_equal,
                                               fill=1.0, base=0, channel_multiplier=1), reads=[ident], writes=[ident])
        identb = sb("identb", [128, 128], BF16)
        S.copy("dve", identb, ident)
        onesb = sb("onesb", [128, 128], BF16)
        S.memset("pool", onesb, 1.0)

        xT = sb("xT", [128, 8, TT])
        hT = sb("hT", [128, 8, TT], BF16)
        sq = sb("sq", [128, 8, TT], BF16)
        rstd = sb("rstd", [128, TT])
        big = sb("big", [128, 8192])
        aT = big.v(lambda a: a.bitcast(BF16).rearrange("p (k t) -> p k t", t=TT)[:, 0:32, :])
        stg = big.v(lambda a: a[:, 0:4 * D].rearrange("p (j f) -> p j f", f=D))
        KT = sb("KT", [128, NTOK], BF16)
        VC = sb("VC", [128, NTOK // 128, 96], BF16)
        KTr = [Reg() for _ in range(ntiles)]
        VCr = [Reg() for _ in range(ntiles)]
        S.memset("pool", VC, 1.0)
        for r in VCr:
            r.w = VC.reg.w
        uT = sb("uT", [128, 2, TT])
        qT = sb("qT", [64, 6, TT], BF16)
        iqT = sb("iqT", [128, 4, TT], BF16)
        iw = sb("iw", [128, 4, 4])
        rqT = sb("rqT", [128, 2, TT], BF16)
        rkT = sb("rkT", [128, 2, TT], BF16)
        rv = sb("rv", [128, 4, 384], BF16)
        rgs = sb("rgs", [128, 4, 384], BF16)
        yaT = sb("yaT", [128, 2, TT], BF16)
        ybT = sb("ybT", [128, 3, TT], BF16)
        ycT = sb("ycT", [128, 3, TT], BF16)
        NSL = 11
        U = st.enter_context(nc.sbuf_tensor("U", [128, NSL * 512], F32))[:]
        Ureg = [Reg() for _ in range(NSL)]

        def carve(s0, ns, shape_fn=None, dt=F32):
            ap = U[:, s0 * 512:(s0 + ns) * 512]
            if dt == BF16:
                ap = ap.bitcast(BF16)
            elif dt == I32:
                ap = ap.bitcast(I32)
            if shape_fn is not None:
                ap = shape_fn(ap)
            return T(ap, tuple(Ureg[s0:s0 + ns]))
        posi = carve(0, 1, None, I32)
        posf = carve(1, 1)
        ang = carve(2, 1)
        rope = carve(3, 6, lambda a: a.rearrange("p (s t) -> p s t", t=TT))
        ropa = carve(9, 1)
        ropb = carve(10, 1)
        TC = 256
        r2 = lambda a: a.rearrange("p (r t) -> p r t", t=TC)
        w12 = carve(0, 1, r2)
        w43 = carve(1, 1, r2)
        vv = carve(2, 1, r2)
        xs_ = carve(3, 1, r2)
        xo = carve(4, 1, r2)
        ypre = carve(5, 2, lambda a: a.rearrange("p (r t) -> p r t", t=TT))
        ypb = carve(7, 1, lambda a: a.rearrange("p (r t) -> p r t", t=TT), BF16)
        gsig = carve(8, 1)
        qd = carve(0, 1, lambda a: a.rearrange("p (r t) -> p r t", t=TT), BF16)
        kd = carve(1, 1, lambda a: a[:, 0:768].rearrange("p (j h d) -> p j h d", j=4, h=4), BF16)
        yr = carve(2, 1, lambda a: a[:, 0:384])
        ysq = carve(3, 1, lambda a: a[:, 0:384])
        ycb = carve(4, 1, lambda a: a[:, 0:384], BF16)
        At = carve(5, 1, lambda a: a[:, 0:64], BF16)
        st4 = carve(6, 1, lambda a: a[:, 0:16].rearrange("p (a b) -> p a b", b=4))
        relu_t = carve(0, 1)
        relu_ts = [relu_t, carve(10, 1)]
        pT = carve(1, 1, lambda a: a[:, 0:768].rearrange("p (g c t) -> p g c t", g=2, c=3), BF16)
        pvs = carve(2, 1, lambda a: a[:, 0:390].rearrange("p (h e) -> p h e", e=65))
        ybt = carve(3, 1, lambda a: a[:, 0:384], BF16)
        m01 = carve(4, 1, lambda a: a[:, 0:128], BF16)
        mT = carve(5, 1, lambda a: a[:, 0:128], BF16)
        bis = carve(6, 1, lambda a: a[:, 0:8])
        lo, mid, nmid, ssA, ssB, gcol, rden6 = bis[:, 0:1], bis[:, 1:2], bis[:, 2:3], bis[:, 3:4], bis[:, 4:5], bis[:, 5:6], None
        rden = carve(7, 1, lambda a: a[:, 0:6])
        ot = carve(8, 2, lambda a: a[:, 0:768])
        junk = sq.v(lambda a: a.rearrange("p k t -> p (k t)"))
        pT2 = sb("pT2", [128, 768], BF16).v(lambda a: a.rearrange("p (g c t) -> p g c t", g=2, c=3))
        mT2 = sb("mT2", [128, 128], BF16)
        pTs, mTs = [pT, pT2], [mT, mT2]
        bsx = st.enter_context(nc.sbuf_tensor("bsx", [128, 8], F32))[:] if "bsx" not in _sbc else None
        if bsx is not None:
            _sbc["bsx"] = bsx
        bsx = _sbc["bsx"]
        nmid, nlo, gcol = T(bsx[:, 0:1]), T(bsx[:, 1:2]), T(bsx[:, 2:3])
        accs = [T(bsx[:, 3:5]), T(bsx[:, 5:7])]
        gA = carve(0, 1)
        gB = carve(1, 1)
        gC = carve(2, 1)
        rl = carve(3, 1, None, BF16)[:, 0:TT]
        NWB = 2
        WBW = 4096
        wbuf = [sb("wbuf%d" % i, [128, WBW], BF16) for i in range(NWB)]
        wctr = [0]
        wpos = [0]

        def wnext(name):
            nm, s_, nkt, cols = PLAN[wpos[0]]
            off = PLAN_OFF[wpos[0]]
            assert nm == name, (nm, name)
            wpos[0] += 1
            n = len(cols)
            b = wbuf[wctr[0] % NWB]
            wctr[0] += 1
            v = b.v(lambda a: a[:, 0:nkt * n])
            regs = tuple(rg for (c0, c1, rg) in wchunks[cur[0]] if c0 < off + nkt * n and c1 > off)
            S.dma("sp", v, T(wf_b[cur[0] * 128:(cur[0] + 1) * 128, off:off + nkt * n], regs))
            return v.v(lambda a: a.rearrange("p (k n) -> p k n", n=n))

        pbank = [T(st.enter_context(nc.psum_tensor("ps%d" % i, [128, 512], F32))[:]) for i in range(8)]
        prot = [0]
        for b_ in pbank:
            S.psum_regs.add(id(b_.reg))
        if os.environ.get('FENCE'):
            fnc = st.enter_context(nc.sbuf_tensor("fnc", [128, 4], F32))[:]
            S.fence = fnc[0:1, 0:1]
        if os.environ.get('DUMMY'):
            dmy = st.enter_context(nc.sbuf_tensor("dmy", [128, 4], F32))[:]
            S.memset("pool", T(dmy), 0.0)
            S.act_dummy = (dmy[:, 0:1], dmy[:, 2:3])

        def ps():
            b = pbank[prot[0] % 6]
            prot[0] += 1
            return b
        PD0, PD1 = pbank[6], pbank[7]

        TWO_PI = 2.0 * math.pi

        C1 = 6.28125
        C2 = 4058.0 / 2 ** 21
        C3 = TWO_PI - C1 - C2
        PI_LO = 3.1415925

        def sincos(out_sin, out_cos, angle, tmp_t, ki_t, kf_t):
            S.ts("dve", tmp_t, angle, 1.0 / TWO_PI, ALU.mult)
            _ce = "dve" if os.environ.get('CVT_DVE') else "pool"
            S.copy(_ce, ki_t, tmp_t)
            S.copy(_ce, kf_t, ki_t)
            S.stt("dve", tmp_t, kf_t, -C1, angle, ALU.mult, ALU.add)
            S.stt("dve", tmp_t, kf_t, -C2, tmp_t, ALU.mult, ALU.add)
            S.stt("dve", tmp_t, kf_t, -C3, tmp_t, ALU.mult, ALU.add)
            S.ts("dve", tmp_t, tmp_t, -PI_LO, ALU.max, PI_LO, ALU.min)
            S.actf(out_sin, tmp_t, AF.Sin)
            S.ts("dve", kf_t, tmp_t, 0.5 * math.pi, ALU.is_gt, -TWO_PI, ALU.mult)
            S.stt("dve", tmp_t, tmp_t, 0.5 * math.pi, kf_t, ALU.add, ALU.add)
            S.ts("dve", tmp_t, tmp_t, -PI_LO, ALU.max, PI_LO, ALU.min)
            S.actf(out_cos, tmp_t, AF.Sin)

        for l in range(nlayers):
            cur[0] = l
            S.dma("sp", Pt, T(P_in[l * 128:(l + 1) * 128, :]))
            x_src = x_in if l == 0 else xmid
            y_dst = y_out if l == nlayers - 1 else xmid
            s5 = sb("s5p", [128, 12, 8])
            lre, lim, lst = pc("lre"), pc("lim"), pc("lst")
            stp, are, th, rr, ct_, st_, lbr, lbi, den, fre, fim, tmp = [s5[:, i, :] for i in range(12)]
            S.actf(stp, lst, AF.Exp)
            S.tt("dve", are, lre, stp, ALU.mult)
            S.tt("dve", th, lim, stp, ALU.mult)
            S.actf(rr, are, AF.Exp)
            s5i = sb("s5i", [128, 8], I32)
            s5f = sb("s5f", [128, 8])
            sincos(st_, ct_, th, tmp, s5i, s5f)
            S.tt("dve", lbr, rr, ct_, ALU.mult)
            S.tt("dve", lbi, rr, st_, ALU.mult)
            S.tt("dve", den, lre, lre, ALU.mult)
            S.tt("dve", tmp, lim, lim, ALU.mult)
            S.tt("dve", den, den, tmp, ALU.add)
            S.op("dve", lambda v: v.reciprocal(den.ap, den.ap), reads=[den], writes=[den])
            S.ts("dve", lbr, lbr, -1.0, ALU.add)
            S.tt("dve", fre, lbr, lre, ALU.mult)
            S.tt("dve", tmp, lbi, lim, ALU.mult)
            S.tt("dve", fre, fre, tmp, ALU.add)
            S.tt("dve", fre, fre, den, ALU.mult)
            S.tt("dve", fim, lbi, lre, ALU.mult)
            S.tt("dve", tmp, lbr, lim, ALU.mult)
            S.tt("dve", fim, fim, tmp, ALU.subtract)
            S.tt("dve", fim, fim, den, ALU.mult)
            bbr = carve(4, 1, lambda a: a[:, 0:128].rearrange("p (k c) -> p k c", c=16))
            bbi = carve(5, 1, lambda a: a[:, 0:128].rearrange("p (k c) -> p k c", c=16))
            tb = carve(6, 1, lambda a: a[:, 0:16])
            k16 = lambda a: a.rearrange("p (k c) -> p k c", c=16)
            bre, bim, cre, cim = pc("bre").v(k16), pc("bim").v(k16), pc("cre").v(k16), pc("cim").v(k16)
            BT = sb("BT", [128, 8, 2, 128])
            CT = sb("CT", [128, 8, 2, 128])
            S.memset("pool", CT, 0.0)
            pad = carve(7, 1, lambda a: a[:, 0:256].rearrange("p (r c) -> p r c", c=128))
            for k in range(8):
                S.ts("dve", bbr[:, k, :], bre[:, k, :], fre[:, k:k + 1], ALU.mult)
                S.ts("dve", tb, bim[:, k, :], fim[:, k:k + 1], ALU.mult)
                S.tt("dve", bbr[:, k, :], bbr[:, k, :], tb, ALU.subtract)
                S.ts("dve", bbi[:, k, :], bim[:, k, :], fre[:, k:k + 1], ALU.mult)
                S.ts("dve", tb, bre[:, k, :], fim[:, k:k + 1], ALU.mult)
                S.tt("dve", bbi[:, k, :], bbi[:, k, :], tb, ALU.add)
                c0 = 32 * (k % 4)
                S.memset("pool", pad, 0.0)
                for ri, src in enumerate((bbr, bbi)):
                    S.copy("pool", pad[0:64, ri, c0:c0 + 16], src[0:64, k, :])
                    S.copy("pool", pad[64:128, ri, c0 + 16:c0 + 32], src[64:128, k, :])
                for ri in range(2):
                    pb = ps()
                    S.tr(pb[:, 0:128], pad[:, ri, :], ident)
                    S.copy("dve", BT[:, k, ri, :], pb[:, 0:128])
                S.copy("pool", CT[0:64, k, 0, c0:c0 + 16], cre[0:64, k, :])
                S.copy("pool", CT[64:128, k, 0, c0 + 16:c0 + 32], cre[64:128, k, :])
                S.ts("dve", CT[0:64, k, 1, c0:c0 + 16], cim[0:64, k, :], -1.0, ALU.mult)
                S.ts("dve", CT[64:128, k, 1, c0 + 16:c0 + 32], cim[64:128, k, :], -1.0, ALU.mult)
            CS = sb("CS", [128, 8, 2, TC])
            for k in range(8):
                S.ts("dve", ropa[:, 0:TC], pc("ramp"), th[:, k:k + 1], ALU.mult)
                sincos(CS[:, k, 1, :], CS[:, k, 0, :], ropa[:, 0:TC], ropb[:, 0:TC], posi[:, 0:TC], posf[:, 0:TC])
            xprev = sb("xprev", [128, 8, 2])
            S.memset("pool", xprev, 0.0)
            Sst = sb("Sst", [128, 2, 96])
            Sbf = sb("Sbf", [128, 2, 96], BF16)
            S.memset("pool", Sst, 0.0)
            S.memset("pool", Sbf, 0.0)
            ogf = sb("ogf", [128, 8])
            omf = sb("omf", [128, 1])
            S.ts("dve", ogf, pc("gf"), pc("flag"), ALU.mult)
            S.ts("dve", omf, pc("flag"), -1.0, ALU.mult, 1.0, ALU.add)
            print("sbuf left after alloc:", nc.sbuf_bytes_remaining)

            def rms_stats():
                S.actf(sq, xT, AF.Square)
                pb = ps()
                for kt in range(8):
                    S.mm(pb, onesb, sq[:, kt, :], kt == 0, kt == 7)
                S.actf(rstd, pb, AF.Sqrt, bias=EPS, scale=1.0 / D)
                S.op("dve", lambda v: v.reciprocal(rstd.ap, rstd.ap), reads=[rstd], writes=[rstd])

            def rmsnorm(gname):
                rms_stats()
                for kt in range(8):
                    S.stt("dve" if kt % 2 == 0 else "pool", hT[:, kt, :], xT[:, kt, :], pc(gname, kt, kt + 1), rstd, ALU.mult, ALU.mult)

            evac_flip = [0]

            def evac_eng():
                evac_flip[0] += 1
                return "dve"

            hk = lambda kt: hT[:, kt, :]

            def fm_mm(wv, col0, m, rhs_of_kt, nkt=8, po=0):
                pb = ps()
                for kt in range(nkt):
                    S.mm(pb[po:po + m, :], wv[:, kt, col0:col0 + m], rhs_of_kt(kt), kt == 0, kt == nkt - 1)
                return pb

            def roped(wv, zc, sc, m, ty, out_t, po=0):
                pz = fm_mm(wv, zc, m, hk, po=po)
                pz2 = fm_mm(wv, sc, m, hk, po=po)
                S.tt("dve", ropa[po:po + m, :], pz[po:po + m, :], rope[po:po + m, 2 * ty, :], ALU.mult)
                S.tt("dve", ropb[po:po + m, :], pz2[po:po + m, :], rope[po:po + m, 2 * ty + 1, :], ALU.mult)
                S.tt("pool", out_t, ropa[po:po + m, :], ropb[po:po + m, :], ALU.add)

            class _Stop(Exception):
                pass

            def kstop(tag):
                if os.environ.get('KSTOP') == tag:
                    raise _Stop()
            for ti in range(ntiles):
              try:
                    t0 = ti * TT
                    wpos[0] = 0
                    S.dma("sp", stg, T(x_src[t0:t0 + TT, :].rearrange("(j p) f -> p j f", p=128), xmr[ti] if l > 0 else None))
                    for j in range(4):
                        for kt in range(0, 8, 4):
                            pb = ps()
                            for q in range(4):
                                S.tr(pb[:, q * 128:(q + 1) * 128], stg[:, j, (kt + q) * 128:(kt + q + 1) * 128], ident)
                            S.copy(evac_eng(), xT[:, kt:kt + 4, j * 128:(j + 1) * 128],
                                   pb.v(lambda a: a.rearrange("p (q t) -> p q t", t=128)))
                    S.dma("sp", posi, T(pos_in[0:1, t0:t0 + TT].partition_broadcast(128)))
                    S.copy("dve", posf, posi)
                    for ty, (inv, sgn) in enumerate((("invD", "sgnD"), ("invI", "sgnI"), ("invR", "sgnR"))):
                        S.ts("dve", ang, posf, pc(inv), ALU.mult)
                        sincos(rope[:, 2 * ty + 1, :], rope[:, 2 * ty, :], ang, ropa, posi, ropb)
                        S.ts("dve", rope[:, 2 * ty + 1, :], rope[:, 2 * ty + 1, :], pc(sgn), ALU.mult)
                    rmsnorm("g1")

                    wv = wnext("u")
                    for g in range(2):
                        pb = fm_mm(wv, 128 * g, 128, hk)
                        S.copy("dve", uT[:, g, :], pb)
                    for i in range(3):
                        wv = wnext("dq%d" % i)
                        for hh in range(2):
                            roped(wv, 128 * hh, 128 * hh + 64, 64, 0, qT[:, 2 * i + hh, :])
                    wv = wnext("dkik")
                    roped(wv, 0, 64, 64, 0, KT[0:64, t0:t0 + TT].wr(KTr[ti]))
                    roped(wv, 128, 160, 32, 1, KT[64:96, t0:t0 + TT].wr(KTr[ti]), po=64)
                    wv = wnext("iq")
                    for h in range(4):
                        roped(wv, 64 * h, 64 * h + 32, 32, 1, iqT[64:96, h, :], po=64)
                    for i in range(2):
                        wv = wnext("rq%d" % i)
                        roped(wv, 0, 128, 128, 2, rqT[:, i, :])
                    for i in range(2):
                        wv = wnext("rk%d" % i)
                        roped(wv, 0, 128, 128, 2, rkT[:, i, :])
                    wv = wnext("vw")
                    for j in range(4):
                        pb = ps()
                        for kt in range(8):
                            S.mm(pb[:, 0:68], hT[:, kt, j * 128:(j + 1) * 128], wv[:, kt, :], kt == 0, kt == 7)
                        S.copy("dve", VC[:, ti * 4 + j, 0:64].wr(VCr[ti]), pb[:, 0:64])
                        S.ts("dve", iw[:, j, :], pb[:, 64:68], 0.5 * 32 ** -0.5, ALU.mult)
                    wv = wnext("rv")
                    for j in range(4):
                        pb = ps()
                        for kt in range(8):
                            S.mm(pb[:, 0:384], hT[:, kt, j * 128:(j + 1) * 128], wv[:, kt, :], kt == 0, kt == 7)
                        S.copy("dve", rv[:, j, :], pb[:, 0:384])
                    wv = wnext("rg")
                    for j in range(4):
                        pb = ps()
                        for kt in range(8):
                            S.mm(pb[:, 0:384], hT[:, kt, j * 128:(j + 1) * 128], wv[:, kt, :], kt == 0, kt == 7)
                        S.actf(rgs[:, j, :], pb[:, 0:384], AF.Silu)

                    for ch in range(TT // TC):
                        cs = slice(ch * TC, (ch + 1) * TC)
                        for ct in range(2):
                            yacc = PD0 if ct == 0 else PD1
                            for kk in range(4):
                                k = 4 * ct + kk
                                pb = ps()
                                for ri in range(2):
                                    S.mm(pb[:, ri * TC:(ri + 1) * TC], BT[:, k, ri, :], uT[:, ct, cs], True, True)
                                bu = pb.v(r2)
                                S.tt("dve", w12, bu, CS[:, k, :, :], ALU.mult)
                                S.tt("dve", w43[:, 0, :], bu[:, 0, :], CS[:, k, 1, :], ALU.mult)
                                S.tt("dve", w43[:, 1, :], bu[:, 1, :], CS[:, k, 0, :], ALU.mult)
                                S.tt("pool", vv[:, 0, :], w12[:, 0, :], w12[:, 1, :], ALU.add)
                                S.tt("pool", vv[:, 1, :], w43[:, 1, :], w43[:, 0, :], ALU.subtract)
                                for ri in range(2):
                                    S.op("dve", lambda v, ri=ri, k=k: v.tensor_tensor_scan(
                                        xs_.ap[:, ri, :], rr.ap[:, k:k + 1].to_broadcast([128, TC]), vv.ap[:, ri, :],
                                        xprev.ap[:, k, ri:ri + 1], ALU.mult, ALU.add),
                                        reads=[rr, vv, xprev], writes=[xs_])
                                S.tt("pool", w12, xs_, CS[:, k, :, :], ALU.mult)
                                S.tt("pool", w43[:, 0, :], xs_[:, 0, :], CS[:, k, 1, :], ALU.mult)
                                S.tt("pool", w43[:, 1, :], xs_[:, 1, :], CS[:, k, 0, :], ALU.mult)
                                S.tt("dve", xo[:, 0, :], w12[:, 0, :], w12[:, 1, :], ALU.subtract)
                                S.tt("dve", xo[:, 1, :], w43[:, 0, :], w43[:, 1, :], ALU.add)
                                S.copy("act", xprev[:, k, :], xo[:, :, TC - 1])
                                for ri in range(2):
                                    S.mm(yacc[:, 0:TC], CT[:, k, ri, :], xo[:, ri, :], kk == 0 and ri == 0, kk == 3 and ri == 1)
                            yv = ypre[:, ct, cs]
                            S.stt("dve", yv, uT[:, ct, cs], pc("ssd", ct, ct + 1), yacc[:, 0:TC], ALU.mult, ALU.add)
                            S.tt("pool", w12[:, 0, :], yv, yv, ALU.mult)
                            S.ts("dve", w12[:, 0, :], w12[:, 0, :], 0.044715, ALU.mult, 1.0, ALU.add)
                            S.tt("pool", w12[:, 0, :], w12[:, 0, :], yv, ALU.mult)
                            S.actf(w12[:, 1, :], w12[:, 0, :], AF.Sigmoid, scale=2.0 * math.sqrt(2.0 / math.pi))
                            S.tt("dve", yv, yv, w12[:, 1, :], ALU.mult)
                    S.copy("act", ypb, ypre)
                    wv = wnext("glu")
                    for ct in range(2):
                        pb = fm_mm(wv, 128 * ct, 128, lambda kt: ypb[:, kt, :], nkt=2)
                        S.actf(gsig, pb, AF.Sigmoid, bias=pc("glub", ct, ct + 1))
                        S.tt("dve", yaT[:, ct, :], ypre[:, ct, :], gsig, ALU.mult)
                    if debug:
                        debug[0](S, locals(), dbg_out, ti, "s5")

                    _skipret = ti >= 1 and 'ret' in os.environ.get('KSKIP', '')
                    c64 = lambda a: a.rearrange("p (c t) -> p c t", t=64)
                    for i in range(2):
                        S.tt("pool", qd[:, i, :].v(c64), rqT[:, i, :].v(c64),
                             pc("qdec", 64 * i, 64 * i + 64).v(lambda a: a.unsqueeze(1).to_broadcast([128, 8, 64])), ALU.mult)
                    for j in range(4):
                        pb = ps()
                        pbv = pb.v(lambda a: a.bitcast(BF16))
                        for h in range(4):
                            base = 64 * (h % 2)
                            S.tr(pbv[:, 64 * h:64 * h + 48], rkT[base:base + 48, h // 2, j * 128:(j + 1) * 128], identb[base:base + 48, base:base + 48])
                        for h in range(4):
                            S.ts("dve", kd[:, j, h, :], pbv[:, 64 * h:64 * h + 48], pc("kdec", h, h + 1), ALU.mult)
                    for j in range(4):
                        for half in range(2):
                            rb = 64 * half
                            cs0 = j * 128 + rb
                            pout = pbank[4 + half] if not os.environ.get('POUT_PD1') else (PD1 if os.environ.get('POUT_PD1') == '1' else pbank[4])
                            for h in range(4):
                                base = 64 * (h % 2)
                                pair = h // 2
                                pa = pbank[(2 * h) % 4]
                                S.mm(pa[rb:rb + 64, 0:64], rkT[base:base + 48, pair, cs0:cs0 + 64], rqT[base:base + 48, pair, cs0:cs0 + 64], True, True)
                                kstop('r%d%d_h%d_s0' % (j, half, h))
                                S.tt("dve", At[rb:rb + 64, :], pa[rb:rb + 64, 0:64], pc("intra", 64 * h, 64 * h + 64)[rb:rb + 64, :], ALU.mult)
                                kstop('r%d%d_h%d_s1' % (j, half, h))
                                if os.environ.get('FAKEDEP'):
                                    S.op("pe", lambda p, h=h, rb=rb: p.matmul(pout.ap[rb:rb + 64, 96 * h:96 * h + 96], lhsT=At.ap[rb:rb + 64, :], rhs=rv.ap[rb:rb + 64, j, 96 * h:96 * h + 96], start=True, stop=False), reads=[At, rv, yr], writes=[pout])
                                else:
                                    S.mm(pout[rb:rb + 64, 96 * h:96 * h + 96], At[rb:rb + 64, :], rv[rb:rb + 64, j, 96 * h:96 * h + 96], True, False)
                                kstop('r%d%d_h%d_s2' % (j, half, h))
                                S.mm(pout[rb:rb + 64, 96 * h:96 * h + 96], qd[base:base + 48, pair, cs0:cs0 + 64], Sbf[base:base + 48, pair, :], False, True)
                                kstop('r%d%d_h%d_s3' % (j, half, h))
                                pu = pbank[(2 * h + 1) % 4]
                                S.mm(pu[base:base + 48, 0:96], kd[rb:rb + 64, j, h, :], rv[rb:rb + 64, j, 96 * h:96 * h + 96], True, True)
                                kstop('r%d%d_h%d_s4' % (j, half, h))
                                S.stt("dve", Sst[base:base + 48, pair, :], Sst[base:base + 48, pair, :], CONST["cdec"][h], pu[base:base + 48, 0:96], ALU.mult, ALU.add)
                                kstop('r%d%d_h%d_s5' % (j, half, h))
                                S.copy("act", Sbf[base:base + 48, pair, :], Sst[base:base + 48, pair, :])
                                kstop('r%d%d_h%d_s6' % (j, half, h))
                            S.copy("dve", yr[rb:rb + 64, :], pout[rb:rb + 64, 0:384])
                            kstop('ret_%d_%d' % (j, half))
                        yr3 = yr.v(lambda a: a.rearrange("p (h e) -> p h e", e=96))
                        S.op("dve", lambda v: v.tensor_reduce(st4.ap[:, 0, :], yr3.ap, AX.X, ALU.add), reads=[yr], writes=[st4])
                        S.tt("pool", ysq, yr, yr, ALU.mult)
                        S.op("dve", lambda v: v.tensor_reduce(st4.ap[:, 1, :], ysq.ap.rearrange("p (h e) -> p h e", e=96), AX.X, ALU.add), reads=[ysq], writes=[st4])
                        S.ts("dve", st4[:, 2, :], st4[:, 0, :], 1.0 / 96, ALU.mult)
                        S.tt("dve", st4[:, 0, :], st4[:, 2, :], st4[:, 2, :], ALU.mult)
                        S.stt("dve", st4[:, 3, :], st4[:, 1, :], 1.0 / 96, st4[:, 0, :], ALU.mult, ALU.subtract)
                        S.actf(st4[:, 3, :], st4[:, 3, :], AF.Sqrt, bias=EPS)
                        S.op("dve", lambda v: v.reciprocal(st4.ap[:, 3, :], st4.ap[:, 3, :]), reads=[st4], writes=[st4])
                        bc = lambda c: st4[:, c, :].v(lambda a: a.unsqueeze(2).to_broadcast([128, 4, 96]))
                        S.tt("dve", yr3, yr3, bc(2), ALU.subtract)
                        S.tt("dve", yr3, yr3, bc(3), ALU.mult)
                        S.tt("pool", yr, yr, pc("retg"), ALU.mult)
                        S.tt("pool", ycb, yr, rgs[:, j, :], ALU.mult)
                        pb = ps()
                        pbv = pb.v(lambda a: a.bitcast(BF16))
                        for c3 in range(3):
                            S.tr(pbv[:, 128 * c3:128 * c3 + 128], ycb[:, 128 * c3:128 * c3 + 128], identb)
                        S.copy("dve", ycT[:, :, j * 128:(j + 1) * 128], pbv[:, 0:384].v(lambda a: a.rearrange("p (c t) -> p c t", t=128)))
                    if debug:
                        debug[0](S, locals(), dbg_out, ti, "ret")

                    for j in range(4 if ti == 0 else int(os.environ.get('KDSAJ', '4'))):
                        qb = ti * 4 + j
                        nkb = qb + 1
                        nk = nkb * 128
                        qs = slice(j * 128, (j + 1) * 128)
                        for k0 in range(0, nk, 512):
                            kw = min(512, nk - k0)
                            tl = [KTr[t] for t in range(k0 // TT, (k0 + kw - 1) // TT + 1)]
                            for h in range(4):
                                pb = ps()
                                S.op("pe", lambda p, pb=pb, h=h, k0=k0, kw=kw: p.matmul(
                                    pb.ap[:, 0:kw], lhsT=iqT.ap[64:96, h, qs], rhs=KT.ap[64:96, k0:k0 + kw], start=True, stop=True),
                                    reads=[iqT] + tl, writes=[pb])
                                if h == 0:
                                    S.actf(big[:, k0:k0 + kw], pb[:, 0:kw], AF.Relu)
                                    S.ts("dve", big[:, k0:k0 + kw], big[:, k0:k0 + kw], iw[:, j, 0:1], ALU.mult)
                                else:
                                    rt_ = relu_ts[h % 2]
                                    S.actf(rt_[:, 0:kw], pb[:, 0:kw], AF.Relu)
                                    S.stt("dve", big[:, k0:k0 + kw], rt_[:, 0:kw], iw[:, j, h:h + 1], big[:, k0:k0 + kw], ALU.mult, ALU.add)
                        S.tt("pool", big[:, nk - 128:nk], big[:, nk - 128:nk], pc("diag"), ALU.add)
                        S.memset("dve", nmid, -(BIS_LO + BIS_W / 2))
                        S.memset("dve", accs[0], 0.0)
                        thr_s = float(2 * TOPK - nk)
                        w_i = BIS_W / 2
                        nA = min(nk, 4096)
                        for it in range(NBIS):
                            acc = accs[it % 2]
                            S.actf(junk[:, 0:nA], big[:, 0:nA], AF.Sign, bias=nmid, accum=acc[:, 0:1])
                            if nk > nA:
                                S.actf(junk[:, 0:nk - nA], big[:, nA:nk], AF.Sign, bias=nmid, accum=acc[:, 1:2])
                            S.memset("dve", accs[(it + 1) % 2], 0.0)
                            if nk > nA:
                                S.tt("dve", acc[:, 0:1], acc[:, 0:1], acc[:, 1:2], ALU.add)
                            S.ts("dve", gcol, acc[:, 0:1], thr_s, ALU.is_ge, -w_i, ALU.mult)
                            if it < NBIS - 1:
                                S.stt("dve", nmid, gcol, w_i / 2, nmid, ALU.add, ALU.add)
                            else:
                                S.stt("dve", nlo, gcol, w_i, nmid, ALU.add, ALU.add)
                            w_i = w_i / 2
                        for kb in range(nkb + 1):
                            if kb < nkb:
                                ks = slice(kb * 128, (kb + 1) * 128)
                                kreg = KTr[kb // 4]
                                pTc, mTc = pTs[kb % 2], mTs[kb % 2]
                                S.ts("dve", m01, big[:, ks], nlo, ALU.add, 0.0, ALU.is_ge)
                                pm = ps()
                                pmv = pm.v(lambda a: a.bitcast(BF16))
                                S.tr(pmv[:, 0:128], m01, identb)
                                S.copy("dve", mTc, pmv[:, 0:128])
                                for g in range(2):
                                    pp = ps()
                                    S.op("pe", lambda p, pp=pp, g=g, ks=ks: p.matmul(
                                        pp.ap[:, 0:384], lhsT=KT.ap[0:64, ks], rhs=qT.ap[0:64, 3 * g:3 * g + 3, qs], start=True, stop=True),
                                        reads=[qT, kreg], writes=[pp])
                                    S.actf(pTc[:, g, :, :], pp[:, 0:384].v(lambda a: a.rearrange("p (c t) -> p c t", t=128)), AF.Exp, scale=0.125)
                                p6 = pTc.v(lambda a: a.rearrange("p g c t -> p (g c) t"))
                                S.tt("pool", p6, p6, mTc.v(lambda a: a.unsqueeze(1).to_broadcast([128, 6, 128])), ALU.mult)
                            if kb >= 1:
                                kp = kb - 1
                                pTp = pTs[kp % 2]
                                for g in range(2):
                                    PDg = PD0 if g == 0 else PD1
                                    S.op("pe", lambda p, g=g, kp=kp, PDg=PDg, pTp=pTp: p.matmul(
                                        PDg.ap[0:96, 0:384], lhsT=VC.ap[:, kp, :], rhs=pTp.ap[:, g, :, :],
                                        start=(kp == 0), stop=(kp == nkb - 1)),
                                        reads=[pTp, VCr[kp // 4]], writes=[PDg], acc=(kp != 0))
                        S.copy("dve", ot[0:96, 0:384], PD0[0:96, 0:384])
                        S.copy("dve", ot[0:96, 384:768], PD1[0:96, 0:384])
                        po = [ps(), ps()]
                        for h in range(6):
                            S.tr(po[h // 3][:, 96 * (h % 3):96 * (h % 3) + 96], ot[0:96, 128 * h:128 * h + 128], ident[0:96, 0:96])
                        for g in range(2):
                            S.copy("dve", pvs[:, 3 * g:3 * g + 3, :], po[g][:, 0:288].v(lambda a: a.rearrange("p (c e) -> p c e", e=96))[:, :, 0:65])
                        S.op("dve", lambda v: v.reciprocal(rden.ap, pvs.ap[:, :, 64]), reads=[pvs], writes=[rden])
                        S.tt("dve", ybt.v(lambda a: a.rearrange("p (h e) -> p h e", e=64)), pvs[:, :, 0:64],
                             rden.v(lambda a: a.unsqueeze(2).to_broadcast([128, 6, 64])), ALU.mult)
                        pb = ps()
                        pbv = pb.v(lambda a: a.bitcast(BF16))
                        for c3 in range(3):
                            S.tr(pbv[:, 128 * c3:128 * c3 + 128], ybt[:, 128 * c3:128 * c3 + 128], identb)
                        S.copy("dve", ybT[:, :, qs], pbv[:, 0:384].v(lambda a: a.rearrange("p (c t) -> p c t", t=128)))
                    if debug:
                        debug[0](S, locals(), dbg_out, ti, "dsa")

                    mrg = sq
                    srcs = [(yaT, 0, 2), (ybT, 2, 3), (ycT, 5, 3)]
                    for ft in range(8):
                        wp = wnext("pr%d" % ft)
                        pP = []
                        for bi, (yt, k0, nk_) in enumerate(srcs):
                            pb = ps()
                            for kt in range(nk_):
                                S.mm(pb, wp[:, k0 + kt, :], yt[:, kt, :], kt == 0, kt == nk_ - 1)
                            pP.append(pb)
                        wg = wnext("gt%d" % ft)
                        for bi, gt in enumerate((gA, gB, gC)):
                            pb = fm_mm(wg, 128 * bi, 128, hk)
                            S.actf(gt, pb, AF.Sigmoid)
                        S.tt("dve", gA, gA, pP[0], ALU.mult)
                        S.tt("dve", gB, gB, pP[1], ALU.mult)
                        S.tt("dve", gC, gC, pP[2], ALU.mult)
                        S.tt("pool", gA, gA, gB, ALU.add)
                        S.tt("pool", mrg[:, ft, :], gA, gC, ALU.add)
                    for ft in range(8):
                        wv = wnext("wo%d" % ft)
                        pb = fm_mm(wv, 0, 128, lambda kt: mrg[:, kt, :])
                        S.tt("dve", xT[:, ft, :], xT[:, ft, :], pb, ALU.add)
                    if debug:
                        debug[0](S, locals(), dbg_out, ti, "mix")

                    rmsnorm("g2")
                    for f4 in range(8):
                        wv = wnext("w1_%d" % f4)
                        for q in range(4):
                            f = f4 * 4 + q
                            pb = fm_mm(wv, 128 * q, 128, hk)
                            S.actf(rl, pb, AF.Relu)
                            S.tt("pool" if f % 2 else "dve", aT[:, f, :], rl, rl, ALU.mult)
                    for ft in range(8):
                        wv = wnext("w2_%d" % ft)
                        pb = fm_mm(wv, 0, 128, lambda kt: aT[:, kt, :], nkt=32)
                        S.tt("dve", xT[:, ft, :], xT[:, ft, :], pb, ALU.add)

                    rms_stats()
                    oT = xT
                    tmpo = rope.v(lambda a: a[:, 0:1, :].rearrange("p s t -> p (s t)"))
                    for kt in range(8):
                        S.stt("dve", tmpo, xT[:, kt, :], ogf[:, kt:kt + 1], rstd, ALU.mult, ALU.mult)
                        S.stt("pool", xT[:, kt, :], xT[:, kt, :], omf[:, 0:1], tmpo, ALU.mult, ALU.add)
                    for j in range(4):
                        for kt in range(0, 8, 4):
                            pb = ps()
                            for q in range(4):
                                S.tr(pb[:, q * 128:(q + 1) * 128], oT[:, kt + q, j * 128:(j + 1) * 128], ident)
                            S.copy(evac_eng(), stg[:, j, kt * 128:(kt + 4) * 128], pb)
                    S.dma("sp", T(y_dst[t0:t0 + TT, :].rearrange("(j p) f -> p j f", p=128), xmr[ti] if l < nlayers - 1 else None), stg)
              except _Stop:
                break
        S.finish()
        print("instructions:", S.nins, "sbuf left:", nc.sbuf_bytes_remaining)
    return nc


_PROG = {}


def _layer_arrays(inp, l, flag):
    ws = {"wa": _arrange_w_in(np.asarray(inp["w_in"][l], np.float32)),
          "wglu": np.asarray(inp["ssm_glu_w"][l], np.float32),
          "wpr": np.concatenate([inp["w_proj_a"][l], inp["w_proj_b"][l], inp["w_proj_c"][l]], axis=0).astype(np.float32),
          "wo": np.asarray(inp["w_out"][l], np.float32),
          "w1": np.asarray(inp["w_ff1"][l], np.float32),
          "w2": np.asarray(inp["w_ff2"][l], np.float32)}
    return _flat_weights(ws), _pack_params(inp, l, flag)


def _maps(inp, layers, xs, ntok, last_is_final=True):
    wfs, Ps = [], []
    for i, l in enumerate(layers):
        flag = 1.0 if (last_is_final and i == len(layers) - 1) else 0.0
        wf, P = _layer_arrays(inp, l, flag)
        wfs.append(wf)
        Ps.append(P)
    wf = np.concatenate(wfs, axis=0)
    P = np.concatenate(Ps, axis=0)
    return [{"x_in": np.ascontiguousarray(xs[b][:ntok], dtype=np.float32),
             "pos": np.ascontiguousarray(inp["positions"][b:b + 1, :ntok], dtype=np.int32),
             "P": P, "wf": wf} for b in range(len(xs))]


def kernel(**inputs):
    inp = {k: np.asarray(v) for k, v in inputs.items()}
    ntiles = SEQ // TT
    key = (ntiles, DEPTH)
    if key not in _PROG:
        _PROG[key] = build_program(ntiles, DEPTH)
    nc = _PROG[key]
    xs = [inp["x"][b] for b in range(BATCH)]
    maps = _maps(inp, list(range(DEPTH)), xs, SEQ)
    res = run_bass_kernel_spmd(nc, maps, core_ids=list(range(BATCH)))
    return np.stack([np.asarray(r["y"]) for r in res.results], axis=0).astype(np.float32)
```
